# Optimizing a Trainium2 kernel written in Bass

```python
import math
import jax, jax.numpy as jnp
from jax import lax
import numpy as np

D_MODEL = 2048
BATCH = 4
SEQ = 2048
DEPTH = 2
DEC_BATCH = 128
DEC_SEQ = 4
PAST_LEN = 16384
PAGE_SIZE = 128

D_S5 = D_MODEL // 2
S5_GROUP = 16
N_S5_GROUPS = D_S5 // S5_GROUP
S5_STATE = 64
DT_MIN = 1e-3
DT_MAX = 1e-1
D_RG = D_MODEL // 2
N_RG_HEADS = 16
RG_HEAD_DIM = D_RG // N_RG_HEADS
CONV_W = 4
RG_C = 8.0
D_IN = D_S5 + 2 * D_RG
D_FF = 3 * D_MODEL
N_EXPERTS = 8
TOP_K = 2
D_FF_EXPERT = D_FF // TOP_K
N_DENSE = (DEPTH + 1) // 2
N_MOE = DEPTH // 2
EPS = 1e-6

kernel_name = 'hybrid_s5_rglru_adaln_moe_step'


def rmsnorm(x, g):
    xf = x.astype(jnp.float32)
    y = xf * lax.rsqrt(jnp.mean(xf * xf, axis=-1, keepdims=True) + EPS)
    return (y * g.astype(jnp.float32)).astype(x.dtype)


def linear_scan(a, b, h0):
    b = b.at[:, 0].add(a[:, 0] * h0)
    def combine(left, right):
        a_l, b_l = left
        a_r, b_r = right
        return a_l * a_r, a_r * b_l + b_r
    _, h = lax.associative_scan(combine, (a, b), axis=1)
    return h


def s5_mixer(u, h0_re, h0_im, lam_re, lam_im, log_dt, b_re, b_im, c_re, c_im, d_skip, w_glu, b_glu):
    f32 = jnp.float32
    Bn, S, _ = u.shape
    uf = u.astype(f32).reshape(Bn, S, N_S5_GROUPS, S5_GROUP)
    lam = lax.complex(lam_re.astype(f32), lam_im.astype(f32))
    dt = jnp.exp(log_dt.astype(f32))[:, None]
    lam_bar = jnp.exp(lam * dt)
    b_bar = ((lam_bar - 1.0) / lam)[:, :, None] * lax.complex(b_re.astype(f32), b_im.astype(f32))
    bu = jnp.einsum('bsgh,gph->bsgp', uf, b_bar)
    h0 = lax.complex(h0_re.astype(f32), h0_im.astype(f32))
    h = linear_scan(jnp.broadcast_to(lam_bar, bu.shape), bu, h0)
    c_mat = lax.complex(c_re.astype(f32), c_im.astype(f32))
    y = jnp.einsum('bsgp,ghp->bsgh', h, c_mat).real + d_skip.astype(f32) * uf
    y = jax.nn.gelu(y.reshape(Bn, S, D_S5))
    y = y * jax.nn.sigmoid(y @ w_glu.astype(f32) + b_glu.astype(f32))
    h_last = h[:, -1]
    return y.astype(u.dtype), h_last.real, h_last.imag


def rglru_mixer(xr, gy, conv_buf, h0, conv_w, conv_b, w_a, b_a, w_i, b_i, lam):
    f32 = jnp.float32
    Bn, S, _ = xr.shape
    xpad = jnp.concatenate([conv_buf.astype(xr.dtype), xr], axis=1)
    xc = conv_b + sum(xpad[:, k:k + S] * conv_w[k] for k in range(CONV_W))
    new_buf = xpad[:, S:]
    xh = xc.astype(f32).reshape(Bn, S, N_RG_HEADS, RG_HEAD_DIM)
    r = jax.nn.sigmoid(jnp.einsum('bshi,hij->bshj', xh, w_a.astype(f32)) + b_a.astype(f32))
    i = jax.nn.sigmoid(jnp.einsum('bshi,hij->bshj', xh, w_i.astype(f32)) + b_i.astype(f32))
    log_a = -RG_C * r * jax.nn.softplus(-lam.astype(f32)).reshape(N_RG_HEADS, RG_HEAD_DIM)
    a = jnp.exp(log_a)
    b = jnp.sqrt(-jnp.expm1(2.0 * log_a)) * (i * xh)
    h = linear_scan(a.reshape(Bn, S, D_RG), b.reshape(Bn, S, D_RG), h0.astype(f32))
    y = h.astype(xr.dtype) * gy
    return y, h[:, -1], new_buf


def swiglu(h, w1, w3, w2):
    return (jax.nn.silu(h @ w1) * (h @ w3)) @ w2


def moe_swiglu(h, w_router, w1, w3, w2):
    Bn, S, D = h.shape
    t = h.reshape(Bn * S, D)
    logits = t.astype(jnp.float32) @ w_router.astype(jnp.float32)
    top_v, top_i = lax.top_k(logits, TOP_K)
    top_w = jax.nn.softmax(top_v, axis=-1)
    comb = jnp.sum(jax.nn.one_hot(top_i, N_EXPERTS, dtype=jnp.float32) * top_w[..., None], axis=1)
    def expert(acc, xs):
        w1e, w3e, w2e, ce = xs
        y = swiglu(t, w1e, w3e, w2e)
        return acc + ce[:, None] * y.astype(jnp.float32), None
    acc, _ = lax.scan(expert, jnp.zeros(t.shape, jnp.float32), (w1, w3, w2, comb.T))
    return acc.astype(h.dtype).reshape(Bn, S, D)


def setup_inputs(seed: int = 0) -> dict:
    key = jax.random.key(seed)
    ks = iter(jax.random.split(key, 64))
    f32 = jnp.float32
    def nrm(shape, scale):
        return jax.random.normal(next(ks), shape, f32) * scale
    D = D_MODEL
    G, P, H = N_S5_GROUPS, S5_STATE, S5_GROUP
    n_idx = jnp.arange(P, dtype=f32)
    u_a = jax.random.uniform(next(ks), (DEPTH, D_RG), f32, 0.81, 0.998)
    a_init = u_a ** (1.0 / RG_C)
    inp = {
        'x_prompt': nrm((BATCH, SEQ, D), 1.0),
        'x_sample': nrm((DEC_BATCH, DEC_SEQ, D), 1.0),
        'c_prompt': nrm((BATCH, D), 1.0),
        'c_sample': nrm((DEC_BATCH, D), 1.0),
        'state_s5_re': nrm((DEPTH, DEC_BATCH, G, P), 0.3),
        'state_s5_im': nrm((DEPTH, DEC_BATCH, G, P), 0.3),
        'state_rglru': nrm((DEPTH, DEC_BATCH, D_RG), 0.5),
        'state_conv': nrm((DEPTH, DEC_BATCH, CONV_W - 1, D_RG), 1.0),
        'norm_mix': 1.0 + nrm((DEPTH, D), 0.02),
        'norm_ffn': 1.0 + nrm((DEPTH, D), 0.02),
        'norm_f': 1.0 + nrm((D,), 0.02),
        'w_ada': nrm((DEPTH, D, 6 * D), 0.5 * D ** -0.5),
        'b_ada': nrm((DEPTH, 6 * D), 0.02),
        'w_in': nrm((DEPTH, D, D_IN), D ** -0.5),
        's5_lam_re': -0.5 + nrm((DEPTH, G, P), 0.01),
        's5_lam_im': math.pi * n_idx + nrm((DEPTH, G, P), 0.01),
        's5_log_dt': jax.random.uniform(next(ks), (DEPTH, G), f32, math.log(DT_MIN), math.log(DT_MAX)),
        's5_b_re': nrm((DEPTH, G, P, H), (2.0 * H) ** -0.5),
        's5_b_im': nrm((DEPTH, G, P, H), (2.0 * H) ** -0.5),
        's5_c_re': nrm((DEPTH, G, H, P), (2.0 * P) ** -0.5),
        's5_c_im': nrm((DEPTH, G, H, P), (2.0 * P) ** -0.5),
        's5_d': nrm((DEPTH, G, H), 1.0),
        's5_w_glu': nrm((DEPTH, D_S5, D_S5), D_S5 ** -0.5),
        's5_b_glu': nrm((DEPTH, D_S5), 0.02),
        'rg_conv_w': nrm((DEPTH, CONV_W, D_RG), CONV_W ** -0.5),
        'rg_conv_b': nrm((DEPTH, D_RG), 0.02),
        'rg_w_a': nrm((DEPTH, N_RG_HEADS, RG_HEAD_DIM, RG_HEAD_DIM), RG_HEAD_DIM ** -0.5),
        'rg_b_a': nrm((DEPTH, N_RG_HEADS, RG_HEAD_DIM), 0.02),
        'rg_w_i': nrm((DEPTH, N_RG_HEADS, RG_HEAD_DIM, RG_HEAD_DIM), RG_HEAD_DIM ** -0.5),
        'rg_b_i': nrm((DEPTH, N_RG_HEADS, RG_HEAD_DIM), 0.02),
        'rg_lam': jnp.log(a_init) - jnp.log1p(-a_init),
        'w_gate': nrm((DEPTH, D, 2 * D), D ** -0.5),
        'b_gate': nrm((DEPTH, 2 * D), 0.02),
        'w_br_s5': nrm((DEPTH, D_S5, D), D_S5 ** -0.5),
        'w_br_rg': nrm((DEPTH, D_RG, D), D_RG ** -0.5),
        'w_out': nrm((DEPTH, D, D), D ** -0.5),
        'ffn_w1': nrm((N_DENSE, D, D_FF), D ** -0.5),
        'ffn_w3': nrm((N_DENSE, D, D_FF), D ** -0.5),
        'ffn_w2': nrm((N_DENSE, D_FF, D), D_FF ** -0.5),
        'moe_router': nrm((N_MOE, D, N_EXPERTS), D ** -0.5),
        'moe_w1': nrm((N_MOE, N_EXPERTS, D, D_FF_EXPERT), D ** -0.5),
        'moe_w3': nrm((N_MOE, N_EXPERTS, D, D_FF_EXPERT), D ** -0.5),
        'moe_w2': nrm((N_MOE, N_EXPERTS, D_FF_EXPERT, D), D_FF_EXPERT ** -0.5),
    }
    return inp


def reference(x_prompt, x_sample, c_prompt, c_sample, state_s5_re, state_s5_im, state_rglru, state_conv,
              norm_mix, norm_ffn, norm_f, w_ada, b_ada, w_in,
              s5_lam_re, s5_lam_im, s5_log_dt, s5_b_re, s5_b_im, s5_c_re, s5_c_im, s5_d, s5_w_glu, s5_b_glu,
              rg_conv_w, rg_conv_b, rg_w_a, rg_b_a, rg_w_i, rg_b_i, rg_lam,
              w_gate, b_gate, w_br_s5, w_br_rg, w_out,
              ffn_w1, ffn_w3, ffn_w2, moe_router, moe_w1, moe_w3, moe_w2):
    D = D_MODEL

    def run(x, c, s5_re0, s5_im0, rg_h0, conv0):
        s5_re_out, s5_im_out, rg_out, conv_out = [], [], [], []
        cs = jax.nn.silu(c)
        for l in range(DEPTH):
            mod = (cs @ w_ada[l] + b_ada[l])[:, None, :]
            sh1, sc1, g1, sh2, sc2, g2 = jnp.split(mod, 6, axis=-1)
            h = rmsnorm(x, norm_mix[l]) * (1.0 + sc1) + sh1
            proj = h @ w_in[l]
            u_s5 = proj[..., :D_S5]
            xr = proj[..., D_S5:D_S5 + D_RG]
            gy = jax.nn.gelu(proj[..., D_S5 + D_RG:])
            y_s5, s5r, s5i = s5_mixer(u_s5, s5_re0[l], s5_im0[l], s5_lam_re[l], s5_lam_im[l], s5_log_dt[l],
                                      s5_b_re[l], s5_b_im[l], s5_c_re[l], s5_c_im[l], s5_d[l],
                                      s5_w_glu[l], s5_b_glu[l])
            y_rg, rgh, cbuf = rglru_mixer(xr, gy, conv0[l], rg_h0[l], rg_conv_w[l], rg_conv_b[l],
                                          rg_w_a[l], rg_b_a[l], rg_w_i[l], rg_b_i[l], rg_lam[l])
            gates = jax.nn.sigmoid(h @ w_gate[l] + b_gate[l])
            merged = gates[..., :D] * (y_s5 @ w_br_s5[l]) + gates[..., D:] * (y_rg @ w_br_rg[l])
            x = x + g1 * (merged @ w_out[l])
            h = rmsnorm(x, norm_ffn[l]) * (1.0 + sc2) + sh2
            if l % 2 == 0:
                y = swiglu(h, ffn_w1[l // 2], ffn_w3[l // 2], ffn_w2[l // 2])
            else:
                y = moe_swiglu(h, moe_router[l // 2], moe_w1[l // 2], moe_w3[l // 2], moe_w2[l // 2])
            x = x + g2 * y
            s5_re_out.append(s5r)
            s5_im_out.append(s5i)
            rg_out.append(rgh)
            conv_out.append(cbuf)
        y_final = rmsnorm(x, norm_f)
        return (y_final, jnp.stack(s5_re_out).astype(state_s5_re.dtype), jnp.stack(s5_im_out).astype(state_s5_im.dtype),
                jnp.stack(rg_out).astype(state_rglru.dtype), jnp.stack(conv_out).astype(state_conv.dtype))

    zeros_s5 = jnp.zeros((DEPTH, BATCH, N_S5_GROUPS, S5_STATE), jnp.float32)
    zeros_rg = jnp.zeros((DEPTH, BATCH, D_RG), jnp.float32)
    zeros_conv = jnp.zeros((DEPTH, BATCH, CONV_W - 1, D_RG), x_prompt.dtype)
    y_prompt, p_s5_re, p_s5_im, p_rg, p_conv = run(x_prompt, c_prompt, zeros_s5, zeros_s5, zeros_rg, zeros_conv)
    y_sample, s_s5_re, s_s5_im, s_rg, s_conv = run(x_sample, c_sample, state_s5_re, state_s5_im, state_rglru, state_conv)
    return (y_prompt, y_sample, p_s5_re, p_s5_im, p_rg, p_conv, s_s5_re, s_s5_im, s_rg, s_conv)
```

```python
import contextlib
import math
import numpy as np
import concourse.bass as bass
import concourse.mybir as mybir
from concourse.bass_utils import run_bass_kernel_spmd

F32 = mybir.dt.float32
BF16 = mybir.dt.bfloat16
I32 = mybir.dt.int32
AF = mybir.ActivationFunctionType
ALU = mybir.AluOpType

D = 2048
FT = 16
SEQ = 2048
NSAMP = 16
TS = 4
DEPTH = 2
NS_SMALL = 352
TWO_PI = 2.0 * math.pi
GELU_K = 1.5957691216057308


class Prog:
    ENG = ("pe", "act", "dve", "pool", "sp")

    def __init__(self, nc):
        self.nc = nc
        self.ops = []
        self.last_writer = {}
        self.readers = {}
        self.dma_key_count = {}
        self.dma_key_waitall = set()

    def op(self, eng, fn, reads=(), writes=(), dma_key=None, wait_all=False):
        ps_r = [k for k in reads if isinstance(k, tuple) and k and k[0] == "ps"]
        if ps_r:
            reads = [k for k in reads if k not in ps_r]
            writes = list(writes) + [k for k in ps_r if k not in writes]
        idx = len(self.ops)
        deps = set()
        war = set()
        for b in reads:
            w = self.last_writer.get(b)
            if w is not None:
                deps.add(w)
        for b in writes:
            w = self.last_writer.get(b)
            if w is not None:
                deps.add(w)
            for r in self.readers.get(b, {}).values():
                war.add(r)
        o = dict(eng=eng, fn=fn, deps=deps, war=war, dma_key=dma_key, signal=False)
        if dma_key is not None:
            self.dma_key_count[dma_key] = self.dma_key_count.get(dma_key, 0) + 1
            o["dma_n"] = self.dma_key_count[dma_key]
            if wait_all:
                self.dma_key_waitall.add(dma_key)
        self.ops.append(o)
        rk = eng if dma_key is None else ("dma", idx)
        for b in reads:
            self.readers.setdefault(b, {})[rk] = idx
        for b in writes:
            self.last_writer[b] = idx
            self.readers[b] = {}
        return idx

    def emit(self, final_wait_keys=()):
        nc = self.nc
        ops = self.ops
        for o in ops:
            nd = set()
            for d in o["deps"] | o["war"]:
                p = ops[d]
                if p["dma_key"] is None and o["dma_key"] is None and p["eng"] == o["eng"]:
                    if o["eng"] == "pe":
                        continue
                nd.add(d)
            o["deps"] = nd
            for d in nd:
                ops[d]["signal"] = True
        cnt = {e: 0 for e in self.ENG}
        for o in ops:
            if o["dma_key"] is not None:
                k = o["dma_key"]
                n = self.dma_key_count[k] if k in self.dma_key_waitall else o["dma_n"]
                o["sig"] = (("dma", k), 16 * n)
            elif o["signal"]:
                cnt[o["eng"]] += 1
                o["sig"] = (("eng", o["eng"]), cnt[o["eng"]])
        per_eng = {e: [o for o in ops if o["eng"] == e] for e in self.ENG}
        sem_names = [("eng", e) for e in self.ENG] + [("dma", k) for k in self.dma_key_count]
        with contextlib.ExitStack() as st:
            sems = {}
            for i, sn in enumerate(sem_names):
                sems[sn] = st.enter_context(nc.semaphore("s%d" % i))
            block = st.enter_context(nc.Block())
            engobj = {"pe": "tensor", "act": "scalar", "dve": "vector", "pool": "gpsimd", "sp": "sync"}

            def run(e, eng):
                known = {}
                for o in per_eng[e]:
                    need = {}
                    for d in o["deps"]:
                        s, v = ops[d]["sig"]
                        if v > need.get(s, 0):
                            need[s] = v
                    for s, v in need.items():
                        if known.get(s, 0) < v:
                            eng.wait_ge(sems[s], v)
                            known[s] = v
                    ins = o["fn"](eng)
                    if o["dma_key"] is not None:
                        ins.then_inc(sems[("dma", o["dma_key"])], 16)
                    elif o["signal"]:
                        ins.then_inc(sems[("eng", e)], 1)
                if e == "sp":
                    for k in final_wait_keys:
                        v = 16 * self.dma_key_count[k]
                        if known.get(("dma", k), 0) < v:
                            eng.wait_ge(sems[("dma", k)], v)

            for e in self.ENG:
                def mk(e):
                    def f(eng):
                        run(e, eng)
                    return f
                getattr(block, engobj[e])(mk(e))


class _Stop(Exception):
    pass


def build_program(n_prompt_blocks=SEQ // 512, stop_stage=None):
    nc = bass.Bass("TRN2", target_bir_lowering=False)

    def stage(n):
        if stop_stage is not None and n == stop_stage:
            raise _Stop()

    P = Prog(nc)
    st = contextlib.ExitStack()

    def din(name, shape):
        return nc.dram_tensor(name, list(shape), F32, kind="ExternalInput").ap()

    def dout(name, shape):
        return nc.dram_tensor(name, list(shape), F32, kind="ExternalOutput").ap()

    def sb(name, shape, dt=F32):
        return st.enter_context(nc.sbuf_tensor(name, list(shape), dt))

    NTOK = SEQ + NSAMP * TS
    xT_d = din("xT", [128, FT, NTOK])
    cT_d = din("cT", [128, FT, 1 + NSAMP])
    small_d = din("small", [DEPTH, 128, NS_SMALL])
    s5m_d = din("s5m", [DEPTH, 128, 4, 32, 32])
    rgw_d = din("rgw", [DEPTH, 128, 2, 8, 128])
    router_d = din("router", [128, FT, 8])
    ident_d = din("ident", [128, 128])
    pmask_d = din("pmask", [128, 4])
    s5st_d = din("s5st", [DEPTH, 2, 128, 32, NSAMP])
    rgst_d = din("rgst", [DEPTH, 128, 8, NSAMP])
    cvst_d = din("cvst", [DEPTH, 128, 8, NSAMP, 3])
    w_ada_d = din("w_ada", [DEPTH, D, 6 * D])
    w_in_d = din("w_in", [DEPTH, D, 3072])
    w_glu_d = din("s5_w_glu", [DEPTH, 1024, 1024])
    w_gate_d = din("w_gate", [DEPTH, D, 2 * D])
    w_bs_d = din("w_br_s5", [DEPTH, 1024, D])
    w_br_d = din("w_br_rg", [DEPTH, 1024, D])
    w_out_d = din("w_out", [DEPTH, D, D])
    f_w1_d = din("ffn_w1", [1, D, 6144])
    f_w3_d = din("ffn_w3", [1, D, 6144])
    f_w2_d = din("ffn_w2", [1, 6144, D])
    m_w1_d = din("moe_w1", [1, 8, D, 3072])
    m_w3_d = din("moe_w3", [1, 8, D, 3072])
    m_w2_d = din("moe_w2", [1, 8, 3072, D])

    yT_d = dout("yT", [128, FT, NTOK])
    o_s5s_d = dout("o_s5s", [DEPTH, 2, 128, 32, NSAMP])
    o_s5p_d = dout("o_s5p", [DEPTH, 2, 128, 32])
    o_rgs_d = dout("o_rgs", [DEPTH, 128, 8, NSAMP])
    o_rgp_d = dout("o_rgp", [DEPTH, 128, 8])
    o_cvs_d = dout("o_cvs", [DEPTH, 128, 8, NSAMP, 3])
    o_cvp_d = dout("o_cvp", [DEPTH, 128, 8, 3])

    NBMAX = 512
    x_sb = sb("x_sb", [128, FT, NBMAX])
    h_sb = sb("h_sb", [128, FT, NBMAX], BF16)
    mix_sb = sb("mix_sb", [128, 24, NBMAX], BF16)
    NSLAB = 3
    slabs = [sb("slab%d" % i, [128, 16, 128], BF16) for i in range(NSLAB)]
    NTMP = 4
    tmps = [sb("tmp%d" % i, [128, NBMAX]) for i in range(NTMP)]
    NR = 11
    rb_all = sb("rb_all", [128, NR, NBMAX])
    rbuf = [rb_all[:, i, :] for i in range(NR)]
    rb_bf = rb_all.bitcast(BF16)

    def mrg(f, NB):
        return rb_bf[:, f // 2, (f % 2) * NBMAX:(f % 2) * NBMAX + NB]
    xrext = sb("xrext", [128, NBMAX + 64])
    bft = [sb("bft%d" % i, [128, NBMAX], BF16) for i in range(2)]
    um = [sb("um%d" % i, [128, NBMAX], BF16) for i in range(4)]
    PADP = 256
    abp = [sb("abp%d" % i, [128, PADP + NBMAX]) for i in range(4)]
    abs_ = [sb("abs%d" % i, [128, NSAMP, 2 + TS]) for i in range(4)]
    hbf = bft
    wcp = [sb("wcp%d" % i, [128, 128], BF16) for i in range(8)]
    small_sb = sb("small_sb", [128, DEPTH, NS_SMALL])
    mod_sb = sb("mod_sb", [128, DEPTH * 6, FT, 1 + NSAMP, 1])
    cT_sb = sb("cT_sb", [128, FT, 1 + NSAMP])
    cs_bf = sb("cs_bf", [128, FT, 1 + NSAMP], BF16)
    ident = sb("ident_sb", [128, 128])
    router_sb = sb("router_sb", [128, FT, 8])
    s5m_sb = x_sb[:, 0:8, :].rearrange("p a b -> p (a b)").rearrange("p (i j m) -> p i j m", i=4, j=32)
    bbar = x_sb[:, 8:12, :].rearrange("p a b -> p (a b)").rearrange("p (i j m) -> p i j m", i=2, j=32)
    wbT = sb("wbT", [128, DEPTH, 2, 8, 128], BF16)
    cb = sb("cb", [128, DEPTH, 2, 32, 32], BF16)
    NLV = 9
    pw = sb("pw", [128, DEPTH, 32, NLV, 3])
    sp_t = [sb("spt%d" % i, [128, 32]) for i in range(14)]
    sp_i = sb("spi", [128, 32], I32)
    rgw_sb = sb("rgw_sb", [128, DEPTH, 2, 8, 128], BF16)
    rgc = sb("rgc", [128, DEPTH, 2, 8])
    s5car = sb("s5car", [128, DEPTH, 2, 32, 1])
    rgcar = sb("rgcar", [128, DEPTH, 8, 1])
    cvtail = sb("cvtail", [128, DEPTH, 8, 1, 3])
    s5in = sb("s5in", [128, DEPTH, 2, 32, NSAMP])
    s5out = s5in
    rgin = sb("rgin", [128, DEPTH, 8, NSAMP])
    rgout = rgin
    cvin = sb("cvin", [128, DEPTH, 8, NSAMP, 3])
    cvout = cvin
    car4 = sb("car4", [128, 4, NSAMP])
    lgT = sb("lgT", [128, 4, 8])
    mx8 = sb("mx8", [128, 4, 8])
    cmb = sb("cmb", [128, 4, 8])
    cmbt = sb("cmbt", [128, 4, 8])
    den = sb("den", [128, 4, 1])
    ones_f = sb("ones_f", [128, 128])
    pmask = sb("pmask_sb", [128, 4])

    psum = [st.enter_context(nc.psum_tensor("ps%d" % i, [128, 512], F32)) for i in range(8)]

    def mm(out, lhsT, rhs, start, stop, reads, writes):
        P.op("pe", lambda e, a=out, b=lhsT, c=rhs, s=start, t=stop: e.matmul(a, lhsT=b, rhs=c, start=s, stop=t),
             reads, writes)

    def act(out, in_, func, reads, writes, bias=None, scale=None):
        kw = {}
        if bias is not None:
            kw["bias"] = bias
        if scale is not None:
            kw["scale"] = scale
        P.op("act", lambda e, a=out, b=in_, f=func, k=kw: e.activation(a, b, f, **k), reads, writes)

    def tt(out, a, b, op, reads, writes, eng="dve"):
        P.op(eng, lambda e, o=out, x=a, y=b, p=op: e.tensor_tensor(out=o, in0=x, in1=y, op=p), reads, writes)

    def ts(out, a, s1, s2, op0, op1, reads, writes, eng="dve"):
        if s2 is None:
            P.op(eng, lambda e, o=out, x=a, u=s1, p=op0: e.tensor_scalar(o, x, u, None, p), reads, writes)
        else:
            P.op(eng, lambda e, o=out, x=a, u=s1, v=s2, p=op0, q=op1: e.tensor_scalar(o, x, u, v, p, q), reads, writes)

    def stt(out, in0, scalar, in1, op0, op1, reads, writes, eng="dve"):
        P.op(eng, lambda e, o=out, x=in0, s=scalar, y=in1, p=op0, q=op1:
             e.scalar_tensor_tensor(out=o, in0=x, scalar=s, in1=y, op0=p, op1=q), reads, writes)

    def cp(eng, out, in_, reads, writes):
        if eng == "act":
            P.op("act", lambda e, o=out, i=in_: e.copy(o, i), reads, writes)
        else:
            P.op(eng, lambda e, o=out, i=in_: e.tensor_copy(o, i), reads, writes)

    def memset(eng, ap, val, writes):
        P.op(eng, lambda e, a=ap, v=val: e.memset(a, v), (), writes)

    def dma(q, out, in_, reads, writes, key, wait_all=False):
        P.op(q, lambda e, o=out, i=in_: e.dma_start(out=o, in_=i), reads, writes, dma_key=key, wait_all=wait_all)

    slab_ctr = [0]

    def load_slab(src_ap, K):
        s = slab_ctr[0] % NSLAB
        slab_ctr[0] += 1
        key = ("slab", s)
        dma("pool", slabs[s][:, 0:K, :], src_ap.rearrange("(k p) m -> p k m", p=128), (), [key], key=("slabq", s))
        return slabs[s], key

    tmp_ctr = [0]

    def T32():
        i = tmp_ctr[0] % NTMP
        tmp_ctr[0] += 1
        return tmps[i], ("tmp", i)

    PS = lambda i: ("ps", i)

    C = "const"
    dma("sp", small_sb[:], small_d.rearrange("l p n -> p l n"), (), ["small"], key=C, wait_all=True)
    dma("sp", cT_sb[:], cT_d, (), ["cT"], key=C, wait_all=True)
    dma("sp", ident[:], ident_d, (), ["ident"], key=C, wait_all=True)
    dma("sp", pmask[:], pmask_d, (), ["pmask"], key=C, wait_all=True)
    dma("sp", router_sb[:], router_d, (), ["router"], key=C, wait_all=True)
    dma("sp", s5in[:], s5st_d.rearrange("l r p j q -> p l r j q"), (), ["s5in"], key=C, wait_all=True)
    dma("sp", rgin[:], rgst_d.rearrange("l p j q -> p l j q"), (), ["rgin"], key=C, wait_all=True)
    dma("sp", cvin[:], cvst_d.rearrange("l p j q k -> p l j q k"), (), ["cvin"], key=C, wait_all=True)
    dma("pool", rgw_sb[:], rgw_d.rearrange("l p a j m -> p l a j m"), (), ["rgw"], key=("rgwq",))
    memset("dve", ones_f[:], 1.0, ["ones"])
    for i in range(4):
        memset("dve", abp[i][:], 0.0, [("abp", i)])
        memset("dve", abs_[i][:], 0.0, [("abs", i)])
    for i in range(8):
        memset("dve", wcp[i][:], 0.0, [("wcp", i)])
    memset("dve", s5car[:], 0.0, ["s5car"])
    memset("dve", rgcar[:], 0.0, ["rgcar"])
    memset("dve", cvtail[:], 0.0, ["cvtail"])

    def smallv(l, c0, n):
        return small_sb[:, l, c0:c0 + n]

    try:
        act(cs_bf[:], cT_sb[:], AF.Silu, ["cT"], ["cs"])
        NSQ = 1 + NSAMP
        for l in range(DEPTH):
            for kind in range(6):
                pb = (l * 6 + kind) % 4
                for ft in range(FT):
                    col0 = (kind * FT + ft) * 128
                    slab, skey = load_slab(w_ada_d[l, :, col0:col0 + 128], 16)
                    for k in range(16):
                        mm(psum[pb][:, ft * NSQ:(ft + 1) * NSQ], slab[:, k, :], cs_bf[:, k, :], k == 0, k == 15,
                           [skey, "cs"], [PS(pb)])
                bias_b = small_sb[:, l, 160 + kind * FT:160 + (kind + 1) * FT].rearrange("p (f o) -> p f o", o=1) \
                    .to_broadcast([128, FT, NSQ])
                tt(mod_sb[:, l * 6 + kind, :, :, 0], psum[pb][:, 0:FT * NSQ].rearrange("p (f s) -> p f s", s=NSQ),
                   bias_b, ALU.add, [PS(pb), "small"], [("mod", l, kind)])
            for which, (kind, ncol) in enumerate(((1, 0), (4, 16))):
                tmpv = mod_sb[:, l * 6 + kind, :, :, 0]
                ts(tmpv, tmpv, 1.0, None, ALU.add, None, [("mod", l, kind)], [("mod", l, kind)])
                nb = small_sb[:, l, ncol:ncol + FT].rearrange("p (f o) -> p f o", o=1).to_broadcast([128, FT, NSQ])
                tt(tmpv, tmpv, nb, ALU.mult, [("mod", l, kind), "small"], [("mod", l, kind)])

        stage(0)
        for l in range(DEPTH):
            lamre = smallv(l, 256, 32)
            lamim = smallv(l, 288, 32)
            logdt = smallv(l, 320, 32)
            t = sp_t
            K = lambda i: ("spt", i)
            S = ["small"]
            act(t[0][:], logdt, AF.Exp, S, [K(0)])
            tt(t[1][:], lamre, t[0][:], ALU.mult, S + [K(0)], [K(1)])
            act(t[1][:], t[1][:], AF.Exp, [K(1)], [K(1)])
            tt(t[2][:], lamim, t[0][:], ALU.mult, S + [K(0)], [K(2)])
            ts(t[2][:], t[2][:], 1.0 / TWO_PI, None, ALU.mult, None, [K(2)], [K(2)])

            def sin_of(dst, kdst, shift):
                ts(t[3][:], t[2][:], shift, None, ALU.add, None, [K(2)], [K(3)])
                cp("dve", sp_i[:], t[3][:], [K(3)], ["spi"])
                cp("dve", t[4][:], sp_i[:], ["spi"], [K(4)])
                tt(t[3][:], t[3][:], t[4][:], ALU.subtract, [K(3), K(4)], [K(3)])
                ts(t[4][:], t[3][:], 0.5, None, ALU.is_gt, None, [K(3)], [K(4)])
                tt(t[3][:], t[3][:], t[4][:], ALU.subtract, [K(3), K(4)], [K(3)])
                ts(t[4][:], t[3][:], -0.5, None, ALU.is_lt, None, [K(3)], [K(4)])
                tt(t[3][:], t[3][:], t[4][:], ALU.add, [K(3), K(4)], [K(3)])
                act(dst, t[3][:], AF.Sin, [K(3)], [kdst], scale=TWO_PI)

            sin_of(t[5][:], K(5), 0.0)
            sin_of(t[6][:], K(6), 0.25)
            lre = pw[:, l, :, 0, 0]
            lim = pw[:, l, :, 0, 1]
            lnim = pw[:, l, :, 0, 2]
            PWK = ("pw", l)
            tt(lre, t[1][:], t[6][:], ALU.mult, [K(1), K(6)], [PWK])
            tt(lim, t[1][:], t[5][:], ALU.mult, [K(1), K(5)], [PWK])
            ts(lnim, lim, -1.0, None, ALU.mult, None, [PWK], [PWK])
            for lv in range(1, NLV):
                a_re = pw[:, l, :, lv - 1, 0]
                a_im = pw[:, l, :, lv - 1, 1]
                tt(t[7][:], a_re, a_re, ALU.mult, [PWK], [K(7)])
                tt(t[8][:], a_im, a_im, ALU.mult, [PWK], [K(8)])
                tt(pw[:, l, :, lv, 0], t[7][:], t[8][:], ALU.subtract, [K(7), K(8)], [PWK])
                tt(t[7][:], a_re, a_im, ALU.mult, [PWK], [K(7)])
                ts(pw[:, l, :, lv, 1], t[7][:], 2.0, None, ALU.mult, None, [K(7)], [PWK])
                ts(pw[:, l, :, lv, 2], t[7][:], -2.0, None, ALU.mult, None, [K(7)], [PWK])
            ts(t[7][:], lre, -1.0, None, ALU.add, None, [PWK], [K(7)])
            tt(t[8][:], lamre, lamre, ALU.mult, S, [K(8)])
            tt(t[9][:], lamim, lamim, ALU.mult, S, [K(9)])
            tt(t[8][:], t[8][:], t[9][:], ALU.add, [K(8), K(9)], [K(8)])
            P.op("dve", lambda e, o=t[8][:]: e.reciprocal(o, o), [K(8)], [K(8)])
            tt(t[9][:], t[7][:], lamre, ALU.mult, [K(7)] + S, [K(9)])
            tt(t[10][:], lim, lamim, ALU.mult, [PWK] + S, [K(10)])
            tt(t[9][:], t[9][:], t[10][:], ALU.add, [K(9), K(10)], [K(9)])
            tt(t[9][:], t[9][:], t[8][:], ALU.mult, [K(9), K(8)], [K(9)])
            tt(t[10][:], lim, lamre, ALU.mult, [PWK] + S, [K(10)])
            tt(t[11][:], t[7][:], lamim, ALU.mult, [K(7)] + S, [K(11)])
            tt(t[10][:], t[10][:], t[11][:], ALU.subtract, [K(10), K(11)], [K(10)])
            tt(t[10][:], t[10][:], t[8][:], ALU.mult, [K(10), K(8)], [K(10)])
            dma("sp", s5m_sb, s5m_d[l], (), ["s5m"], key=("s5m",))
            bre = s5m_sb[:, 0]
            bim = s5m_sb[:, 1]
            for hlf in range(2):
                js = slice(hlf * 16, hlf * 16 + 16)
                s0 = rbuf[0][:, :].rearrange("p (j m) -> p j m", m=32)
                s1 = rbuf[1][:, :].rearrange("p (j m) -> p j m", m=32)
                cre_h = t[9][:, js].rearrange("p (j o) -> p j o", o=1).to_broadcast([128, 16, 32])
                cim_h = t[10][:, js].rearrange("p (j o) -> p j o", o=1).to_broadcast([128, 16, 32])
                tt(s0, bre[:, js, :], cre_h, ALU.mult, ["s5m", K(9)], [("rb", 0)])
                tt(s1, bim[:, js, :], cim_h, ALU.mult, ["s5m", K(10)], [("rb", 1)])
                tt(bbar[:, 0, js, :], s0, s1, ALU.subtract, [("rb", 0), ("rb", 1)], ["bbar"])
                tt(s0, bim[:, js, :], cre_h, ALU.mult, ["s5m", K(9)], [("rb", 0)])
                tt(s1, bre[:, js, :], cim_h, ALU.mult, ["s5m", K(10)], [("rb", 1)])
                tt(bbar[:, 1, js, :], s0, s1, ALU.add, [("rb", 0), ("rb", 1)], ["bbar"])
            for ri in range(2):
                for ct in range(8):
                    pb = 4 + (ri * 8 + ct) % 4
                    src = bbar[:, ri, ct * 4:(ct + 1) * 4, :].rearrange("p j m -> p (j m)")
                    P.op("pe", lambda e, o=psum[pb][:, 0:128], i=src: e.transpose(o, i, ident[:]),
                         ["bbar", "ident"], [PS(pb)])
                    cp("act", wbT[:, l, ri, ct, :], psum[pb][:, 0:128], [PS(pb)], [("wbT", l)])
            cp("act", cb[:, l, 0], s5m_sb[:, 2], ["s5m"], [("cb", l)])
            ts(cb[:, l, 1], s5m_sb[:, 3], -1.0, None, ALU.mult, None, ["s5m"], [("cb", l)])
            rl = smallv(l, 128, 8)
            act(rgc[:, l, 0, :], rl, AF.Exp, S, [("rgc", l)], scale=-1.0)
            act(rgc[:, l, 0, :], rgc[:, l, 0, :], AF.Ln, [("rgc", l)], [("rgc", l)], bias=1.0)
            ts(rgc[:, l, 1, :], rgc[:, l, 0, :], -16.0, None, ALU.mult, None, [("rgc", l)], [("rgc", l)])
            ts(rgc[:, l, 0, :], rgc[:, l, 0, :], -8.0, None, ALU.mult, None, [("rgc", l)], [("rgc", l)])

        stage(1)
        blocks = []
        for b in range(n_prompt_blocks):
            blocks.append(dict(c0=b * 512, NB=512, nseq=1, T=512, samp=False, last=(b == SEQ // 512 - 1)))
        blocks.append(dict(c0=SEQ, NB=NSAMP * TS, nseq=NSAMP, T=TS, samp=True, last=True))

        def v3(ap, blk):
            return ap.rearrange("p (q t) -> p q t", t=blk["T"])

        def modb(idx_tensor, idx, ft, blk):
            s0, s1 = (1, 1 + NSAMP) if blk["samp"] else (0, 1)
            return idx_tensor[:, idx, ft, s0:s1, :].to_broadcast([128, blk["nseq"], blk["T"]])

        def gelu_from(src32, skey, out_bf, okeys):
            t1, k1 = T32()
            act(t1[:, 0:NBc[0]], src32, AF.Square, [skey], [k1])
            ts(t1[:, 0:NBc[0]], t1[:, 0:NBc[0]], 0.044715, 1.0, ALU.mult, ALU.add, [k1], [k1])
            tt(t1[:, 0:NBc[0]], t1[:, 0:NBc[0]], src32, ALU.mult, [k1, skey], [k1])
            act(t1[:, 0:NBc[0]], t1[:, 0:NBc[0]], AF.Sigmoid, [k1], [k1], scale=GELU_K)
            tt(out_bf, src32, t1[:, 0:NBc[0]], ALU.mult, [skey, k1], okeys)

        NBc = [512]

        def rmsnorm_mod(l, which, blk, want_router=False):
            NB = blk["NB"]
            shk = 0 if which == 0 else 3
            pb = 0
            for ft in range(FT):
                tq, kq = T32()
                act(tq[:, 0:NB], x_sb[:, ft, 0:NB], AF.Square, [("x", ft)], [kq])
                mm(psum[pb][:, 0:NB], ones_f[:], tq[:, 0:NB], ft == 0, ft == FT - 1, ["ones", kq], [PS(pb)])
            rstd = rbuf[10]
            act(rstd[:, 0:NB], psum[pb][:, 0:NB], AF.Ln, [PS(pb)], [("rb", 10)], bias=1e-6, scale=1.0 / D)
            act(rstd[:, 0:NB], rstd[:, 0:NB], AF.Exp, [("rb", 10)], [("rb", 10)], scale=-0.5)
            for ft in range(FT):
                tq, kq = T32()
                tt(tq[:, 0:NB], x_sb[:, ft, 0:NB], rstd[:, 0:NB], ALU.mult, [("x", ft), ("rb", 10)], [kq])
                tt(v3(tq[:, 0:NB], blk), v3(tq[:, 0:NB], blk), modb(mod_sb, l * 6 + (1 if which == 0 else 4), ft, blk), ALU.mult,
                   [kq, ("mod", l, 1 if which == 0 else 4)], [kq])
                if want_router:
                    tt(v3(tq[:, 0:NB], blk), v3(tq[:, 0:NB], blk), modb(mod_sb, l * 6 + shk, ft, blk), ALU.add,
                       [kq, ("mod", l, shk)], [kq])
                    mm(psum[1][0:8, 0:NB], router_sb[:, ft, :], tq[:, 0:NB], ft == 0, ft == FT - 1,
                       ["router", kq], [PS(1)])
                    cp("act", h_sb[:, ft, 0:NB], tq[:, 0:NB], [kq], [("h", ft)])
                else:
                    tt(v3(h_sb[:, ft, 0:NB], blk), v3(tq[:, 0:NB], blk), modb(mod_sb, l * 6 + shk, ft, blk), ALU.add,
                       [kq, ("mod", l, shk)], [("h", ft)])

        def proj_tile(w_ap, kchunks, rhs_fn, rhs_keys, pb, NB):
            slab, skey = load_slab(w_ap, kchunks)
            for k in range(kchunks):
                mm(psum[pb][:, 0:NB], slab[:, k, :], rhs_fn(k), k == 0, k == kchunks - 1,
                   [skey] + [rhs_keys(k)], [PS(pb)])

        H_ALL = [("h", f) for f in range(FT)]

        def mixer(l, blk):
            NB, nseq, T = blk["NB"], blk["nseq"], blk["T"]
            NBc[0] = NB
            samp = blk["samp"]
            hk = lambda k: ("h", k)
            hf = lambda k: h_sb[:, k, 0:NB]
            AB = abs_ if samp else abp
            ABK = "abs" if samp else "abp"
            pad = 2 if samp else PADP
            levels = []
            d = 1
            lv = 0
            while d < T:
                levels.append((lv, d))
                d *= 2
                lv += 1

            def abv(i, off):
                if samp:
                    return AB[i][:, :, pad - off:pad - off + T]
                return AB[i][:, pad - off:pad - off + T].rearrange("p (q t) -> p q t", q=1)

            for ct in range(8):
                proj_tile(w_in_d[l, :, ct * 128:(ct + 1) * 128], 16, hf, hk, 0, NB)
                stage(27)
                u32 = rbuf[0]
                cp("act", u32[:, 0:NB], psum[0][:, 0:NB], [PS(0)], [("rb", 0)])
                stage(28)
                for jj in range(4):
                    ts(um[jj][:, 0:NB], psum[0][:, 0:NB], pmask[:, jj:jj + 1], None, ALU.mult, None,
                       [PS(0), "pmask"], [("um", jj)])
                stage(21)
                for jj in range(4):
                    for ri in range(2):
                        cp("act", wcp[jj * 2 + ri][:, 32 * jj:32 * jj + 32], cb[:, l, ri, ct * 4 + jj, :],
                           [("cb", l)], [("wcp", jj * 2 + ri)])
                stage(22)
                for jj in range(4):
                    j = ct * 4 + jj
                    for ri in range(2):
                        mm(psum[1 + ri][:, 0:NB], wbT[:, l, ri, ct, :], um[jj][:, 0:NB], True, True,
                           [("wbT", l), ("um", jj)], [PS(1 + ri)])
                    for ri in range(2):
                        cp("act", abv(ri, 0), v3(psum[1 + ri][:, 0:NB], blk), [PS(1 + ri)], [(ABK, ri)])
                    stage(23)
                    if samp:
                        cre = s5in[:, l, 0, j, :]
                        cim = s5in[:, l, 1, j, :]
                        ckeys = ["s5in"]
                    else:
                        cre = s5car[:, l, 0, j, :]
                        cim = s5car[:, l, 1, j, :]
                        ckeys = ["s5car"]
                    L_re = pw[:, l, j, 0, 0:1]
                    L_im = pw[:, l, j, 0, 1:2]
                    L_nim = pw[:, l, j, 0, 2:3]
                    a0re = abv(0, 0)[:, :, 0]
                    a0im = abv(1, 0)[:, :, 0]
                    PWK = ("pw", l)
                    stt(a0re, cre, L_re, a0re, ALU.mult, ALU.add, ckeys + [PWK, (ABK, 0)], [(ABK, 0)])
                    stt(a0re, cim, L_nim, a0re, ALU.mult, ALU.add, ckeys + [PWK, (ABK, 0)], [(ABK, 0)])
                    stt(a0im, cim, L_re, a0im, ALU.mult, ALU.add, ckeys + [PWK, (ABK, 1)], [(ABK, 1)])
                    stt(a0im, cre, L_im, a0im, ALU.mult, ALU.add, ckeys + [PWK, (ABK, 1)], [(ABK, 1)])
                    stage(24)
                    cur = 0
                    for (lv_, d_) in levels:
                        s_re, s_im = cur * 2, cur * 2 + 1
                        d_re, d_im = (1 - cur) * 2, (1 - cur) * 2 + 1
                        ar = pw[:, l, j, lv_, 0:1]
                        ai = pw[:, l, j, lv_, 1:2]
                        nai = pw[:, l, j, lv_, 2:3]
                        stt(abv(d_re, 0), abv(s_re, d_), ar, abv(s_re, 0), ALU.mult, ALU.add,
                            [(ABK, s_re), PWK], [(ABK, d_re)])
                        stt(abv(d_re, 0), abv(s_im, d_), nai, abv(d_re, 0), ALU.mult, ALU.add,
                            [(ABK, s_im), (ABK, d_re), PWK], [(ABK, d_re)])
                        stt(abv(d_im, 0), abv(s_im, d_), ar, abv(s_im, 0), ALU.mult, ALU.add,
                            [(ABK, s_im), PWK], [(ABK, d_im)])
                        stt(abv(d_im, 0), abv(s_re, d_), ai, abv(d_im, 0), ALU.mult, ALU.add,
                            [(ABK, s_re), (ABK, d_im), PWK], [(ABK, d_im)])
                        cur = 1 - cur
                    f_re, f_im = cur * 2, cur * 2 + 1
                    stage(25)
                    cp("act", v3(hbf[0][:, 0:NB], blk), abv(f_re, 0), [(ABK, f_re)], [("hbf", 0)])
                    cp("act", v3(hbf[1][:, 0:NB], blk), abv(f_im, 0), [(ABK, f_im)], [("hbf", 1)])
                    if samp:
                        cp("dve", s5out[:, l, 0, j, :], abv(f_re, 0)[:, :, T - 1], [(ABK, f_re)], ["s5in"])
                        cp("dve", s5out[:, l, 1, j, :], abv(f_im, 0)[:, :, T - 1], [(ABK, f_im)], ["s5in"])
                    else:
                        cp("dve", s5car[:, l, 0, j, :], abv(f_re, 0)[:, :, T - 1], [(ABK, f_re)], ["s5car"])
                        cp("dve", s5car[:, l, 1, j, :], abv(f_im, 0)[:, :, T - 1], [(ABK, f_im)], ["s5car"])
                    for ri in range(2):
                        mm(psum[3][:, 0:NB], wcp[jj * 2 + ri][:], hbf[ri][:, 0:NB], jj == 0 and ri == 0,
                           jj == 3 and ri == 1, [("wcp", jj * 2 + ri), ("hbf", ri)], [PS(3)])
                stage(26)
                ypre = rbuf[1]
                stt(ypre[:, 0:NB], u32[:, 0:NB], small_sb[:, l, 152 + ct:153 + ct], psum[3][:, 0:NB], ALU.mult, ALU.add,
                    [("rb", 0), "small", PS(3)], [("rb", 1)])
                gelu_from(ypre[:, 0:NB], ("rb", 1), mix_sb[:, ct, 0:NB], [("mix", ct)])
            stage(3)
            for ct in range(8):
                pb = 1 + ct % 2
                proj_tile(w_glu_d[l, :, ct * 128:(ct + 1) * 128], 8, lambda k: mix_sb[:, k, 0:NB], lambda k: ("mix", k), pb, NB)
                tg, kg = T32()
                act(tg[:, 0:NB], psum[pb][:, 0:NB], AF.Sigmoid, [PS(pb), "small"], [kg], bias=small_sb[:, l, 80 + ct:81 + ct])
                tt(mix_sb[:, 8 + ct, 0:NB], mix_sb[:, ct, 0:NB], tg[:, 0:NB], ALU.mult, [("mix", ct), kg], [("mix", 8 + ct)])

            stage(4)
            HX = 3
            for j in range(8):
                proj_tile(w_in_d[l, :, 1024 + j * 128:1024 + (j + 1) * 128], 16, hf, hk, 4, NB)
                proj_tile(w_in_d[l, :, 2048 + j * 128:2048 + (j + 1) * 128], 16, hf, hk, 5, NB)
                xe = xrext[:, 0:nseq * (T + HX)].rearrange("p (q t) -> p q t", t=T + HX)
                XK = "xrext"
                cp("act", xe[:, :, HX:HX + T], v3(psum[4][:, 0:NB], blk), [PS(4)], [XK])
                if samp:
                    cp("dve", xe[:, :, 0:HX], cvin[:, l, j, :, :], ["cvin", XK], [XK])
                else:
                    cp("dve", xe[:, :, 0:HX], cvtail[:, l, j, :, :], ["cvtail", XK], [XK])
                gy32 = rbuf[2]
                cp("act", gy32[:, 0:NB], psum[5][:, 0:NB], [PS(5)], [("rb", 2)])
                gelu_from(gy32[:, 0:NB], ("rb", 2), bft[0][:, 0:NB], [("bft", 0)])
                xc = rbuf[3]
                xcv = v3(xc[:, 0:NB], blk)
                cw = lambda k: small_sb[:, l, 88 + k * 8 + j:89 + k * 8 + j]
                ts(xcv, xe[:, :, 0:T], cw(0), small_sb[:, l, 120 + j:121 + j], ALU.mult, ALU.add, [XK, "small"], [("rb", 3)])
                for k in range(1, 4):
                    stt(xcv, xe[:, :, k:k + T], cw(k), xcv, ALU.mult, ALU.add, [XK, "small", ("rb", 3)], [("rb", 3)])
                if samp:
                    cp("dve", cvout[:, l, j, :, :], xe[:, :, T:T + HX], [XK], ["cvin"])
                else:
                    cp("dve", cvtail[:, l, j, :, :], xe[:, :, T:T + HX], [XK], ["cvtail"])
                cp("act", bft[1][:, 0:NB], xc[:, 0:NB], [("rb", 3)], [("bft", 1)])
                mm(psum[6][:, 0:NB], rgw_sb[:, l, 0, j, :], bft[1][:, 0:NB], True, True, ["rgw", ("bft", 1)], [PS(6)])
                mm(psum[7][:, 0:NB], rgw_sb[:, l, 1, j, :], bft[1][:, 0:NB], True, True, ["rgw", ("bft", 1)], [PS(7)])
                r32, i32, a32, e2 = rbuf[4], rbuf[5], rbuf[6], rbuf[7]
                act(r32[:, 0:NB], psum[6][:, 0:NB], AF.Sigmoid, [PS(6), "small"], [("rb", 4)], bias=small_sb[:, l, 136 + j:137 + j])
                act(i32[:, 0:NB], psum[7][:, 0:NB], AF.Sigmoid, [PS(7), "small"], [("rb", 5)], bias=small_sb[:, l, 144 + j:145 + j])
                act(a32[:, 0:NB], r32[:, 0:NB], AF.Exp, [("rb", 4), ("rgc", l)], [("rb", 6)], scale=rgc[:, l, 0, j:j + 1])
                act(e2[:, 0:NB], r32[:, 0:NB], AF.Exp, [("rb", 4), ("rgc", l)], [("rb", 7)], scale=rgc[:, l, 1, j:j + 1])
                act(e2[:, 0:NB], e2[:, 0:NB], AF.Ln, [("rb", 7)], [("rb", 7)], bias=1.0, scale=-1.0)
                act(e2[:, 0:NB], e2[:, 0:NB], AF.Exp, [("rb", 7)], [("rb", 7)], scale=0.5)
                bx = rbuf[8]
                tt(bx[:, 0:NB], i32[:, 0:NB], xc[:, 0:NB], ALU.mult, [("rb", 5), ("rb", 3)], [("rb", 8)])
                tt(bx[:, 0:NB], bx[:, 0:NB], e2[:, 0:NB], ALU.mult, [("rb", 8), ("rb", 7)], [("rb", 8)])
                a3 = v3(a32[:, 0:NB], blk)
                b3 = v3(bx[:, 0:NB], blk)
                if samp:
                    h0 = rgin[:, l, j, :]
                    h0k = ["rgin"]
                else:
                    h0 = rgcar[:, l, j, :]
                    h0k = ["rgcar"]
                tcar = car4[:, 0, 0:nseq]
                tt(tcar, a3[:, :, 0], h0, ALU.mult, [("rb", 6)] + h0k, ["car4"])
                tt(b3[:, :, 0], b3[:, :, 0], tcar, ALU.add, [("rb", 8), "car4"], [("rb", 8)])
                memset("dve", a3[:, :, 0], 0.0, [("rb", 6)])
                hh = rbuf[9]
                P.op("dve", lambda e, o=hh[:, 0:NB], a=a32[:, 0:NB], b=bx[:, 0:NB]:
                     e.tensor_tensor_scan(out=o, data0=a, data1=b, initial=0.0, op0=ALU.mult, op1=ALU.add),
                     [("rb", 6), ("rb", 8)], [("rb", 9)])
                h3 = v3(hh[:, 0:NB], blk)
                if samp:
                    cp("dve", rgout[:, l, j, :], h3[:, :, T - 1], [("rb", 9)], ["rgin"])
                else:
                    cp("dve", rgcar[:, l, j, :], h3[:, :, T - 1], [("rb", 9)], ["rgcar"])
                tt(mix_sb[:, 16 + j, 0:NB], hh[:, 0:NB], bft[0][:, 0:NB], ALU.mult, [("rb", 9), ("bft", 0)], [("mix", 16 + j)])

            stage(5)
            for f in range(FT):
                pb0 = 4 * (f % 2)
                proj_tile(w_gate_d[l, :, f * 128:(f + 1) * 128], 16, hf, hk, pb0, NB)
                proj_tile(w_gate_d[l, :, D + f * 128:D + (f + 1) * 128], 16, hf, hk, pb0 + 1, NB)
                proj_tile(w_bs_d[l, :, f * 128:(f + 1) * 128], 8, lambda k: mix_sb[:, 8 + k, 0:NB], lambda k: ("mix", 8 + k), pb0 + 2, NB)
                proj_tile(w_br_d[l, :, f * 128:(f + 1) * 128], 8, lambda k: mix_sb[:, 16 + k, 0:NB], lambda k: ("mix", 16 + k), pb0 + 3, NB)
                sa, ka = T32()
                sb_, kb = T32()
                act(sa[:, 0:NB], psum[pb0][:, 0:NB], AF.Sigmoid, [PS(pb0), "small"], [ka], bias=small_sb[:, l, 48 + f:49 + f])
                act(sb_[:, 0:NB], psum[pb0 + 1][:, 0:NB], AF.Sigmoid, [PS(pb0 + 1), "small"], [kb], bias=small_sb[:, l, 64 + f:65 + f])
                tt(sa[:, 0:NB], sa[:, 0:NB], psum[pb0 + 2][:, 0:NB], ALU.mult, [ka, PS(pb0 + 2)], [ka])
                tt(sb_[:, 0:NB], sb_[:, 0:NB], psum[pb0 + 3][:, 0:NB], ALU.mult, [kb, PS(pb0 + 3)], [kb])
                tt(mrg(f, NB), sa[:, 0:NB], sb_[:, 0:NB], ALU.add, [ka, kb], [("rb", f // 2)])
            for f in range(FT):
                pb = f % 4
                proj_tile(w_out_d[l, :, f * 128:(f + 1) * 128], 16, lambda k: mrg(k, NB), lambda k: ("rb", k // 2), pb, NB)
                tq, kq = T32()
                tt(v3(tq[:, 0:NB], blk), v3(psum[pb][:, 0:NB], blk), modb(mod_sb, l * 6 + 2, f, blk), ALU.mult,
                   [PS(pb), ("mod", l, 2)], [kq])
                tt(x_sb[:, f, 0:NB], x_sb[:, f, 0:NB], tq[:, 0:NB], ALU.add, [("x", f), kq], [("x", f)])

        def ffn_half(l, blk, w1_ap, w3_ap, w2_ap, comb_ap, comb_key):
            NB = blk["NB"]
            hk = lambda k: ("h", k)
            hf = lambda k: h_sb[:, k, 0:NB]
            for t_ in range(24):
                pb = 2 * (t_ % 2)
                proj_tile(w1_ap[:, t_ * 128:(t_ + 1) * 128], 16, hf, hk, pb, NB)
                proj_tile(w3_ap[:, t_ * 128:(t_ + 1) * 128], 16, hf, hk, pb + 1, NB)
                tq, kq = T32()
                act(tq[:, 0:NB], psum[pb][:, 0:NB], AF.Silu, [PS(pb)], [kq])
                tt(mix_sb[:, t_, 0:NB], tq[:, 0:NB], psum[pb + 1][:, 0:NB], ALU.mult, [kq, PS(pb + 1)], [("mix", t_)])
            for f in range(FT):
                pb = 4 + f % 3
                s1, k1 = load_slab(w2_ap[0:2048, f * 128:(f + 1) * 128], 16)
                s2, k2 = load_slab(w2_ap[2048:3072, f * 128:(f + 1) * 128], 8)
                for k in range(24):
                    sl, sk = (s1, k1) if k < 16 else (s2, k2)
                    mm(psum[pb][:, 0:NB], sl[:, k % 16, :], mix_sb[:, k, 0:NB], k == 0, k == 23, [sk, ("mix", k)], [PS(pb)])
                tq, kq = T32()
                tt(v3(tq[:, 0:NB], blk), v3(psum[pb][:, 0:NB], blk), modb(mod_sb, l * 6 + 5, f, blk), ALU.mult,
                   [PS(pb), ("mod", l, 5)], [kq])
                if comb_ap is not None:
                    tt(tq[:, 0:NB], tq[:, 0:NB], comb_ap[:, 0:NB], ALU.mult, [kq, comb_key], [kq])
                tt(x_sb[:, f, 0:NB], x_sb[:, f, 0:NB], tq[:, 0:NB], ALU.add, [("x", f), kq], [("x", f)])

        def moe_router(blk):
            NB = blk["NB"]
            nsub = max(1, NB // 128)
            tk = min(128, NB)
            lg_sb = rbuf[0][0:8, :]
            combT = rbuf[1][0:8, :]
            cp("act", lg_sb[:, 0:NB], psum[1][0:8, 0:NB], [PS(1)], [("rb", 0)])
            for s in range(nsub):
                P.op("pe", lambda e, o=psum[2][0:tk, s * 8:(s + 1) * 8], i=lg_sb[:, s * tk:(s + 1) * tk]:
                     e.transpose(o, i, ident[0:8, 0:8]), [("rb", 0), "ident"], [PS(2)])
            cp("act", lgT[0:tk, 0:nsub, :], psum[2][0:tk, 0:nsub * 8].rearrange("p (s e) -> p s e", e=8), [PS(2)], ["lgT"])
            for s in range(nsub):
                P.op("dve", lambda e, o=mx8[0:tk, s, :], i=lgT[0:tk, s, :]: e.max(o, i), ["lgT"], ["mx8"])
                ts(cmb[0:tk, s, :], lgT[0:tk, s, :], mx8[0:tk, s, 0:1], None, ALU.subtract, None, ["lgT", "mx8"], ["cmb"])
                act(cmb[0:tk, s, :], cmb[0:tk, s, :], AF.Exp, ["cmb"], ["cmb"])
                ts(cmbt[0:tk, s, :], lgT[0:tk, s, :], mx8[0:tk, s, 1:2], None, ALU.is_ge, None, ["lgT", "mx8"], ["cmbt"])
                tt(cmb[0:tk, s, :], cmb[0:tk, s, :], cmbt[0:tk, s, :], ALU.mult, ["cmb", "cmbt"], ["cmb"])
                P.op("dve", lambda e, o=den[0:tk, s, :], i=cmb[0:tk, s, :]:
                     e.reduce_sum(o, i, axis=mybir.AxisListType.X), ["cmb"], ["den"])
                P.op("dve", lambda e, o=den[0:tk, s, :]: e.reciprocal(o, o), ["den"], ["den"])
                ts(cmb[0:tk, s, :], cmb[0:tk, s, :], den[0:tk, s, 0:1], None, ALU.mult, None, ["cmb", "den"], ["cmb"])
                P.op("pe", lambda e, o=psum[3][0:8, s * tk:(s + 1) * tk], i=cmb[0:tk, s, :]:
                     e.transpose(o, i, ident[0:tk, 0:tk]), ["cmb", "ident"], [PS(3)])
            cp("act", combT[:, 0:NB], psum[3][0:8, 0:NB], [PS(3)], [("rb", 1)])

        for bi, blk in enumerate(blocks):
            NB = blk["NB"]
            c0 = blk["c0"]
            dma("sp", x_sb[:, :, 0:NB], xT_d[:, :, c0:c0 + NB], (), [("x", f) for f in range(FT)] + ["s5m", "bbar"], key=("xin",))
            for l in range(DEPTH):
                rmsnorm_mod(l, 0, blk)
                stage(2)
                mixer(l, blk)
                stage(6)
                if l % 2 == 0:
                    rmsnorm_mod(l, 1, blk)
                    for hh_ in range(2):
                        ffn_half(l, blk, f_w1_d[0, :, hh_ * 3072:(hh_ + 1) * 3072], f_w3_d[0, :, hh_ * 3072:(hh_ + 1) * 3072],
                                 f_w2_d[0, hh_ * 3072:(hh_ + 1) * 3072, :], None, None)
                else:
                    rmsnorm_mod(l, 1, blk, want_router=True)
                    moe_router(blk)
                    for e_ in range(8):
                        cbuf = rbuf[2 + e_ % 2]
                        ck = ("rb", 2 + e_ % 2)
                        msk = rbuf[4 + e_ % 2][0:8, :]
                        mk_ = ("rb", 4 + e_ % 2)
                        ts(msk[:, 0:NB], rbuf[1][0:8, 0:NB], ident[0:8, e_:e_ + 1], None, ALU.mult, None,
                           [("rb", 1), "ident"], [mk_])
                        mm(psum[7][:, 0:NB], ones_f[0:8, :], msk[:, 0:NB], True, True, ["ones", mk_], [PS(7)])
                        cp("act", cbuf[:, 0:NB], psum[7][:, 0:NB], [PS(7)], [ck])
                        ffn_half(l, blk, m_w1_d[0, e_], m_w3_d[0, e_], m_w2_d[0, e_], cbuf, ck)
            pb = 0
            for ft in range(FT):
                tq, kq = T32()
                act(tq[:, 0:NB], x_sb[:, ft, 0:NB], AF.Square, [("x", ft)], [kq])
                mm(psum[pb][:, 0:NB], ones_f[:], tq[:, 0:NB], ft == 0, ft == FT - 1, ["ones", kq], [PS(pb)])
            rstd = rbuf[10]
            act(rstd[:, 0:NB], psum[pb][:, 0:NB], AF.Ln, [PS(pb)], [("rb", 10)], bias=1e-6, scale=1.0 / D)
            act(rstd[:, 0:NB], rstd[:, 0:NB], AF.Exp, [("rb", 10)], [("rb", 10)], scale=-0.5)
            for ft in range(FT):
                tt(x_sb[:, ft, 0:NB], x_sb[:, ft, 0:NB], rstd[:, 0:NB], ALU.mult, [("x", ft), ("rb", 10)], [("x", ft)])
                ts(x_sb[:, ft, 0:NB], x_sb[:, ft, 0:NB], small_sb[:, 0, 32 + ft:33 + ft], None, ALU.mult, None,
                   [("x", ft), "small"], [("x", ft)])
            dma("sp", yT_d[:, :, c0:c0 + NB], x_sb[:, :, 0:NB], [("x", f) for f in range(FT)], (), key=("out",))

    except _Stop:
        pass

    dma("sp", o_s5s_d.rearrange("l r p j q -> p l r j q"), s5out[:], ["s5in"], (), key=("out",))
    dma("sp", o_s5p_d.rearrange("l r p j -> p l r j"), s5car[:, :, :, :, 0], ["s5car"], (), key=("out",))
    dma("sp", o_rgs_d.rearrange("l p j q -> p l j q"), rgout[:], ["rgin"], (), key=("out",))
    dma("sp", o_rgp_d.rearrange("l p j -> p l j"), rgcar[:, :, :, 0], ["rgcar"], (), key=("out",))
    dma("sp", o_cvs_d.rearrange("l p j q k -> p l j q k"), cvout[:], ["cvin"], (), key=("out",))
    dma("sp", o_cvp_d.rearrange("l p j k -> p l j k"), cvtail[:, :, :, 0, :], ["cvtail"], (), key=("out",))

    P.emit(final_wait_keys=[("out",)])
    st.close()
    return nc


def _ft_layout(v):
    k = v.shape[-1] // 128
    return np.ascontiguousarray(v.reshape(k, 128).T)


def _prep_small(inp):
    out = np.zeros((DEPTH, 128, NS_SMALL), np.float32)
    for l in range(DEPTH):
        o = out[l]
        o[:, 0:16] = _ft_layout(inp["norm_mix"][l])
        o[:, 16:32] = _ft_layout(inp["norm_ffn"][l])
        o[:, 32:48] = _ft_layout(inp["norm_f"])
        o[:, 48:80] = _ft_layout(inp["b_gate"][l])
        o[:, 80:88] = _ft_layout(inp["s5_b_glu"][l])
        for k in range(4):
            o[:, 88 + k * 8:96 + k * 8] = _ft_layout(inp["rg_conv_w"][l, k])
        o[:, 120:128] = _ft_layout(inp["rg_conv_b"][l])
        o[:, 128:136] = _ft_layout(inp["rg_lam"][l])
        o[:, 136:144] = _ft_layout(inp["rg_b_a"][l].reshape(-1))
        o[:, 144:152] = _ft_layout(inp["rg_b_i"][l].reshape(-1))
        o[:, 152:160] = _ft_layout(inp["s5_d"][l].reshape(-1))
        o[:, 160:256] = _ft_layout(inp["b_ada"][l])
        sp = lambda a: np.ascontiguousarray(a.reshape(32, 2, 64).transpose(1, 2, 0).reshape(128, 32))
        o[:, 256:288] = sp(inp["s5_lam_re"][l])
        o[:, 288:320] = sp(inp["s5_lam_im"][l])
        o[:, 320:352] = sp(np.repeat(inp["s5_log_dt"][l][:, None], 64, axis=1))
    return out


def _prep_s5m(inp):
    out = np.zeros((DEPTH, 128, 4, 32, 32), np.float32)
    for l in range(DEPTH):
        for idx, name in enumerate(("s5_b_re", "s5_b_im")):
            b = inp[name][l].reshape(32, 2, 64, 16)
            for g2 in range(2):
                out[l, g2 * 64:(g2 + 1) * 64, idx, :, g2 * 16:(g2 + 1) * 16] = b[:, g2].transpose(1, 0, 2)
        for idx, name in enumerate(("s5_c_re", "s5_c_im")):
            c = inp[name][l].reshape(32, 2, 16, 64)
            for g2 in range(2):
                out[l, g2 * 64:(g2 + 1) * 64, 2 + idx, :, g2 * 16:(g2 + 1) * 16] = c[:, g2].transpose(2, 0, 1)
    return out


def _prep_rgw(inp):
    out = np.zeros((DEPTH, 128, 2, 8, 128), np.float32)
    for l in range(DEPTH):
        for a, name in enumerate(("rg_w_a", "rg_w_i")):
            w = inp[name][l]
            for j in range(8):
                for h2 in range(2):
                    out[l, h2 * 64:(h2 + 1) * 64, a, j, h2 * 64:(h2 + 1) * 64] = w[j * 2 + h2]
    return out


_NC_CACHE = {}


def kernel(**inp):
    inp = {k: np.asarray(v) for k, v in inp.items()}
    if "nc" not in _NC_CACHE:
        _NC_CACHE["nc"] = build_program()
    nc = _NC_CACHE["nc"]
    in_maps = _make_in_maps(inp)
    res = run_bass_kernel_spmd(nc, in_maps, core_ids=list(range(8)))
    return _assemble(res.results)


def _make_in_maps(inp):
    small = _prep_small(inp)
    s5m = _prep_s5m(inp)
    rgw = _prep_rgw(inp)
    router = np.ascontiguousarray(inp["moe_router"][0].reshape(FT, 128, 8).transpose(1, 0, 2))
    ident = np.eye(128, dtype=np.float32)
    pmask = np.zeros((128, 4), np.float32)
    for jj in range(4):
        pmask[32 * jj:32 * jj + 32, jj] = 1.0
    shared = dict(small=small, s5m=s5m, rgw=rgw, router=router, ident=ident, pmask=pmask)
    for k in ("w_ada", "w_in", "s5_w_glu", "w_gate", "w_br_s5", "w_br_rg", "w_out", "ffn_w1", "ffn_w3", "ffn_w2",
              "moe_w1", "moe_w3", "moe_w2"):
        shared[k] = np.ascontiguousarray(inp[k], dtype=np.float32)
    in_maps = []
    for c in range(8):
        b = c % 4
        qs = slice(c * NSAMP, (c + 1) * NSAMP)
        xtok = np.concatenate([inp["x_prompt"][b], inp["x_sample"][qs].reshape(NSAMP * TS, D)], axis=0)
        xT = np.ascontiguousarray(xtok.reshape(-1, FT, 128).transpose(2, 1, 0))
        cc = np.concatenate([inp["c_prompt"][b:b + 1], inp["c_sample"][qs]], axis=0)
        cT = np.ascontiguousarray(cc.reshape(-1, FT, 128).transpose(2, 1, 0))
        s5st = np.stack([inp["state_s5_re"][:, qs], inp["state_s5_im"][:, qs]], axis=1)
        s5st = s5st.reshape(DEPTH, 2, NSAMP, 32, 2, 64).transpose(0, 1, 4, 5, 3, 2).reshape(DEPTH, 2, 128, 32, NSAMP)
        rgst = inp["state_rglru"][:, qs].reshape(DEPTH, NSAMP, 8, 128).transpose(0, 3, 2, 1)
        cvst = inp["state_conv"][:, qs].reshape(DEPTH, NSAMP, 3, 8, 128).transpose(0, 4, 3, 1, 2)
        m = dict(shared)
        m.update(xT=xT, cT=cT, s5st=np.ascontiguousarray(s5st), rgst=np.ascontiguousarray(rgst),
                 cvst=np.ascontiguousarray(cvst))
        in_maps.append(m)
    return in_maps


def _assemble(R):
    B = 4
    y_prompt = np.zeros((B, SEQ, D), np.float32)
    y_sample = np.zeros((8 * NSAMP, TS, D), np.float32)
    p_s5_re = np.zeros((DEPTH, B, 64, 64), np.float32)
    p_s5_im = np.zeros_like(p_s5_re)
    p_rg = np.zeros((DEPTH, B, 1024), np.float32)
    p_conv = np.zeros((DEPTH, B, 3, 1024), np.float32)
    s_s5_re = np.zeros((DEPTH, 8 * NSAMP, 64, 64), np.float32)
    s_s5_im = np.zeros_like(s_s5_re)
    s_rg = np.zeros((DEPTH, 8 * NSAMP, 1024), np.float32)
    s_conv = np.zeros((DEPTH, 8 * NSAMP, 3, 1024), np.float32)
    for c in range(8):
        r = R[c]
        yT = np.asarray(r["yT"])
        ytok = yT.transpose(2, 1, 0).reshape(-1, D)
        qs = slice(c * NSAMP, (c + 1) * NSAMP)
        y_sample[qs] = ytok[SEQ:].reshape(NSAMP, TS, D)
        s5s = np.asarray(r["o_s5s"]).reshape(DEPTH, 2, 2, 64, 32, NSAMP)
        s5s = s5s.transpose(0, 1, 5, 4, 2, 3).reshape(DEPTH, 2, NSAMP, 64, 64)
        s_s5_re[:, qs] = s5s[:, 0]
        s_s5_im[:, qs] = s5s[:, 1]
        s_rg[:, qs] = np.asarray(r["o_rgs"]).transpose(0, 3, 2, 1).reshape(DEPTH, NSAMP, 1024)
        s_conv[:, qs] = np.asarray(r["o_cvs"]).transpose(0, 3, 4, 2, 1).reshape(DEPTH, NSAMP, 3, 1024)
        if c < B:
            y_prompt[c] = ytok[:SEQ]
            s5p = np.asarray(r["o_s5p"]).reshape(DEPTH, 2, 2, 64, 32)
            s5p = s5p.transpose(0, 1, 4, 2, 3).reshape(DEPTH, 2, 64, 64)
            p_s5_re[:, c] = s5p[:, 0]
            p_s5_im[:, c] = s5p[:, 1]
            p_rg[:, c] = np.asarray(r["o_rgp"]).transpose(0, 2, 1).reshape(DEPTH, 1024)
            p_conv[:, c] = np.asarray(r["o_cvp"]).transpose(0, 3, 2, 1).reshape(DEPTH, 3, 1024)
    return (y_prompt, y_sample, p_s5_re, p_s5_im, p_rg, p_conv, s_s5_re, s_s5_im, s_rg, s_conv)
```

```python
import contextlib
import math
import numpy as np
import concourse.bass as bass
import concourse.mybir as mybir
from concourse.bass_utils import run_bass_kernel_spmd

F32 = mybir.dt.float32
BF16 = mybir.dt.bfloat16
I32 = mybir.dt.int32
AF = mybir.ActivationFunctionType
ALU = mybir.AluOpType

D = 2048
FT = 16
SEQ = 2048
NSAMP = 16
TS = 4
DEPTH = 2
NS_SMALL = 352
TWO_PI = 2.0 * math.pi
GELU_K = 1.5957691216057308


class Prog:
    ENG = ("pe", "act", "dve", "pool", "sp")

    def __init__(self, nc):
        self.nc = nc
        self.ops = []
        self.last_writer = {}
        self.readers = {}
        self.dma_key_count = {}
        self.dma_key_waitall = set()

    def op(self, eng, fn, reads=(), writes=(), dma_key=None, wait_all=False):
        ps_r = [k for k in reads if isinstance(k, tuple) and k and k[0] == "ps"]
        if ps_r:
            reads = [k for k in reads if k not in ps_r]
            writes = list(writes) + [k for k in ps_r if k not in writes]
        idx = len(self.ops)
        deps = set()
        war = set()
        for b in reads:
            w = self.last_writer.get(b)
            if w is not None:
                deps.add(w)
        for b in writes:
            w = self.last_writer.get(b)
            if w is not None:
                deps.add(w)
            for r in self.readers.get(b, {}).values():
                war.add(r)
        o = dict(eng=eng, fn=fn, deps=deps, war=war, dma_key=dma_key, signal=False)
        if dma_key is not None:
            self.dma_key_count[dma_key] = self.dma_key_count.get(dma_key, 0) + 1
            o["dma_n"] = self.dma_key_count[dma_key]
            if wait_all:
                self.dma_key_waitall.add(dma_key)
        self.ops.append(o)
        rk = eng if dma_key is None else ("dma", idx)
        for b in reads:
            self.readers.setdefault(b, {})[rk] = idx
        for b in writes:
            self.last_writer[b] = idx
            self.readers[b] = {}
        return idx

    def emit(self, final_wait_keys=()):
        nc = self.nc
        ops = self.ops
        for o in ops:
            nd = set()
            for d in o["deps"] | o["war"]:
                p = ops[d]
                if p["dma_key"] is None and o["dma_key"] is None and p["eng"] == o["eng"]:
                    if o["eng"] == "pe":
                        continue
                nd.add(d)
            o["deps"] = nd
            for d in nd:
                ops[d]["signal"] = True
        cnt = {e: 0 for e in self.ENG}
        for o in ops:
            if o["dma_key"] is not None:
                k = o["dma_key"]
                n = self.dma_key_count[k] if k in self.dma_key_waitall else o["dma_n"]
                o["sig"] = (("dma", k), 16 * n)
            elif o["signal"]:
                cnt[o["eng"]] += 1
                o["sig"] = (("eng", o["eng"]), cnt[o["eng"]])
        per_eng = {e: [o for o in ops if o["eng"] == e] for e in self.ENG}
        sem_names = [("eng", e) for e in self.ENG] + [("dma", k) for k in self.dma_key_count]
        with contextlib.ExitStack() as st:
            sems = {}
            for i, sn in enumerate(sem_names):
                sems[sn] = st.enter_context(nc.semaphore("s%d" % i))
            block = st.enter_context(nc.Block())
            engobj = {"pe": "tensor", "act": "scalar", "dve": "vector", "pool": "gpsimd", "sp": "sync"}

            def run(e, eng):
                known = {}
                for o in per_eng[e]:
                    need = {}
                    for d in o["deps"]:
                        s, v = ops[d]["sig"]
                        if v > need.get(s, 0):
                            need[s] = v
                    for s, v in need.items():
                        if known.get(s, 0) < v:
                            eng.wait_ge(sems[s], v)
                            known[s] = v
                    ins = o["fn"](eng)
                    if o["dma_key"] is not None:
                        ins.then_inc(sems[("dma", o["dma_key"])], 16)
                    elif o["signal"]:
                        ins.then_inc(sems[("eng", e)], 1)
                if e == "sp":
                    for k in final_wait_keys:
                        v = 16 * self.dma_key_count[k]
                        if known.get(("dma", k), 0) < v:
                            eng.wait_ge(sems[("dma", k)], v)

            for e in self.ENG:
                def mk(e):
                    def f(eng):
                        run(e, eng)
                    return f
                getattr(block, engobj[e])(mk(e))


class _Stop(Exception):
    pass


def build_program(n_prompt_blocks=SEQ // 512, stop_stage=None):
    nc = bass.Bass("TRN2", target_bir_lowering=False)

    def stage(n):
        if stop_stage is not None and n == stop_stage:
            raise _Stop()

    P = Prog(nc)
    st = contextlib.ExitStack()

    def din(name, shape):
        return nc.dram_tensor(name, list(shape), F32, kind="ExternalInput").ap()

    def dout(name, shape):
        return nc.dram_tensor(name, list(shape), F32, kind="ExternalOutput").ap()

    def sb(name, shape, dt=F32):
        return st.enter_context(nc.sbuf_tensor(name, list(shape), dt))

    NTOK = SEQ + NSAMP * TS
    xT_d = din("xT", [128, FT, NTOK])
    cT_d = din("cT", [128, FT, 1 + NSAMP])
    small_d = din("small", [DEPTH, 128, NS_SMALL])
    s5m_d = din("s5m", [DEPTH, 128, 4, 32, 32])
    rgw_d = din("rgw", [DEPTH, 128, 2, 8, 128])
    router_d = din("router", [128, FT, 8])
    ident_d = din("ident", [128, 128])
    pmask_d = din("pmask", [128, 4])
    s5st_d = din("s5st", [DEPTH, 2, 128, 32, NSAMP])
    rgst_d = din("rgst", [DEPTH, 128, 8, NSAMP])
    cvst_d = din("cvst", [DEPTH, 128, 8, NSAMP, 3])
    w_ada_d = din("w_ada", [DEPTH, D, 6 * D])
    w_in_d = din("w_in", [DEPTH, D, 3072])
    w_glu_d = din("s5_w_glu", [DEPTH, 1024, 1024])
    w_gate_d = din("w_gate", [DEPTH, D, 2 * D])
    w_bs_d = din("w_br_s5", [DEPTH, 1024, D])
    w_br_d = din("w_br_rg", [DEPTH, 1024, D])
    w_out_d = din("w_out", [DEPTH, D, D])
    f_w1_d = din("ffn_w1", [1, D, 6144])
    f_w3_d = din("ffn_w3", [1, D, 6144])
    f_w2_d = din("ffn_w2", [1, 6144, D])
    m_w1_d = din("moe_w1", [1, 8, D, 3072])
    m_w3_d = din("moe_w3", [1, 8, D, 3072])
    m_w2_d = din("moe_w2", [1, 8, 3072, D])

    yT_d = dout("yT", [128, FT, NTOK])
    o_s5s_d = dout("o_s5s", [DEPTH, 2, 128, 32, NSAMP])
    o_s5p_d = dout("o_s5p", [DEPTH, 2, 128, 32])
    o_rgs_d = dout("o_rgs", [DEPTH, 128, 8, NSAMP])
    o_rgp_d = dout("o_rgp", [DEPTH, 128, 8])
    o_cvs_d = dout("o_cvs", [DEPTH, 128, 8, NSAMP, 3])
    o_cvp_d = dout("o_cvp", [DEPTH, 128, 8, 3])

    NBMAX = 512
    x_sb = sb("x_sb", [128, FT, NBMAX])
    h_sb = sb("h_sb", [128, FT, NBMAX], BF16)
    mix_sb = sb("mix_sb", [128, 24, NBMAX], BF16)
    NSLAB = 3
    slabs = [sb("slab%d" % i, [128, 16, 128], BF16) for i in range(NSLAB)]
    NTMP = 4
    tmps = [sb("tmp%d" % i, [128, NBMAX]) for i in range(NTMP)]
    NR = 11
    rb_all = sb("rb_all", [128, NR, NBMAX])
    rbuf = [rb_all[:, i, :] for i in range(NR)]
    rb_bf = rb_all.bitcast(BF16)

    def mrg(f, NB):
        return rb_bf[:, f // 2, (f % 2) * NBMAX:(f % 2) * NBMAX + NB]
    xrext = sb("xrext", [128, NBMAX + 64])
    bft = [sb("bft%d" % i, [128, NBMAX], BF16) for i in range(2)]
    um = [sb("um%d" % i, [128, NBMAX], BF16) for i in range(4)]
    PADP = 256
    abp = [sb("abp%d" % i, [128, PADP + NBMAX]) for i in range(4)]
    abs_ = [sb("abs%d" % i, [128, NSAMP, 2 + TS]) for i in range(4)]
    abs2_ = [sb("abs2%d" % i, [128, NSAMP, 2 + TS]) for i in range(4)]
    bft2 = [sb("bft2%d" % i, [128, NBMAX], BF16) for i in range(2)]
    hbfs = [bft, bft2]
    wcp = [sb("wcp%d" % i, [128, 128], BF16) for i in range(8)]
    small_sb = sb("small_sb", [128, DEPTH, NS_SMALL])
    mod_sb = sb("mod_sb", [128, DEPTH * 6, FT, 1 + NSAMP, 1])
    cT_sb = sb("cT_sb", [128, FT, 1 + NSAMP])
    cs_bf = sb("cs_bf", [128, FT, 1 + NSAMP], BF16)
    ident = sb("ident_sb", [128, 128])
    router_sb = sb("router_sb", [128, FT, 8])
    s5m_sb = x_sb[:, 0:8, :].rearrange("p a b -> p (a b)").rearrange("p (i j m) -> p i j m", i=4, j=32)
    bbar = x_sb[:, 8:12, :].rearrange("p a b -> p (a b)").rearrange("p (i j m) -> p i j m", i=2, j=32)
    wbT = sb("wbT", [128, DEPTH, 2, 8, 128], BF16)
    cb = sb("cb", [128, DEPTH, 2, 32, 32], BF16)
    NLV = 9
    pw = sb("pw", [128, DEPTH, 32, NLV, 3])
    sp_t = [sb("spt%d" % i, [128, 32]) for i in range(14)]
    sp_i = sb("spi", [128, 32], I32)
    rgw_sb = sb("rgw_sb", [128, DEPTH, 2, 8, 128], BF16)
    rgc = sb("rgc", [128, DEPTH, 2, 8])
    s5car = sb("s5car", [128, DEPTH, 2, 32, 1])
    rgcar = sb("rgcar", [128, DEPTH, 8, 1])
    cvtail = sb("cvtail", [128, DEPTH, 8, 1, 3])
    s5in = sb("s5in", [128, DEPTH, 2, 32, NSAMP])
    s5out = s5in
    rgin = sb("rgin", [128, DEPTH, 8, NSAMP])
    rgout = rgin
    cvin = sb("cvin", [128, DEPTH, 8, NSAMP, 3])
    cvout = cvin
    car4 = sb("car4", [128, 4, NSAMP])
    lgT = sb("lgT", [128, 4, 8])
    mx8 = sb("mx8", [128, 4, 8])
    cmb = sb("cmb", [128, 4, 8])
    cmbt = sb("cmbt", [128, 4, 8])
    den = sb("den", [128, 4, 1])
    ones_f = sb("ones_f", [128, 128])
    pmask = sb("pmask_sb", [128, 4])

    psum = [st.enter_context(nc.psum_tensor("ps%d" % i, [128, 512], F32)) for i in range(8)]

    def mm(out, lhsT, rhs, start, stop, reads, writes):
        P.op("pe", lambda e, a=out, b=lhsT, c=rhs, s=start, t=stop: e.matmul(a, lhsT=b, rhs=c, start=s, stop=t),
             reads, writes)

    def act(out, in_, func, reads, writes, bias=None, scale=None):
        kw = {}
        if bias is not None:
            kw["bias"] = bias
        if scale is not None:
            kw["scale"] = scale
        P.op("act", lambda e, a=out, b=in_, f=func, k=kw: e.activation(a, b, f, **k), reads, writes)

    def tt(out, a, b, op, reads, writes, eng="dve"):
        P.op(eng, lambda e, o=out, x=a, y=b, p=op: e.tensor_tensor(out=o, in0=x, in1=y, op=p), reads, writes)

    def ts(out, a, s1, s2, op0, op1, reads, writes, eng="dve"):
        if s2 is None:
            P.op(eng, lambda e, o=out, x=a, u=s1, p=op0: e.tensor_scalar(o, x, u, None, p), reads, writes)
        else:
            P.op(eng, lambda e, o=out, x=a, u=s1, v=s2, p=op0, q=op1: e.tensor_scalar(o, x, u, v, p, q), reads, writes)

    def stt(out, in0, scalar, in1, op0, op1, reads, writes, eng="dve"):
        P.op(eng, lambda e, o=out, x=in0, s=scalar, y=in1, p=op0, q=op1:
             e.scalar_tensor_tensor(out=o, in0=x, scalar=s, in1=y, op0=p, op1=q), reads, writes)

    def cp(eng, out, in_, reads, writes):
        if eng == "act":
            P.op("act", lambda e, o=out, i=in_: e.copy(o, i), reads, writes)
        else:
            P.op(eng, lambda e, o=out, i=in_: e.tensor_copy(o, i), reads, writes)

    def memset(eng, ap, val, writes):
        P.op(eng, lambda e, a=ap, v=val: e.memset(a, v), (), writes)

    def dma(q, out, in_, reads, writes, key, wait_all=False):
        P.op(q, lambda e, o=out, i=in_: e.dma_start(out=o, in_=i), reads, writes, dma_key=key, wait_all=wait_all)

    slab_ctr = [0]
    NSCR = 1056
    SCR_PER = 448
    wscrs = [nc.dram_tensor("wscr%d" % i, [SCR_PER, 128, 2048], BF16).ap() for i in range(3)]
    scr_seen = set()
    blk_tile = [None]

    def load_slab(src_ap, K):
        s = slab_ctr[0] % NSLAB
        slab_ctr[0] += 1
        key = ("slab", s)
        dst = slabs[s][:, 0:K, :]
        if blk_tile[0] is None:
            dma("pool", dst, src_ap.rearrange("(k p) m -> p k m", p=128), (), [key], key=("slabq", s))
            return slabs[s], key
        tid = blk_tile[0]
        blk_tile[0] += 1
        assert tid < NSCR
        scr_v = wscrs[tid // SCR_PER][tid % SCR_PER, :, 0:K * 128].rearrange("p (k m) -> p k m", m=128)
        if tid not in scr_seen:
            scr_seen.add(tid)
            dma("pool", dst, src_ap.rearrange("(k p) m -> p k m", p=128), (), [key], key=("slabq", s))
            dma("sp", scr_v, dst, [key], [("scr", tid)], key=("wbq", s))
        else:
            dma("sp", dst, scr_v, [("scr", tid)], [key], key=("slabh", s))
        return slabs[s], key

    tmp_ctr = [0]

    def T32():
        i = tmp_ctr[0] % NTMP
        tmp_ctr[0] += 1
        return tmps[i], ("tmp", i)

    PS = lambda i: ("ps", i)

    C = "const"
    dma("sp", small_sb[:], small_d.rearrange("l p n -> p l n"), (), ["small"], key=C, wait_all=True)
    dma("sp", cT_sb[:], cT_d, (), ["cT"], key=C, wait_all=True)
    dma("sp", ident[:], ident_d, (), ["ident"], key=C, wait_all=True)
    dma("sp", pmask[:], pmask_d, (), ["pmask"], key=C, wait_all=True)
    dma("sp", router_sb[:], router_d, (), ["router"], key=C, wait_all=True)
    dma("sp", s5in[:], s5st_d.rearrange("l r p j q -> p l r j q"), (), [("s5in", j) for j in range(32)], key=C, wait_all=True)
    dma("sp", rgin[:], rgst_d.rearrange("l p j q -> p l j q"), (), ["rgin"], key=C, wait_all=True)
    dma("sp", cvin[:], cvst_d.rearrange("l p j q k -> p l j q k"), (), ["cvin"], key=C, wait_all=True)
    dma("pool", rgw_sb[:], rgw_d.rearrange("l p a j m -> p l a j m"), (), ["rgw"], key=("rgwq",))
    memset("dve", ones_f[:], 1.0, ["ones"])
    for i in range(4):
        memset("dve", abp[i][:], 0.0, [("abp", i)])
        memset("dve", abs_[i][:], 0.0, [("abs", i)])
        memset("dve", abs2_[i][:], 0.0, [("abs2", i)])
    for i in range(8):
        memset("dve", wcp[i][:], 0.0, [("wcp", i)])
    memset("dve", s5car[:], 0.0, [("s5car", j) for j in range(32)])
    memset("dve", rgcar[:], 0.0, ["rgcar"])
    memset("dve", cvtail[:], 0.0, ["cvtail"])

    def smallv(l, c0, n):
        return small_sb[:, l, c0:c0 + n]

    try:
        act(cs_bf[:], cT_sb[:], AF.Silu, ["cT"], ["cs"])
        NSQ = 1 + NSAMP
        for l in range(DEPTH):
            for kind in range(6):
                pb = (l * 6 + kind) % 4
                for ft in range(FT):
                    col0 = (kind * FT + ft) * 128
                    slab, skey = load_slab(w_ada_d[l, :, col0:col0 + 128], 16)
                    for k in range(16):
                        mm(psum[pb][:, ft * NSQ:(ft + 1) * NSQ], slab[:, k, :], cs_bf[:, k, :], k == 0, k == 15,
                           [skey, "cs"], [PS(pb)])
                bias_b = small_sb[:, l, 160 + kind * FT:160 + (kind + 1) * FT].rearrange("p (f o) -> p f o", o=1) \
                    .to_broadcast([128, FT, NSQ])
                tt(mod_sb[:, l * 6 + kind, :, :, 0], psum[pb][:, 0:FT * NSQ].rearrange("p (f s) -> p f s", s=NSQ),
                   bias_b, ALU.add, [PS(pb), "small"], [("mod", l, kind)])
            for which, (kind, ncol) in enumerate(((1, 0), (4, 16))):
                tmpv = mod_sb[:, l * 6 + kind, :, :, 0]
                ts(tmpv, tmpv, 1.0, None, ALU.add, None, [("mod", l, kind)], [("mod", l, kind)])
                nb = small_sb[:, l, ncol:ncol + FT].rearrange("p (f o) -> p f o", o=1).to_broadcast([128, FT, NSQ])
                tt(tmpv, tmpv, nb, ALU.mult, [("mod", l, kind), "small"], [("mod", l, kind)])

        stage(0)
        for l in range(DEPTH):
            lamre = smallv(l, 256, 32)
            lamim = smallv(l, 288, 32)
            logdt = smallv(l, 320, 32)
            t = sp_t
            K = lambda i: ("spt", i)
            S = ["small"]
            act(t[0][:], logdt, AF.Exp, S, [K(0)])
            tt(t[1][:], lamre, t[0][:], ALU.mult, S + [K(0)], [K(1)])
            act(t[1][:], t[1][:], AF.Exp, [K(1)], [K(1)])
            tt(t[2][:], lamim, t[0][:], ALU.mult, S + [K(0)], [K(2)])
            ts(t[2][:], t[2][:], 1.0 / TWO_PI, None, ALU.mult, None, [K(2)], [K(2)])

            def sin_of(dst, kdst, shift):
                ts(t[3][:], t[2][:], shift, None, ALU.add, None, [K(2)], [K(3)])
                cp("dve", sp_i[:], t[3][:], [K(3)], ["spi"])
                cp("dve", t[4][:], sp_i[:], ["spi"], [K(4)])
                tt(t[3][:], t[3][:], t[4][:], ALU.subtract, [K(3), K(4)], [K(3)])
                ts(t[4][:], t[3][:], 0.5, None, ALU.is_gt, None, [K(3)], [K(4)])
                tt(t[3][:], t[3][:], t[4][:], ALU.subtract, [K(3), K(4)], [K(3)])
                ts(t[4][:], t[3][:], -0.5, None, ALU.is_lt, None, [K(3)], [K(4)])
                tt(t[3][:], t[3][:], t[4][:], ALU.add, [K(3), K(4)], [K(3)])
                act(dst, t[3][:], AF.Sin, [K(3)], [kdst], scale=TWO_PI)

            sin_of(t[5][:], K(5), 0.0)
            sin_of(t[6][:], K(6), 0.25)
            lre = pw[:, l, :, 0, 0]
            lim = pw[:, l, :, 0, 1]
            lnim = pw[:, l, :, 0, 2]
            PWK = ("pw", l)
            tt(lre, t[1][:], t[6][:], ALU.mult, [K(1), K(6)], [PWK])
            tt(lim, t[1][:], t[5][:], ALU.mult, [K(1), K(5)], [PWK])
            ts(lnim, lim, -1.0, None, ALU.mult, None, [PWK], [PWK])
            for lv in range(1, NLV):
                a_re = pw[:, l, :, lv - 1, 0]
                a_im = pw[:, l, :, lv - 1, 1]
                tt(t[7][:], a_re, a_re, ALU.mult, [PWK], [K(7)])
                tt(t[8][:], a_im, a_im, ALU.mult, [PWK], [K(8)])
                tt(pw[:, l, :, lv, 0], t[7][:], t[8][:], ALU.subtract, [K(7), K(8)], [PWK])
                tt(t[7][:], a_re, a_im, ALU.mult, [PWK], [K(7)])
                ts(pw[:, l, :, lv, 1], t[7][:], 2.0, None, ALU.mult, None, [K(7)], [PWK])
                ts(pw[:, l, :, lv, 2], t[7][:], -2.0, None, ALU.mult, None, [K(7)], [PWK])
            ts(t[7][:], lre, -1.0, None, ALU.add, None, [PWK], [K(7)])
            tt(t[8][:], lamre, lamre, ALU.mult, S, [K(8)])
            tt(t[9][:], lamim, lamim, ALU.mult, S, [K(9)])
            tt(t[8][:], t[8][:], t[9][:], ALU.add, [K(8), K(9)], [K(8)])
            P.op("dve", lambda e, o=t[8][:]: e.reciprocal(o, o), [K(8)], [K(8)])
            tt(t[9][:], t[7][:], lamre, ALU.mult, [K(7)] + S, [K(9)])
            tt(t[10][:], lim, lamim, ALU.mult, [PWK] + S, [K(10)])
            tt(t[9][:], t[9][:], t[10][:], ALU.add, [K(9), K(10)], [K(9)])
            tt(t[9][:], t[9][:], t[8][:], ALU.mult, [K(9), K(8)], [K(9)])
            tt(t[10][:], lim, lamre, ALU.mult, [PWK] + S, [K(10)])
            tt(t[11][:], t[7][:], lamim, ALU.mult, [K(7)] + S, [K(11)])
            tt(t[10][:], t[10][:], t[11][:], ALU.subtract, [K(10), K(11)], [K(10)])
            tt(t[10][:], t[10][:], t[8][:], ALU.mult, [K(10), K(8)], [K(10)])
            dma("sp", s5m_sb, s5m_d[l], (), ["s5m"], key=("s5m",))
            bre = s5m_sb[:, 0]
            bim = s5m_sb[:, 1]
            for hlf in range(2):
                js = slice(hlf * 16, hlf * 16 + 16)
                s0 = rbuf[0][:, :].rearrange("p (j m) -> p j m", m=32)
                s1 = rbuf[1][:, :].rearrange("p (j m) -> p j m", m=32)
                cre_h = t[9][:, js].rearrange("p (j o) -> p j o", o=1).to_broadcast([128, 16, 32])
                cim_h = t[10][:, js].rearrange("p (j o) -> p j o", o=1).to_broadcast([128, 16, 32])
                tt(s0, bre[:, js, :], cre_h, ALU.mult, ["s5m", K(9)], [("rb", 0)])
                tt(s1, bim[:, js, :], cim_h, ALU.mult, ["s5m", K(10)], [("rb", 1)])
                tt(bbar[:, 0, js, :], s0, s1, ALU.subtract, [("rb", 0), ("rb", 1)], ["bbar"])
                tt(s0, bim[:, js, :], cre_h, ALU.mult, ["s5m", K(9)], [("rb", 0)])
                tt(s1, bre[:, js, :], cim_h, ALU.mult, ["s5m", K(10)], [("rb", 1)])
                tt(bbar[:, 1, js, :], s0, s1, ALU.add, [("rb", 0), ("rb", 1)], ["bbar"])
            for ri in range(2):
                for ct in range(8):
                    pb = 4 + (ri * 8 + ct) % 4
                    src = bbar[:, ri, ct * 4:(ct + 1) * 4, :].rearrange("p j m -> p (j m)")
                    P.op("pe", lambda e, o=psum[pb][:, 0:128], i=src: e.transpose(o, i, ident[:]),
                         ["bbar", "ident"], [PS(pb)])
                    cp("act", wbT[:, l, ri, ct, :], psum[pb][:, 0:128], [PS(pb)], [("wbT", l)])
            cp("act", cb[:, l, 0], s5m_sb[:, 2], ["s5m"], [("cb", l)])
            ts(cb[:, l, 1], s5m_sb[:, 3], -1.0, None, ALU.mult, None, ["s5m"], [("cb", l)])
            rl = smallv(l, 128, 8)
            act(rgc[:, l, 0, :], rl, AF.Exp, S, [("rgc", l)], scale=-1.0)
            act(rgc[:, l, 0, :], rgc[:, l, 0, :], AF.Ln, [("rgc", l)], [("rgc", l)], bias=1.0)
            ts(rgc[:, l, 1, :], rgc[:, l, 0, :], -16.0, None, ALU.mult, None, [("rgc", l)], [("rgc", l)])
            ts(rgc[:, l, 0, :], rgc[:, l, 0, :], -8.0, None, ALU.mult, None, [("rgc", l)], [("rgc", l)])

        stage(1)
        blocks = []
        for b in range(n_prompt_blocks):
            blocks.append(dict(c0=b * 512, NB=512, nseq=1, T=512, samp=False, last=(b == SEQ // 512 - 1)))
        blocks.append(dict(c0=SEQ, NB=NSAMP * TS, nseq=NSAMP, T=TS, samp=True, last=True))

        def v3(ap, blk):
            return ap.rearrange("p (q t) -> p q t", t=blk["T"])

        def modb(idx_tensor, idx, ft, blk):
            s0, s1 = (1, 1 + NSAMP) if blk["samp"] else (0, 1)
            return idx_tensor[:, idx, ft, s0:s1, :].to_broadcast([128, blk["nseq"], blk["T"]])

        def gelu_from(src32, skey, out_bf, okeys):
            t1, k1 = T32()
            act(t1[:, 0:NBc[0]], src32, AF.Square, [skey], [k1])
            ts(t1[:, 0:NBc[0]], t1[:, 0:NBc[0]], 0.044715, 1.0, ALU.mult, ALU.add, [k1], [k1])
            tt(t1[:, 0:NBc[0]], t1[:, 0:NBc[0]], src32, ALU.mult, [k1, skey], [k1])
            act(t1[:, 0:NBc[0]], t1[:, 0:NBc[0]], AF.Sigmoid, [k1], [k1], scale=GELU_K)
            tt(out_bf, src32, t1[:, 0:NBc[0]], ALU.mult, [skey, k1], okeys)

        NBc = [512]

        def rmsnorm_mod(l, which, blk, want_router=False):
            NB = blk["NB"]
            shk = 0 if which == 0 else 3
            pb = 0
            for ft in range(FT):
                tq, kq = T32()
                act(tq[:, 0:NB], x_sb[:, ft, 0:NB], AF.Square, [("x", ft)], [kq])
                mm(psum[pb][:, 0:NB], ones_f[:], tq[:, 0:NB], ft == 0, ft == FT - 1, ["ones", kq], [PS(pb)])
            rstd = rbuf[10]
            act(rstd[:, 0:NB], psum[pb][:, 0:NB], AF.Ln, [PS(pb)], [("rb", 10)], bias=1e-6, scale=1.0 / D)
            act(rstd[:, 0:NB], rstd[:, 0:NB], AF.Exp, [("rb", 10)], [("rb", 10)], scale=-0.5)
            for ft in range(FT):
                tq, kq = T32()
                tt(tq[:, 0:NB], x_sb[:, ft, 0:NB], rstd[:, 0:NB], ALU.mult, [("x", ft), ("rb", 10)], [kq])
                tt(v3(tq[:, 0:NB], blk), v3(tq[:, 0:NB], blk), modb(mod_sb, l * 6 + (1 if which == 0 else 4), ft, blk), ALU.mult,
                   [kq, ("mod", l, 1 if which == 0 else 4)], [kq])
                if want_router:
                    tt(v3(tq[:, 0:NB], blk), v3(tq[:, 0:NB], blk), modb(mod_sb, l * 6 + shk, ft, blk), ALU.add,
                       [kq, ("mod", l, shk)], [kq])
                    mm(psum[1][0:8, 0:NB], router_sb[:, ft, :], tq[:, 0:NB], ft == 0, ft == FT - 1,
                       ["router", kq], [PS(1)])
                    cp("act", h_sb[:, ft, 0:NB], tq[:, 0:NB], [kq], [("h", ft)])
                else:
                    tt(v3(h_sb[:, ft, 0:NB], blk), v3(tq[:, 0:NB], blk), modb(mod_sb, l * 6 + shk, ft, blk), ALU.add,
                       [kq, ("mod", l, shk)], [("h", ft)])

        def proj_tile(w_ap, kchunks, rhs_fn, rhs_keys, pb, NB):
            slab, skey = load_slab(w_ap, kchunks)
            for k in range(kchunks):
                mm(psum[pb][:, 0:NB], slab[:, k, :], rhs_fn(k), k == 0, k == kchunks - 1,
                   [skey] + [rhs_keys(k)], [PS(pb)])

        H_ALL = [("h", f) for f in range(FT)]

        def mixer(l, blk):
            NB, nseq, T = blk["NB"], blk["nseq"], blk["T"]
            NBc[0] = NB
            samp = blk["samp"]
            hk = lambda k: ("h", k)
            hf = lambda k: h_sb[:, k, 0:NB]
            pad = 2 if samp else PADP
            if samp:
                ABS = [abs_, abs2_]
                ABKS = [[[("abs", i)] for i in range(4)], [[("abs2", i)] for i in range(4)]]
            else:
                ab_rb = [rb_all[:, 2 + 2 * i:4 + 2 * i, :].rearrange("p a b -> p (a b)")[:, 0:PADP + NBMAX] for i in range(4)]
                ABS = [[a[:] for a in abp], ab_rb]
                ABKS = [[[("abp", i)] for i in range(4)], [[("rb", 2 + 2 * i), ("rb", 3 + 2 * i)] for i in range(4)]]
                for i in range(4):
                    memset("pool", ab_rb[i][:, 0:PADP], 0.0, ABKS[1][i])

            def abk(par, i):
                return list(ABKS[par][i])
            levels = []
            d = 1
            lv = 0
            while d < T:
                levels.append((lv, d))
                d *= 2
                lv += 1

            def abv(par, i, off):
                if samp:
                    return ABS[par][i][:, :, pad - off:pad - off + T]
                return ABS[par][i][:, pad - off:pad - off + T].rearrange("p (q t) -> p q t", q=1)

            for ct in range(8):
                proj_tile(w_in_d[l, :, ct * 128:(ct + 1) * 128], 16, hf, hk, 0, NB)
                stage(27)
                u32 = rbuf[0]
                cp("act", u32[:, 0:NB], psum[0][:, 0:NB], [PS(0)], [("rb", 0)])
                stage(28)
                for jj in range(4):
                    ts(um[jj][:, 0:NB], psum[0][:, 0:NB], pmask[:, jj:jj + 1], None, ALU.mult, None,
                       [PS(0), "pmask"], [("um", jj)])
                stage(21)
                for jj in range(4):
                    for ri in range(2):
                        cp("act", wcp[jj * 2 + ri][:, 32 * jj:32 * jj + 32], cb[:, l, ri, ct * 4 + jj, :],
                           [("cb", l)], [("wcp", jj * 2 + ri)])
                stage(22)
                PWK = ("pw", l)

                def s5_front(jj):
                    par = jj % 2
                    pbs = (1, 2) if par == 0 else (4, 5)
                    for ri in range(2):
                        mm(psum[pbs[ri]][:, 0:NB], wbT[:, l, ri, ct, :], um[jj][:, 0:NB], True, True,
                           [("wbT", l), ("um", jj)], [PS(pbs[ri])])
                    for ri in range(2):
                        cp("act", abv(par, ri, 0), v3(psum[pbs[ri]][:, 0:NB], blk), [PS(pbs[ri])], abk(par, ri))

                def s5_scan(jj):
                    par = jj % 2
                    E = "dve"
                    j = ct * 4 + jj
                    if samp:
                        cre = s5in[:, l, 0, j, :]
                        cim = s5in[:, l, 1, j, :]
                        ckeys = [("s5in", j)]
                    else:
                        cre = s5car[:, l, 0, j, :]
                        cim = s5car[:, l, 1, j, :]
                        ckeys = [("s5car", j)]
                    L_re = pw[:, l, j, 0, 0:1]
                    L_im = pw[:, l, j, 0, 1:2]
                    L_nim = pw[:, l, j, 0, 2:3]
                    a0re = abv(par, 0, 0)[:, :, 0]
                    a0im = abv(par, 1, 0)[:, :, 0]
                    stt(a0re, cre, L_re, a0re, ALU.mult, ALU.add, ckeys + [PWK] + abk(par, 0), abk(par, 0), eng=E)
                    stt(a0re, cim, L_nim, a0re, ALU.mult, ALU.add, ckeys + [PWK] + abk(par, 0), abk(par, 0), eng=E)
                    stt(a0im, cim, L_re, a0im, ALU.mult, ALU.add, ckeys + [PWK] + abk(par, 1), abk(par, 1), eng=E)
                    stt(a0im, cre, L_im, a0im, ALU.mult, ALU.add, ckeys + [PWK] + abk(par, 1), abk(par, 1), eng=E)
                    cur = 0
                    for (lv_, d_) in levels:
                        s_re, s_im = cur * 2, cur * 2 + 1
                        d_re, d_im = (1 - cur) * 2, (1 - cur) * 2 + 1
                        ar = pw[:, l, j, lv_, 0:1]
                        ai = pw[:, l, j, lv_, 1:2]
                        nai = pw[:, l, j, lv_, 2:3]
                        stt(abv(par, d_re, 0), abv(par, s_re, d_), ar, abv(par, s_re, 0), ALU.mult, ALU.add,
                            abk(par, s_re) + [PWK], abk(par, d_re), eng=E)
                        stt(abv(par, d_re, 0), abv(par, s_im, d_), nai, abv(par, d_re, 0), ALU.mult, ALU.add,
                            abk(par, s_im) + abk(par, d_re) + [PWK], abk(par, d_re), eng=E)
                        stt(abv(par, d_im, 0), abv(par, s_im, d_), ar, abv(par, s_im, 0), ALU.mult, ALU.add,
                            abk(par, s_im) + [PWK], abk(par, d_im), eng=E)
                        stt(abv(par, d_im, 0), abv(par, s_re, d_), ai, abv(par, d_im, 0), ALU.mult, ALU.add,
                            abk(par, s_re) + abk(par, d_im) + [PWK], abk(par, d_im), eng=E)
                        cur = 1 - cur
                    return cur

                def s5_back(jj, cur):
                    par = jj % 2
                    E = "dve"
                    j = ct * 4 + jj
                    f_re, f_im = cur * 2, cur * 2 + 1
                    hb = hbfs[par]
                    hbk = [("hbf", par, 0), ("hbf", par, 1)]
                    cp("act", v3(hb[0][:, 0:NB], blk), abv(par, f_re, 0), abk(par, f_re), [hbk[0]])
                    cp("act", v3(hb[1][:, 0:NB], blk), abv(par, f_im, 0), abk(par, f_im), [hbk[1]])
                    if samp:
                        cp(E, s5out[:, l, 0, j, :], abv(par, f_re, 0)[:, :, T - 1], abk(par, f_re), [("s5in", j)])
                        cp(E, s5out[:, l, 1, j, :], abv(par, f_im, 0)[:, :, T - 1], abk(par, f_im), [("s5in", j)])
                    else:
                        cp(E, s5car[:, l, 0, j, :], abv(par, f_re, 0)[:, :, T - 1], abk(par, f_re), [("s5car", j)])
                        cp(E, s5car[:, l, 1, j, :], abv(par, f_im, 0)[:, :, T - 1], abk(par, f_im), [("s5car", j)])
                    for ri in range(2):
                        mm(psum[3][:, 0:NB], wcp[jj * 2 + ri][:], hb[ri][:, 0:NB], jj == 0 and ri == 0,
                           jj == 3 and ri == 1, [("wcp", jj * 2 + ri), hbk[ri]], [PS(3)])

                for grp in ((0, 1), (2, 3)):
                    for jj in grp:
                        s5_front(jj)
                    curs = [s5_scan(jj) for jj in grp]
                    for jj, cur in zip(grp, curs):
                        s5_back(jj, cur)
                stage(26)
                ypre = rbuf[1]
                stt(ypre[:, 0:NB], u32[:, 0:NB], small_sb[:, l, 152 + ct:153 + ct], psum[3][:, 0:NB], ALU.mult, ALU.add,
                    [("rb", 0), "small", PS(3)], [("rb", 1)])
                gelu_from(ypre[:, 0:NB], ("rb", 1), mix_sb[:, ct, 0:NB], [("mix", ct)])
            stage(3)
            for ct in range(8):
                pb = 1 + ct % 2
                proj_tile(w_glu_d[l, :, ct * 128:(ct + 1) * 128], 8, lambda k: mix_sb[:, k, 0:NB], lambda k: ("mix", k), pb, NB)
                tg, kg = T32()
                act(tg[:, 0:NB], psum[pb][:, 0:NB], AF.Sigmoid, [PS(pb), "small"], [kg], bias=small_sb[:, l, 80 + ct:81 + ct])
                tt(mix_sb[:, 8 + ct, 0:NB], mix_sb[:, ct, 0:NB], tg[:, 0:NB], ALU.mult, [("mix", ct), kg], [("mix", 8 + ct)])

            stage(4)
            HX = 3
            for j in range(8):
                proj_tile(w_in_d[l, :, 1024 + j * 128:1024 + (j + 1) * 128], 16, hf, hk, 4, NB)
                proj_tile(w_in_d[l, :, 2048 + j * 128:2048 + (j + 1) * 128], 16, hf, hk, 5, NB)
                xe = xrext[:, 0:nseq * (T + HX)].rearrange("p (q t) -> p q t", t=T + HX)
                XK = "xrext"
                cp("act", xe[:, :, HX:HX + T], v3(psum[4][:, 0:NB], blk), [PS(4)], [XK])
                if samp:
                    cp("dve", xe[:, :, 0:HX], cvin[:, l, j, :, :], ["cvin", XK], [XK])
                else:
                    cp("dve", xe[:, :, 0:HX], cvtail[:, l, j, :, :], ["cvtail", XK], [XK])
                gy32 = rbuf[2]
                cp("act", gy32[:, 0:NB], psum[5][:, 0:NB], [PS(5)], [("rb", 2)])
                gelu_from(gy32[:, 0:NB], ("rb", 2), bft[0][:, 0:NB], [("bft", 0)])
                xc = rbuf[3]
                xcv = v3(xc[:, 0:NB], blk)
                cw = lambda k: small_sb[:, l, 88 + k * 8 + j:89 + k * 8 + j]
                ts(xcv, xe[:, :, 0:T], cw(0), small_sb[:, l, 120 + j:121 + j], ALU.mult, ALU.add, [XK, "small"], [("rb", 3)])
                for k in range(1, 4):
                    stt(xcv, xe[:, :, k:k + T], cw(k), xcv, ALU.mult, ALU.add, [XK, "small", ("rb", 3)], [("rb", 3)])
                if samp:
                    cp("dve", cvout[:, l, j, :, :], xe[:, :, T:T + HX], [XK], ["cvin"])
                else:
                    cp("dve", cvtail[:, l, j, :, :], xe[:, :, T:T + HX], [XK], ["cvtail"])
                cp("act", bft[1][:, 0:NB], xc[:, 0:NB], [("rb", 3)], [("bft", 1)])
                mm(psum[6][:, 0:NB], rgw_sb[:, l, 0, j, :], bft[1][:, 0:NB], True, True, ["rgw", ("bft", 1)], [PS(6)])
                mm(psum[7][:, 0:NB], rgw_sb[:, l, 1, j, :], bft[1][:, 0:NB], True, True, ["rgw", ("bft", 1)], [PS(7)])
                r32, i32, a32, e2 = rbuf[4], rbuf[5], rbuf[6], rbuf[7]
                act(r32[:, 0:NB], psum[6][:, 0:NB], AF.Sigmoid, [PS(6), "small"], [("rb", 4)], bias=small_sb[:, l, 136 + j:137 + j])
                act(i32[:, 0:NB], psum[7][:, 0:NB], AF.Sigmoid, [PS(7), "small"], [("rb", 5)], bias=small_sb[:, l, 144 + j:145 + j])
                act(a32[:, 0:NB], r32[:, 0:NB], AF.Exp, [("rb", 4), ("rgc", l)], [("rb", 6)], scale=rgc[:, l, 0, j:j + 1])
                act(e2[:, 0:NB], r32[:, 0:NB], AF.Exp, [("rb", 4), ("rgc", l)], [("rb", 7)], scale=rgc[:, l, 1, j:j + 1])
                act(e2[:, 0:NB], e2[:, 0:NB], AF.Ln, [("rb", 7)], [("rb", 7)], bias=1.0, scale=-1.0)
                act(e2[:, 0:NB], e2[:, 0:NB], AF.Exp, [("rb", 7)], [("rb", 7)], scale=0.5)
                bx = rbuf[8]
                tt(bx[:, 0:NB], i32[:, 0:NB], xc[:, 0:NB], ALU.mult, [("rb", 5), ("rb", 3)], [("rb", 8)])
                tt(bx[:, 0:NB], bx[:, 0:NB], e2[:, 0:NB], ALU.mult, [("rb", 8), ("rb", 7)], [("rb", 8)])
                a3 = v3(a32[:, 0:NB], blk)
                b3 = v3(bx[:, 0:NB], blk)
                if samp:
                    h0 = rgin[:, l, j, :]
                    h0k = ["rgin"]
                else:
                    h0 = rgcar[:, l, j, :]
                    h0k = ["rgcar"]
                tcar = car4[:, 0, 0:nseq]
                tt(tcar, a3[:, :, 0], h0, ALU.mult, [("rb", 6)] + h0k, ["car4"])
                tt(b3[:, :, 0], b3[:, :, 0], tcar, ALU.add, [("rb", 8), "car4"], [("rb", 8)])
                memset("dve", a3[:, :, 0], 0.0, [("rb", 6)])
                hh = rbuf[9]
                P.op("dve", lambda e, o=hh[:, 0:NB], a=a32[:, 0:NB], b=bx[:, 0:NB]:
                     e.tensor_tensor_scan(out=o, data0=a, data1=b, initial=0.0, op0=ALU.mult, op1=ALU.add),
                     [("rb", 6), ("rb", 8)], [("rb", 9)])
                h3 = v3(hh[:, 0:NB], blk)
                if samp:
                    cp("dve", rgout[:, l, j, :], h3[:, :, T - 1], [("rb", 9)], ["rgin"])
                else:
                    cp("dve", rgcar[:, l, j, :], h3[:, :, T - 1], [("rb", 9)], ["rgcar"])
                tt(mix_sb[:, 16 + j, 0:NB], hh[:, 0:NB], bft[0][:, 0:NB], ALU.mult, [("rb", 9), ("bft", 0)], [("mix", 16 + j)])

            stage(5)
            for f in range(FT):
                pb0 = 4 * (f % 2)
                proj_tile(w_gate_d[l, :, f * 128:(f + 1) * 128], 16, hf, hk, pb0, NB)
                proj_tile(w_gate_d[l, :, D + f * 128:D + (f + 1) * 128], 16, hf, hk, pb0 + 1, NB)
                proj_tile(w_bs_d[l, :, f * 128:(f + 1) * 128], 8, lambda k: mix_sb[:, 8 + k, 0:NB], lambda k: ("mix", 8 + k), pb0 + 2, NB)
                proj_tile(w_br_d[l, :, f * 128:(f + 1) * 128], 8, lambda k: mix_sb[:, 16 + k, 0:NB], lambda k: ("mix", 16 + k), pb0 + 3, NB)
                sa, ka = T32()
                sb_, kb = T32()
                act(sa[:, 0:NB], psum[pb0][:, 0:NB], AF.Sigmoid, [PS(pb0), "small"], [ka], bias=small_sb[:, l, 48 + f:49 + f])
                act(sb_[:, 0:NB], psum[pb0 + 1][:, 0:NB], AF.Sigmoid, [PS(pb0 + 1), "small"], [kb], bias=small_sb[:, l, 64 + f:65 + f])
                tt(sa[:, 0:NB], sa[:, 0:NB], psum[pb0 + 2][:, 0:NB], ALU.mult, [ka, PS(pb0 + 2)], [ka])
                tt(sb_[:, 0:NB], sb_[:, 0:NB], psum[pb0 + 3][:, 0:NB], ALU.mult, [kb, PS(pb0 + 3)], [kb])
                tt(mrg(f, NB), sa[:, 0:NB], sb_[:, 0:NB], ALU.add, [ka, kb], [("rb", f // 2)])
            for f in range(FT):
                pb = f % 4
                proj_tile(w_out_d[l, :, f * 128:(f + 1) * 128], 16, lambda k: mrg(k, NB), lambda k: ("rb", k // 2), pb, NB)
                tq, kq = T32()
                tt(v3(tq[:, 0:NB], blk), v3(psum[pb][:, 0:NB], blk), modb(mod_sb, l * 6 + 2, f, blk), ALU.mult,
                   [PS(pb), ("mod", l, 2)], [kq])
                tt(x_sb[:, f, 0:NB], x_sb[:, f, 0:NB], tq[:, 0:NB], ALU.add, [("x", f), kq], [("x", f)])

        def ffn_half(l, blk, w1_ap, w3_ap, w2_ap, comb_ap, comb_key):
            NB = blk["NB"]
            hk = lambda k: ("h", k)
            hf = lambda k: h_sb[:, k, 0:NB]
            for t_ in range(24):
                pb = 2 * (t_ % 2)
                proj_tile(w1_ap[:, t_ * 128:(t_ + 1) * 128], 16, hf, hk, pb, NB)
                proj_tile(w3_ap[:, t_ * 128:(t_ + 1) * 128], 16, hf, hk, pb + 1, NB)
                tq, kq = T32()
                act(tq[:, 0:NB], psum[pb][:, 0:NB], AF.Silu, [PS(pb)], [kq])
                tt(mix_sb[:, t_, 0:NB], tq[:, 0:NB], psum[pb + 1][:, 0:NB], ALU.mult, [kq, PS(pb + 1)], [("mix", t_)])
            for f in range(FT):
                pb = 4 + f % 3
                s1, k1 = load_slab(w2_ap[0:2048, f * 128:(f + 1) * 128], 16)
                s2, k2 = load_slab(w2_ap[2048:3072, f * 128:(f + 1) * 128], 8)
                for k in range(24):
                    sl, sk = (s1, k1) if k < 16 else (s2, k2)
                    mm(psum[pb][:, 0:NB], sl[:, k % 16, :], mix_sb[:, k, 0:NB], k == 0, k == 23, [sk, ("mix", k)], [PS(pb)])
                tq, kq = T32()
                tt(v3(tq[:, 0:NB], blk), v3(psum[pb][:, 0:NB], blk), modb(mod_sb, l * 6 + 5, f, blk), ALU.mult,
                   [PS(pb), ("mod", l, 5)], [kq])
                if comb_ap is not None:
                    tt(tq[:, 0:NB], tq[:, 0:NB], comb_ap[:, 0:NB], ALU.mult, [kq, comb_key], [kq])
                tt(x_sb[:, f, 0:NB], x_sb[:, f, 0:NB], tq[:, 0:NB], ALU.add, [("x", f), kq], [("x", f)])

        def moe_router(blk):
            NB = blk["NB"]
            nsub = max(1, NB // 128)
            tk = min(128, NB)
            lg_sb = rbuf[0][0:8, :]
            combT = rbuf[1][0:8, :]
            cp("act", lg_sb[:, 0:NB], psum[1][0:8, 0:NB], [PS(1)], [("rb", 0)])
            for s in range(nsub):
                P.op("pe", lambda e, o=psum[2][0:tk, s * 8:(s + 1) * 8], i=lg_sb[:, s * tk:(s + 1) * tk]:
                     e.transpose(o, i, ident[0:8, 0:8]), [("rb", 0), "ident"], [PS(2)])
            cp("act", lgT[0:tk, 0:nsub, :], psum[2][0:tk, 0:nsub * 8].rearrange("p (s e) -> p s e", e=8), [PS(2)], ["lgT"])
            for s in range(nsub):
                P.op("dve", lambda e, o=mx8[0:tk, s, :], i=lgT[0:tk, s, :]: e.max(o, i), ["lgT"], ["mx8"])
                ts(cmb[0:tk, s, :], lgT[0:tk, s, :], mx8[0:tk, s, 0:1], None, ALU.subtract, None, ["lgT", "mx8"], ["cmb"])
                act(cmb[0:tk, s, :], cmb[0:tk, s, :], AF.Exp, ["cmb"], ["cmb"])
                ts(cmbt[0:tk, s, :], lgT[0:tk, s, :], mx8[0:tk, s, 1:2], None, ALU.is_ge, None, ["lgT", "mx8"], ["cmbt"])
                tt(cmb[0:tk, s, :], cmb[0:tk, s, :], cmbt[0:tk, s, :], ALU.mult, ["cmb", "cmbt"], ["cmb"])
                P.op("dve", lambda e, o=den[0:tk, s, :], i=cmb[0:tk, s, :]:
                     e.reduce_sum(o, i, axis=mybir.AxisListType.X), ["cmb"], ["den"])
                P.op("dve", lambda e, o=den[0:tk, s, :]: e.reciprocal(o, o), ["den"], ["den"])
                ts(cmb[0:tk, s, :], cmb[0:tk, s, :], den[0:tk, s, 0:1], None, ALU.mult, None, ["cmb", "den"], ["cmb"])
                P.op("pe", lambda e, o=psum[3][0:8, s * tk:(s + 1) * tk], i=cmb[0:tk, s, :]:
                     e.transpose(o, i, ident[0:tk, 0:tk]), ["cmb", "ident"], [PS(3)])
            cp("act", combT[:, 0:NB], psum[3][0:8, 0:NB], [PS(3)], [("rb", 1)])

        for bi, blk in enumerate(blocks):
            NB = blk["NB"]
            c0 = blk["c0"]
            blk_tile[0] = 0
            dma("sp", x_sb[:, :, 0:NB], xT_d[:, :, c0:c0 + NB], (), [("x", f) for f in range(FT)] + ["s5m", "bbar"], key=("xin",))
            for l in range(DEPTH):
                rmsnorm_mod(l, 0, blk)
                stage(2)
                mixer(l, blk)
                stage(6)
                if l % 2 == 0:
                    rmsnorm_mod(l, 1, blk)
                    for hh_ in range(2):
                        ffn_half(l, blk, f_w1_d[0, :, hh_ * 3072:(hh_ + 1) * 3072], f_w3_d[0, :, hh_ * 3072:(hh_ + 1) * 3072],
                                 f_w2_d[0, hh_ * 3072:(hh_ + 1) * 3072, :], None, None)
                else:
                    rmsnorm_mod(l, 1, blk, want_router=True)
                    moe_router(blk)
                    for e_ in range(8):
                        cbuf = rbuf[2 + e_ % 2]
                        ck = ("rb", 2 + e_ % 2)
                        msk = rbuf[4 + e_ % 2][0:8, :]
                        mk_ = ("rb", 4 + e_ % 2)
                        ts(msk[:, 0:NB], rbuf[1][0:8, 0:NB], ident[0:8, e_:e_ + 1], None, ALU.mult, None,
                           [("rb", 1), "ident"], [mk_])
                        mm(psum[7][:, 0:NB], ones_f[0:8, :], msk[:, 0:NB], True, True, ["ones", mk_], [PS(7)])
                        cp("act", cbuf[:, 0:NB], psum[7][:, 0:NB], [PS(7)], [ck])
                        ffn_half(l, blk, m_w1_d[0, e_], m_w3_d[0, e_], m_w2_d[0, e_], cbuf, ck)
            pb = 0
            for ft in range(FT):
                tq, kq = T32()
                act(tq[:, 0:NB], x_sb[:, ft, 0:NB], AF.Square, [("x", ft)], [kq])
                mm(psum[pb][:, 0:NB], ones_f[:], tq[:, 0:NB], ft == 0, ft == FT - 1, ["ones", kq], [PS(pb)])
            rstd = rbuf[10]
            act(rstd[:, 0:NB], psum[pb][:, 0:NB], AF.Ln, [PS(pb)], [("rb", 10)], bias=1e-6, scale=1.0 / D)
            act(rstd[:, 0:NB], rstd[:, 0:NB], AF.Exp, [("rb", 10)], [("rb", 10)], scale=-0.5)
            for ft in range(FT):
                tt(x_sb[:, ft, 0:NB], x_sb[:, ft, 0:NB], rstd[:, 0:NB], ALU.mult, [("x", ft), ("rb", 10)], [("x", ft)])
                ts(x_sb[:, ft, 0:NB], x_sb[:, ft, 0:NB], small_sb[:, 0, 32 + ft:33 + ft], None, ALU.mult, None,
                   [("x", ft), "small"], [("x", ft)])
            dma("sp", yT_d[:, :, c0:c0 + NB], x_sb[:, :, 0:NB], [("x", f) for f in range(FT)], (), key=("out",))

    except _Stop:
        pass

    dma("sp", o_s5s_d.rearrange("l r p j q -> p l r j q"), s5out[:], [("s5in", j) for j in range(32)], (), key=("out",))
    dma("sp", o_s5p_d.rearrange("l r p j -> p l r j"), s5car[:, :, :, :, 0], [("s5car", j) for j in range(32)], (), key=("out",))
    dma("sp", o_rgs_d.rearrange("l p j q -> p l j q"), rgout[:], ["rgin"], (), key=("out",))
    dma("sp", o_rgp_d.rearrange("l p j -> p l j"), rgcar[:, :, :, 0], ["rgcar"], (), key=("out",))
    dma("sp", o_cvs_d.rearrange("l p j q k -> p l j q k"), cvout[:], ["cvin"], (), key=("out",))
    dma("sp", o_cvp_d.rearrange("l p j k -> p l j k"), cvtail[:, :, :, 0, :], ["cvtail"], (), key=("out",))

    P.emit(final_wait_keys=[("out",)])
    st.close()
    return nc


def _ft_layout(v):
    k = v.shape[-1] // 128
    return np.ascontiguousarray(v.reshape(k, 128).T)


def _prep_small(inp):
    out = np.zeros((DEPTH, 128, NS_SMALL), np.float32)
    for l in range(DEPTH):
        o = out[l]
        o[:, 0:16] = _ft_layout(inp["norm_mix"][l])
        o[:, 16:32] = _ft_layout(inp["norm_ffn"][l])
        o[:, 32:48] = _ft_layout(inp["norm_f"])
        o[:, 48:80] = _ft_layout(inp["b_gate"][l])
        o[:, 80:88] = _ft_layout(inp["s5_b_glu"][l])
        for k in range(4):
            o[:, 88 + k * 8:96 + k * 8] = _ft_layout(inp["rg_conv_w"][l, k])
        o[:, 120:128] = _ft_layout(inp["rg_conv_b"][l])
        o[:, 128:136] = _ft_layout(inp["rg_lam"][l])
        o[:, 136:144] = _ft_layout(inp["rg_b_a"][l].reshape(-1))
        o[:, 144:152] = _ft_layout(inp["rg_b_i"][l].reshape(-1))
        o[:, 152:160] = _ft_layout(inp["s5_d"][l].reshape(-1))
        o[:, 160:256] = _ft_layout(inp["b_ada"][l])
        sp = lambda a: np.ascontiguousarray(a.reshape(32, 2, 64).transpose(1, 2, 0).reshape(128, 32))
        o[:, 256:288] = sp(inp["s5_lam_re"][l])
        o[:, 288:320] = sp(inp["s5_lam_im"][l])
        o[:, 320:352] = sp(np.repeat(inp["s5_log_dt"][l][:, None], 64, axis=1))
    return out


def _prep_s5m(inp):
    out = np.zeros((DEPTH, 128, 4, 32, 32), np.float32)
    for l in range(DEPTH):
        for idx, name in enumerate(("s5_b_re", "s5_b_im")):
            b = inp[name][l].reshape(32, 2, 64, 16)
            for g2 in range(2):
                out[l, g2 * 64:(g2 + 1) * 64, idx, :, g2 * 16:(g2 + 1) * 16] = b[:, g2].transpose(1, 0, 2)
        for idx, name in enumerate(("s5_c_re", "s5_c_im")):
            c = inp[name][l].reshape(32, 2, 16, 64)
            for g2 in range(2):
                out[l, g2 * 64:(g2 + 1) * 64, 2 + idx, :, g2 * 16:(g2 + 1) * 16] = c[:, g2].transpose(2, 0, 1)
    return out


def _prep_rgw(inp):
    out = np.zeros((DEPTH, 128, 2, 8, 128), np.float32)
    for l in range(DEPTH):
        for a, name in enumerate(("rg_w_a", "rg_w_i")):
            w = inp[name][l]
            for j in range(8):
                for h2 in range(2):
                    out[l, h2 * 64:(h2 + 1) * 64, a, j, h2 * 64:(h2 + 1) * 64] = w[j * 2 + h2]
    return out


_NC_CACHE = {}


def kernel(**inp):
    inp = {k: np.asarray(v) for k, v in inp.items()}
    if "nc" not in _NC_CACHE:
        _NC_CACHE["nc"] = build_program()
    nc = _NC_CACHE["nc"]
    in_maps = _make_in_maps(inp)
    res = run_bass_kernel_spmd(nc, in_maps, core_ids=list(range(8)))
    return _assemble(res.results)


def _make_in_maps(inp):
    small = _prep_small(inp)
    s5m = _prep_s5m(inp)
    rgw = _prep_rgw(inp)
    router = np.ascontiguousarray(inp["moe_router"][0].reshape(FT, 128, 8).transpose(1, 0, 2))
    ident = np.eye(128, dtype=np.float32)
    pmask = np.zeros((128, 4), np.float32)
    for jj in range(4):
        pmask[32 * jj:32 * jj + 32, jj] = 1.0
    shared = dict(small=small, s5m=s5m, rgw=rgw, router=router, ident=ident, pmask=pmask)
    for k in ("w_ada", "w_in", "s5_w_glu", "w_gate", "w_br_s5", "w_br_rg", "w_out", "ffn_w1", "ffn_w3", "ffn_w2",
              "moe_w1", "moe_w3", "moe_w2"):
        shared[k] = np.ascontiguousarray(inp[k], dtype=np.float32)
    in_maps = []
    for c in range(8):
        b = c % 4
        qs = slice(c * NSAMP, (c + 1) * NSAMP)
        xtok = np.concatenate([inp["x_prompt"][b], inp["x_sample"][qs].reshape(NSAMP * TS, D)], axis=0)
        xT = np.ascontiguousarray(xtok.reshape(-1, FT, 128).transpose(2, 1, 0))
        cc = np.concatenate([inp["c_prompt"][b:b + 1], inp["c_sample"][qs]], axis=0)
        cT = np.ascontiguousarray(cc.reshape(-1, FT, 128).transpose(2, 1, 0))
        s5st = np.stack([inp["state_s5_re"][:, qs], inp["state_s5_im"][:, qs]], axis=1)
        s5st = s5st.reshape(DEPTH, 2, NSAMP, 32, 2, 64).transpose(0, 1, 4, 5, 3, 2).reshape(DEPTH, 2, 128, 32, NSAMP)
        rgst = inp["state_rglru"][:, qs].reshape(DEPTH, NSAMP, 8, 128).transpose(0, 3, 2, 1)
        cvst = inp["state_conv"][:, qs].reshape(DEPTH, NSAMP, 3, 8, 128).transpose(0, 4, 3, 1, 2)
        m = dict(shared)
        m.update(xT=xT, cT=cT, s5st=np.ascontiguousarray(s5st), rgst=np.ascontiguousarray(rgst),
                 cvst=np.ascontiguousarray(cvst))
        in_maps.append(m)
    return in_maps


def _assemble(R):
    B = 4
    y_prompt = np.zeros((B, SEQ, D), np.float32)
    y_sample = np.zeros((8 * NSAMP, TS, D), np.float32)
    p_s5_re = np.zeros((DEPTH, B, 64, 64), np.float32)
    p_s5_im = np.zeros_like(p_s5_re)
    p_rg = np.zeros((DEPTH, B, 1024), np.float32)
    p_conv = np.zeros((DEPTH, B, 3, 1024), np.float32)
    s_s5_re = np.zeros((DEPTH, 8 * NSAMP, 64, 64), np.float32)
    s_s5_im = np.zeros_like(s_s5_re)
    s_rg = np.zeros((DEPTH, 8 * NSAMP, 1024), np.float32)
    s_conv = np.zeros((DEPTH, 8 * NSAMP, 3, 1024), np.float32)
    for c in range(8):
        r = R[c]
        yT = np.asarray(r["yT"])
        ytok = yT.transpose(2, 1, 0).reshape(-1, D)
        qs = slice(c * NSAMP, (c + 1) * NSAMP)
        y_sample[qs] = ytok[SEQ:].reshape(NSAMP, TS, D)
        s5s = np.asarray(r["o_s5s"]).reshape(DEPTH, 2, 2, 64, 32, NSAMP)
        s5s = s5s.transpose(0, 1, 5, 4, 2, 3).reshape(DEPTH, 2, NSAMP, 64, 64)
        s_s5_re[:, qs] = s5s[:, 0]
        s_s5_im[:, qs] = s5s[:, 1]
        s_rg[:, qs] = np.asarray(r["o_rgs"]).transpose(0, 3, 2, 1).reshape(DEPTH, NSAMP, 1024)
        s_conv[:, qs] = np.asarray(r["o_cvs"]).transpose(0, 3, 4, 2, 1).reshape(DEPTH, NSAMP, 3, 1024)
        if c < B:
            y_prompt[c] = ytok[:SEQ]
            s5p = np.asarray(r["o_s5p"]).reshape(DEPTH, 2, 2, 64, 32)
            s5p = s5p.transpose(0, 1, 4, 2, 3).reshape(DEPTH, 2, 64, 64)
            p_s5_re[:, c] = s5p[:, 0]
            p_s5_im[:, c] = s5p[:, 1]
            p_rg[:, c] = np.asarray(r["o_rgp"]).transpose(0, 2, 1).reshape(DEPTH, 1024)
            p_conv[:, c] = np.asarray(r["o_cvp"]).transpose(0, 3, 2, 1).reshape(DEPTH, 3, 1024)
    return (y_prompt, y_sample, p_s5_re, p_s5_im, p_rg, p_conv, s_s5_re, s_s5_im, s_rg, s_conv)
```

```python
import contextlib
import math
import numpy as np
import concourse.bass as bass
import concourse.mybir as mybir
from concourse.bass_utils import run_bass_kernel_spmd

F32 = mybir.dt.float32
BF16 = mybir.dt.bfloat16
I32 = mybir.dt.int32
AF = mybir.ActivationFunctionType
ALU = mybir.AluOpType

D = 2048
FT = 16
SEQ = 2048
NSAMP = 16
TS = 4
DEPTH = 2
NS_SMALL = 352
TWO_PI = 2.0 * math.pi
GELU_K = 1.5957691216057308


class Prog:
    ENG = ("pe", "act", "dve", "pool", "sp")

    def __init__(self, nc):
        self.nc = nc
        self.ops = []
        self.last_writer = {}
        self.readers = {}
        self.dma_key_count = {}
        self.dma_key_waitall = set()

    def op(self, eng, fn, reads=(), writes=(), dma_key=None, wait_all=False):
        ps_r = [k for k in reads if isinstance(k, tuple) and k and k[0] == "ps"]
        if ps_r:
            reads = [k for k in reads if k not in ps_r]
            writes = list(writes) + [k for k in ps_r if k not in writes]
        idx = len(self.ops)
        deps = set()
        war = set()
        for b in reads:
            w = self.last_writer.get(b)
            if w is not None:
                deps.add(w)
        for b in writes:
            w = self.last_writer.get(b)
            if w is not None:
                deps.add(w)
            for r in self.readers.get(b, {}).values():
                war.add(r)
        o = dict(eng=eng, fn=fn, deps=deps, war=war, dma_key=dma_key, signal=False)
        if dma_key is not None:
            self.dma_key_count[dma_key] = self.dma_key_count.get(dma_key, 0) + 1
            o["dma_n"] = self.dma_key_count[dma_key]
            if wait_all:
                self.dma_key_waitall.add(dma_key)
        self.ops.append(o)
        rk = eng if dma_key is None else ("dma", idx)
        for b in reads:
            self.readers.setdefault(b, {})[rk] = idx
        for b in writes:
            self.last_writer[b] = idx
            self.readers[b] = {}
        return idx

    def emit(self, final_wait_keys=()):
        nc = self.nc
        ops = self.ops
        for o in ops:
            nd = set()
            for d in o["deps"] | o["war"]:
                p = ops[d]
                if p["dma_key"] is None and o["dma_key"] is None and p["eng"] == o["eng"]:
                    if o["eng"] == "pe":
                        continue
                nd.add(d)
            o["deps"] = nd
            for d in nd:
                ops[d]["signal"] = True
        cnt = {e: 0 for e in self.ENG}
        for o in ops:
            if o["dma_key"] is not None:
                k = o["dma_key"]
                n = self.dma_key_count[k] if k in self.dma_key_waitall else o["dma_n"]
                o["sig"] = (("dma", k), 16 * n)
            elif o["signal"]:
                cnt[o["eng"]] += 1
                o["sig"] = (("eng", o["eng"]), cnt[o["eng"]])
        per_eng = {e: [o for o in ops if o["eng"] == e] for e in self.ENG}
        sem_names = [("eng", e) for e in self.ENG] + [("dma", k) for k in self.dma_key_count]
        with contextlib.ExitStack() as st:
            sems = {}
            for i, sn in enumerate(sem_names):
                sems[sn] = st.enter_context(nc.semaphore("s%d" % i))
            block = st.enter_context(nc.Block())
            engobj = {"pe": "tensor", "act": "scalar", "dve": "vector", "pool": "gpsimd", "sp": "sync"}

            def run(e, eng):
                known = {}
                for o in per_eng[e]:
                    need = {}
                    for d in o["deps"]:
                        s, v = ops[d]["sig"]
                        if v > need.get(s, 0):
                            need[s] = v
                    for s, v in need.items():
                        if known.get(s, 0) < v:
                            eng.wait_ge(sems[s], v)
                            known[s] = v
                    ins = o["fn"](eng)
                    if o["dma_key"] is not None:
                        ins.then_inc(sems[("dma", o["dma_key"])], 16)
                    elif o["signal"]:
                        ins.then_inc(sems[("eng", e)], 1)
                if e == "sp":
                    for k in final_wait_keys:
                        v = 16 * self.dma_key_count[k]
                        if known.get(("dma", k), 0) < v:
                            eng.wait_ge(sems[("dma", k)], v)

            for e in self.ENG:
                def mk(e):
                    def f(eng):
                        run(e, eng)
                    return f
                getattr(block, engobj[e])(mk(e))


class _Stop(Exception):
    pass


def build_program(n_prompt_blocks=SEQ // 512, stop_stage=None):
    nc = bass.Bass("TRN2", target_bir_lowering=False)

    def stage(n):
        if stop_stage is not None and n == stop_stage:
            raise _Stop()

    P = Prog(nc)
    st = contextlib.ExitStack()

    def din(name, shape):
        return nc.dram_tensor(name, list(shape), F32, kind="ExternalInput").ap()

    def dout(name, shape):
        return nc.dram_tensor(name, list(shape), F32, kind="ExternalOutput").ap()

    def sb(name, shape, dt=F32):
        return st.enter_context(nc.sbuf_tensor(name, list(shape), dt))

    NTOK = SEQ + NSAMP * TS
    xT_d = din("xT", [128, FT, NTOK])
    cT_d = din("cT", [128, FT, 1 + NSAMP])
    small_d = din("small", [DEPTH, 128, NS_SMALL])
    s5m_d = din("s5m", [DEPTH, 128, 4, 32, 32])
    rgw_d = din("rgw", [DEPTH, 128, 2, 8, 128])
    router_d = din("router", [128, FT, 8])
    ident_d = din("ident", [128, 128])
    pmask_d = din("pmask", [128, 4])
    s5st_d = din("s5st", [DEPTH, 2, 128, 32, NSAMP])
    rgst_d = din("rgst", [DEPTH, 128, 8, NSAMP])
    cvst_d = din("cvst", [DEPTH, 128, 8, NSAMP, 3])
    w_ada_d = din("w_ada", [DEPTH, D, 6 * D])
    w_in_d = din("w_in", [DEPTH, D, 3072])
    w_glu_d = din("s5_w_glu", [DEPTH, 1024, 1024])
    w_gate_d = din("w_gate", [DEPTH, D, 2 * D])
    w_bs_d = din("w_br_s5", [DEPTH, 1024, D])
    w_br_d = din("w_br_rg", [DEPTH, 1024, D])
    w_out_d = din("w_out", [DEPTH, D, D])
    f_w1_d = din("ffn_w1", [1, D, 6144])
    f_w3_d = din("ffn_w3", [1, D, 6144])
    f_w2_d = din("ffn_w2", [1, 6144, D])
    m_w1_d = din("moe_w1", [1, 8, D, 3072])
    m_w3_d = din("moe_w3", [1, 8, D, 3072])
    m_w2_d = din("moe_w2", [1, 8, 3072, D])

    yT_d = dout("yT", [128, FT, NTOK])
    o_s5s_d = dout("o_s5s", [DEPTH, 2, 128, 32, NSAMP])
    o_s5p_d = dout("o_s5p", [DEPTH, 2, 128, 32])
    o_rgs_d = dout("o_rgs", [DEPTH, 128, 8, NSAMP])
    o_rgp_d = dout("o_rgp", [DEPTH, 128, 8])
    o_cvs_d = dout("o_cvs", [DEPTH, 128, 8, NSAMP, 3])
    o_cvp_d = dout("o_cvp", [DEPTH, 128, 8, 3])

    NBMAX = 512
    x_sb = sb("x_sb", [128, FT, NBMAX])
    h_sb = sb("h_sb", [128, FT, NBMAX], BF16)
    mix_sb = sb("mix_sb", [128, 24, NBMAX], BF16)
    NSLAB = 3
    slabs = [sb("slab%d" % i, [128, 16, 128], BF16) for i in range(NSLAB)]
    NTMP = 4
    tmps = [sb("tmp%d" % i, [128, NBMAX]) for i in range(NTMP)]
    NR = 11
    rb_all = sb("rb_all", [128, NR, NBMAX])
    rbuf = [rb_all[:, i, :] for i in range(NR)]
    rb_bf = rb_all.bitcast(BF16)

    def mrg(f, NB):
        return rb_bf[:, f // 2, (f % 2) * NBMAX:(f % 2) * NBMAX + NB]
    xrext = sb("xrext", [128, NBMAX + 64])
    bft = [sb("bft%d" % i, [128, NBMAX], BF16) for i in range(2)]
    um = [sb("um%d" % i, [128, NBMAX], BF16) for i in range(4)]
    tabb = [sb("tabb%d" % i, [128, 2, NBMAX]) for i in range(2)]
    ones512 = sb("ones512", [128, NBMAX])
    mask01 = sb("mask01", [128, NSAMP, TS])
    upw = sb("upw", [128, 32, 9, 3])
    tab_d = nc.dram_tensor("tabscr", [DEPTH * 32, 128, 2 * NBMAX], F32).ap()
    bft2 = [sb("bft2%d" % i, [128, NBMAX], BF16) for i in range(2)]
    hbfs = [bft, bft2]
    wcp = [sb("wcp%d" % i, [128, 128], BF16) for i in range(8)]
    small_sb = sb("small_sb", [128, DEPTH, NS_SMALL])
    mod_sb = sb("mod_sb", [128, DEPTH * 6, FT, 1 + NSAMP, 1])
    cT_sb = sb("cT_sb", [128, FT, 1 + NSAMP])
    cs_bf = sb("cs_bf", [128, FT, 1 + NSAMP], BF16)
    ident = sb("ident_sb", [128, 128])
    router_sb = sb("router_sb", [128, FT, 8])
    s5m_sb = x_sb[:, 0:8, :].rearrange("p a b -> p (a b)").rearrange("p (i j m) -> p i j m", i=4, j=32)
    bbar = x_sb[:, 8:12, :].rearrange("p a b -> p (a b)").rearrange("p (i j m) -> p i j m", i=2, j=32)
    wbT = sb("wbT", [128, DEPTH, 2, 8, 128], BF16)
    cb = sb("cb", [128, DEPTH, 2, 32, 32], BF16)
    NLV = 9
    pw = sb("pw", [128, DEPTH, 32, NLV, 3])
    sp_t = [sb("spt%d" % i, [128, 32]) for i in range(14)]
    pw_rho = sb("pw_rho", [128, DEPTH, 32])
    sp_i = sb("spi", [128, 32], I32)
    rgw_sb = sb("rgw_sb", [128, DEPTH, 2, 8, 128], BF16)
    rgc = sb("rgc", [128, DEPTH, 2, 8])
    s5car = sb("s5car", [128, DEPTH, 2, 32, 1])
    rgcar = sb("rgcar", [128, DEPTH, 8, 1])
    cvtail = sb("cvtail", [128, DEPTH, 8, 1, 3])
    s5in = sb("s5in", [128, DEPTH, 2, 32, NSAMP])
    s5out = s5in
    rgin = sb("rgin", [128, DEPTH, 8, NSAMP])
    rgout = rgin
    cvin = sb("cvin", [128, DEPTH, 8, NSAMP, 3])
    cvout = cvin
    car4 = sb("car4", [128, 4, NSAMP])
    lgT = sb("lgT", [128, 4, 8])
    mx8 = sb("mx8", [128, 4, 8])
    cmb = sb("cmb", [128, 4, 8])
    cmbt = sb("cmbt", [128, 4, 8])
    den = sb("den", [128, 4, 1])
    ones_f = sb("ones_f", [128, 128])
    pmask = sb("pmask_sb", [128, 4])

    psum = [st.enter_context(nc.psum_tensor("ps%d" % i, [128, 512], F32)) for i in range(8)]

    def mm(out, lhsT, rhs, start, stop, reads, writes):
        P.op("pe", lambda e, a=out, b=lhsT, c=rhs, s=start, t=stop: e.matmul(a, lhsT=b, rhs=c, start=s, stop=t),
             reads, writes)

    def act(out, in_, func, reads, writes, bias=None, scale=None):
        kw = {}
        if bias is not None:
            kw["bias"] = bias
        if scale is not None:
            kw["scale"] = scale
        P.op("act", lambda e, a=out, b=in_, f=func, k=kw: e.activation(a, b, f, **k), reads, writes)

    def tt(out, a, b, op, reads, writes, eng="dve"):
        P.op(eng, lambda e, o=out, x=a, y=b, p=op: e.tensor_tensor(out=o, in0=x, in1=y, op=p), reads, writes)

    def ts(out, a, s1, s2, op0, op1, reads, writes, eng="dve"):
        if s2 is None:
            P.op(eng, lambda e, o=out, x=a, u=s1, p=op0: e.tensor_scalar(o, x, u, None, p), reads, writes)
        else:
            P.op(eng, lambda e, o=out, x=a, u=s1, v=s2, p=op0, q=op1: e.tensor_scalar(o, x, u, v, p, q), reads, writes)

    def stt(out, in0, scalar, in1, op0, op1, reads, writes, eng="dve"):
        P.op(eng, lambda e, o=out, x=in0, s=scalar, y=in1, p=op0, q=op1:
             e.scalar_tensor_tensor(out=o, in0=x, scalar=s, in1=y, op0=p, op1=q), reads, writes)

    def cp(eng, out, in_, reads, writes):
        if eng == "act":
            P.op("act", lambda e, o=out, i=in_: e.copy(o, i), reads, writes)
        else:
            P.op(eng, lambda e, o=out, i=in_: e.tensor_copy(o, i), reads, writes)

    def memset(eng, ap, val, writes):
        P.op(eng, lambda e, a=ap, v=val: e.memset(a, v), (), writes)

    def dma(q, out, in_, reads, writes, key, wait_all=False):
        P.op(q, lambda e, o=out, i=in_: e.dma_start(out=o, in_=i), reads, writes, dma_key=key, wait_all=wait_all)

    slab_ctr = [0]
    NSCR = 1056
    SCR_PER = 448
    wscrs = [nc.dram_tensor("wscr%d" % i, [SCR_PER, 128, 2048], BF16).ap() for i in range(3)]
    scr_seen = set()
    blk_tile = [None]

    def load_slab(src_ap, K):
        s = slab_ctr[0] % NSLAB
        slab_ctr[0] += 1
        key = ("slab", s)
        dst = slabs[s][:, 0:K, :]
        if blk_tile[0] is None:
            dma("pool", dst, src_ap.rearrange("(k p) m -> p k m", p=128), (), [key], key=("slabq", s))
            return slabs[s], key
        tid = blk_tile[0]
        blk_tile[0] += 1
        assert tid < NSCR
        scr_v = wscrs[tid // SCR_PER][tid % SCR_PER, :, 0:K * 128].rearrange("p (k m) -> p k m", m=128)
        if tid not in scr_seen:
            scr_seen.add(tid)
            dma("pool", dst, src_ap.rearrange("(k p) m -> p k m", p=128), (), [key], key=("slabq", s))
            dma("sp", scr_v, dst, [key], [("scr", tid)], key=("wbq", s))
        else:
            dma("sp", dst, scr_v, [("scr", tid)], [key], key=("slabh", s))
        return slabs[s], key

    tmp_ctr = [0]

    def T32():
        i = tmp_ctr[0] % NTMP
        tmp_ctr[0] += 1
        return tmps[i], ("tmp", i)

    PS = lambda i: ("ps", i)

    C = "const"
    dma("sp", small_sb[:], small_d.rearrange("l p n -> p l n"), (), ["small"], key=C, wait_all=True)
    dma("sp", cT_sb[:], cT_d, (), ["cT"], key=C, wait_all=True)
    dma("sp", ident[:], ident_d, (), ["ident"], key=C, wait_all=True)
    dma("sp", pmask[:], pmask_d, (), ["pmask"], key=C, wait_all=True)
    dma("sp", router_sb[:], router_d, (), ["router"], key=C, wait_all=True)
    dma("sp", s5in[:], s5st_d.rearrange("l r p j q -> p l r j q"), (), [("s5in", j) for j in range(32)], key=C, wait_all=True)
    dma("sp", rgin[:], rgst_d.rearrange("l p j q -> p l j q"), (), ["rgin"], key=C, wait_all=True)
    dma("sp", cvin[:], cvst_d.rearrange("l p j q k -> p l j q k"), (), ["cvin"], key=C, wait_all=True)
    dma("pool", rgw_sb[:], rgw_d.rearrange("l p a j m -> p l a j m"), (), ["rgw"], key=("rgwq",))
    memset("dve", ones_f[:], 1.0, ["ones"])
    memset("dve", ones512[:], 1.0, ["ones512"])
    memset("dve", mask01[:], 1.0, ["mask01"])
    memset("dve", mask01[:, :, 0:1], 0.0, ["mask01"])
    for i in range(8):
        memset("dve", wcp[i][:], 0.0, [("wcp", i)])
    memset("dve", s5car[:], 0.0, [("s5car", j) for j in range(32)])
    memset("dve", rgcar[:], 0.0, ["rgcar"])
    memset("dve", cvtail[:], 0.0, ["cvtail"])

    def smallv(l, c0, n):
        return small_sb[:, l, c0:c0 + n]

    try:
        act(cs_bf[:], cT_sb[:], AF.Silu, ["cT"], ["cs"])
        NSQ = 1 + NSAMP
        for l in range(DEPTH):
            for kind in range(6):
                pb = (l * 6 + kind) % 4
                for ft in range(FT):
                    col0 = (kind * FT + ft) * 128
                    slab, skey = load_slab(w_ada_d[l, :, col0:col0 + 128], 16)
                    for k in range(16):
                        mm(psum[pb][:, ft * NSQ:(ft + 1) * NSQ], slab[:, k, :], cs_bf[:, k, :], k == 0, k == 15,
                           [skey, "cs"], [PS(pb)])
                bias_b = small_sb[:, l, 160 + kind * FT:160 + (kind + 1) * FT].rearrange("p (f o) -> p f o", o=1) \
                    .to_broadcast([128, FT, NSQ])
                tt(mod_sb[:, l * 6 + kind, :, :, 0], psum[pb][:, 0:FT * NSQ].rearrange("p (f s) -> p f s", s=NSQ),
                   bias_b, ALU.add, [PS(pb), "small"], [("mod", l, kind)])
            for which, (kind, ncol) in enumerate(((1, 0), (4, 16))):
                tmpv = mod_sb[:, l * 6 + kind, :, :, 0]
                ts(tmpv, tmpv, 1.0, None, ALU.add, None, [("mod", l, kind)], [("mod", l, kind)])
                nb = small_sb[:, l, ncol:ncol + FT].rearrange("p (f o) -> p f o", o=1).to_broadcast([128, FT, NSQ])
                tt(tmpv, tmpv, nb, ALU.mult, [("mod", l, kind), "small"], [("mod", l, kind)])

        stage(0)
        for l in range(DEPTH):
            lamre = smallv(l, 256, 32)
            lamim = smallv(l, 288, 32)
            logdt = smallv(l, 320, 32)
            t = sp_t
            K = lambda i: ("spt", i)
            S = ["small"]
            act(t[0][:], logdt, AF.Exp, S, [K(0)])
            tt(t[1][:], lamre, t[0][:], ALU.mult, S + [K(0)], [K(1)])
            act(t[1][:], t[1][:], AF.Exp, [K(1)], [K(1)])
            tt(t[2][:], lamim, t[0][:], ALU.mult, S + [K(0)], [K(2)])
            ts(t[2][:], t[2][:], 1.0 / TWO_PI, None, ALU.mult, None, [K(2)], [K(2)])

            def sin_of(dst, kdst, shift):
                ts(t[3][:], t[2][:], shift, None, ALU.add, None, [K(2)], [K(3)])
                cp("dve", sp_i[:], t[3][:], [K(3)], ["spi"])
                cp("dve", t[4][:], sp_i[:], ["spi"], [K(4)])
                tt(t[3][:], t[3][:], t[4][:], ALU.subtract, [K(3), K(4)], [K(3)])
                ts(t[4][:], t[3][:], 0.5, None, ALU.is_gt, None, [K(3)], [K(4)])
                tt(t[3][:], t[3][:], t[4][:], ALU.subtract, [K(3), K(4)], [K(3)])
                ts(t[4][:], t[3][:], -0.5, None, ALU.is_lt, None, [K(3)], [K(4)])
                tt(t[3][:], t[3][:], t[4][:], ALU.add, [K(3), K(4)], [K(3)])
                act(dst, t[3][:], AF.Sin, [K(3)], [kdst], scale=TWO_PI)

            cp("dve", pw_rho[:, l, :], t[1][:], [K(1)], [("rho", l)])
            sin_of(t[5][:], K(5), 0.0)
            sin_of(t[6][:], K(6), 0.25)
            lre = pw[:, l, :, 0, 0]
            lim = pw[:, l, :, 0, 1]
            lnim = pw[:, l, :, 0, 2]
            PWK = ("pw", l)
            tt(lre, t[1][:], t[6][:], ALU.mult, [K(1), K(6)], [PWK])
            tt(lim, t[1][:], t[5][:], ALU.mult, [K(1), K(5)], [PWK])
            ts(lnim, lim, -1.0, None, ALU.mult, None, [PWK], [PWK])
            for lv in range(1, NLV):
                a_re = pw[:, l, :, lv - 1, 0]
                a_im = pw[:, l, :, lv - 1, 1]
                tt(t[7][:], a_re, a_re, ALU.mult, [PWK], [K(7)])
                tt(t[8][:], a_im, a_im, ALU.mult, [PWK], [K(8)])
                tt(pw[:, l, :, lv, 0], t[7][:], t[8][:], ALU.subtract, [K(7), K(8)], [PWK])
                tt(t[7][:], a_re, a_im, ALU.mult, [PWK], [K(7)])
                ts(pw[:, l, :, lv, 1], t[7][:], 2.0, None, ALU.mult, None, [K(7)], [PWK])
                ts(pw[:, l, :, lv, 2], t[7][:], -2.0, None, ALU.mult, None, [K(7)], [PWK])
            UK = "upw"
            cp("dve", upw[:, :, 0, 0], t[6][:], [K(6)], [UK])
            cp("dve", upw[:, :, 0, 1], t[5][:], [K(5)], [UK])
            ts(upw[:, :, 0, 2], t[5][:], -1.0, None, ALU.mult, None, [K(5)], [UK])
            for m_ in range(1, 9):
                a_re = upw[:, :, m_ - 1, 0]
                a_im = upw[:, :, m_ - 1, 1]
                tt(t[12][:], a_re, a_re, ALU.mult, [UK], [K(12)])
                tt(t[13][:], a_im, a_im, ALU.mult, [UK], [K(13)])
                tt(upw[:, :, m_, 0], t[12][:], t[13][:], ALU.subtract, [K(12), K(13)], [UK])
                tt(t[12][:], a_re, a_im, ALU.mult, [UK], [K(12)])
                ts(upw[:, :, m_, 1], t[12][:], 2.0, None, ALU.mult, None, [K(12)], [UK])
                ts(upw[:, :, m_, 2], t[12][:], -2.0, None, ALU.mult, None, [K(12)], [UK])
            for j in range(32):
                tb = x_sb[:, 12 + 2 * (j % 2):14 + 2 * (j % 2), :]
                TK = ("tabst", j % 2)
                cp("dve", tb[:, 0, 0:1], upw[:, j, 0, 0:1], [UK], [TK])
                cp("dve", tb[:, 1, 0:1], upw[:, j, 0, 1:2], [UK], [TK])
                n_ = 1
                m_ = 0
                while n_ < NBMAX:
                    ar = upw[:, j, m_, 0:1]
                    ai = upw[:, j, m_, 1:2]
                    nai = upw[:, j, m_, 2:3]
                    ts(tb[:, 0, n_:2 * n_], tb[:, 0, 0:n_], ar, None, ALU.mult, None, [TK, UK], [TK])
                    stt(tb[:, 0, n_:2 * n_], tb[:, 1, 0:n_], nai, tb[:, 0, n_:2 * n_], ALU.mult, ALU.add, [TK, UK], [TK])
                    ts(tb[:, 1, n_:2 * n_], tb[:, 1, 0:n_], ar, None, ALU.mult, None, [TK, UK], [TK])
                    stt(tb[:, 1, n_:2 * n_], tb[:, 0, 0:n_], ai, tb[:, 1, n_:2 * n_], ALU.mult, ALU.add, [TK, UK], [TK])
                    n_ *= 2
                    m_ += 1
                dma("sp", tab_d[l * 32 + j].rearrange("p (c t) -> p c t", c=2), tb, [TK], [("tabd", l, j)], key=("tabw", j % 2))
            ts(t[7][:], lre, -1.0, None, ALU.add, None, [PWK], [K(7)])
            tt(t[8][:], lamre, lamre, ALU.mult, S, [K(8)])
            tt(t[9][:], lamim, lamim, ALU.mult, S, [K(9)])
            tt(t[8][:], t[8][:], t[9][:], ALU.add, [K(8), K(9)], [K(8)])
            P.op("dve", lambda e, o=t[8][:]: e.reciprocal(o, o), [K(8)], [K(8)])
            tt(t[9][:], t[7][:], lamre, ALU.mult, [K(7)] + S, [K(9)])
            tt(t[10][:], lim, lamim, ALU.mult, [PWK] + S, [K(10)])
            tt(t[9][:], t[9][:], t[10][:], ALU.add, [K(9), K(10)], [K(9)])
            tt(t[9][:], t[9][:], t[8][:], ALU.mult, [K(9), K(8)], [K(9)])
            tt(t[10][:], lim, lamre, ALU.mult, [PWK] + S, [K(10)])
            tt(t[11][:], t[7][:], lamim, ALU.mult, [K(7)] + S, [K(11)])
            tt(t[10][:], t[10][:], t[11][:], ALU.subtract, [K(10), K(11)], [K(10)])
            tt(t[10][:], t[10][:], t[8][:], ALU.mult, [K(10), K(8)], [K(10)])
            dma("sp", s5m_sb, s5m_d[l], (), ["s5m"], key=("s5m",))
            bre = s5m_sb[:, 0]
            bim = s5m_sb[:, 1]
            for hlf in range(2):
                js = slice(hlf * 16, hlf * 16 + 16)
                s0 = rbuf[0][:, :].rearrange("p (j m) -> p j m", m=32)
                s1 = rbuf[1][:, :].rearrange("p (j m) -> p j m", m=32)
                cre_h = t[9][:, js].rearrange("p (j o) -> p j o", o=1).to_broadcast([128, 16, 32])
                cim_h = t[10][:, js].rearrange("p (j o) -> p j o", o=1).to_broadcast([128, 16, 32])
                tt(s0, bre[:, js, :], cre_h, ALU.mult, ["s5m", K(9)], [("rb", 0)])
                tt(s1, bim[:, js, :], cim_h, ALU.mult, ["s5m", K(10)], [("rb", 1)])
                tt(bbar[:, 0, js, :], s0, s1, ALU.subtract, [("rb", 0), ("rb", 1)], ["bbar"])
                tt(s0, bim[:, js, :], cre_h, ALU.mult, ["s5m", K(9)], [("rb", 0)])
                tt(s1, bre[:, js, :], cim_h, ALU.mult, ["s5m", K(10)], [("rb", 1)])
                tt(bbar[:, 1, js, :], s0, s1, ALU.add, [("rb", 0), ("rb", 1)], ["bbar"])
            for ri in range(2):
                for ct in range(8):
                    pb = 4 + (ri * 8 + ct) % 4
                    src = bbar[:, ri, ct * 4:(ct + 1) * 4, :].rearrange("p j m -> p (j m)")
                    P.op("pe", lambda e, o=psum[pb][:, 0:128], i=src: e.transpose(o, i, ident[:]),
                         ["bbar", "ident"], [PS(pb)])
                    cp("act", wbT[:, l, ri, ct, :], psum[pb][:, 0:128], [PS(pb)], [("wbT", l)])
            cp("act", cb[:, l, 0], s5m_sb[:, 2], ["s5m"], [("cb", l)])
            ts(cb[:, l, 1], s5m_sb[:, 3], -1.0, None, ALU.mult, None, ["s5m"], [("cb", l)])
            rl = smallv(l, 128, 8)
            act(rgc[:, l, 0, :], rl, AF.Exp, S, [("rgc", l)], scale=-1.0)
            act(rgc[:, l, 0, :], rgc[:, l, 0, :], AF.Ln, [("rgc", l)], [("rgc", l)], bias=1.0)
            ts(rgc[:, l, 1, :], rgc[:, l, 0, :], -16.0, None, ALU.mult, None, [("rgc", l)], [("rgc", l)])
            ts(rgc[:, l, 0, :], rgc[:, l, 0, :], -8.0, None, ALU.mult, None, [("rgc", l)], [("rgc", l)])

        stage(1)
        blocks = []
        for b in range(n_prompt_blocks):
            blocks.append(dict(c0=b * 512, NB=512, nseq=1, T=512, samp=False, last=(b == SEQ // 512 - 1)))
        blocks.append(dict(c0=SEQ, NB=NSAMP * TS, nseq=NSAMP, T=TS, samp=True, last=True))

        def v3(ap, blk):
            return ap.rearrange("p (q t) -> p q t", t=blk["T"])

        def modb(idx_tensor, idx, ft, blk):
            s0, s1 = (1, 1 + NSAMP) if blk["samp"] else (0, 1)
            return idx_tensor[:, idx, ft, s0:s1, :].to_broadcast([128, blk["nseq"], blk["T"]])

        def gelu_from(src32, skey, out_bf, okeys):
            t1, k1 = T32()
            act(t1[:, 0:NBc[0]], src32, AF.Square, [skey], [k1])
            ts(t1[:, 0:NBc[0]], t1[:, 0:NBc[0]], 0.044715, 1.0, ALU.mult, ALU.add, [k1], [k1])
            tt(t1[:, 0:NBc[0]], t1[:, 0:NBc[0]], src32, ALU.mult, [k1, skey], [k1])
            act(t1[:, 0:NBc[0]], t1[:, 0:NBc[0]], AF.Sigmoid, [k1], [k1], scale=GELU_K)
            tt(out_bf, src32, t1[:, 0:NBc[0]], ALU.mult, [skey, k1], okeys)

        NBc = [512]

        def rmsnorm_mod(l, which, blk, want_router=False):
            NB = blk["NB"]
            shk = 0 if which == 0 else 3
            pb = 0
            for ft in range(FT):
                tq, kq = T32()
                act(tq[:, 0:NB], x_sb[:, ft, 0:NB], AF.Square, [("x", ft)], [kq])
                mm(psum[pb][:, 0:NB], ones_f[:], tq[:, 0:NB], ft == 0, ft == FT - 1, ["ones", kq], [PS(pb)])
            rstd = rbuf[10]
            act(rstd[:, 0:NB], psum[pb][:, 0:NB], AF.Ln, [PS(pb)], [("rb", 10)], bias=1e-6, scale=1.0 / D)
            act(rstd[:, 0:NB], rstd[:, 0:NB], AF.Exp, [("rb", 10)], [("rb", 10)], scale=-0.5)
            for ft in range(FT):
                tq, kq = T32()
                tt(tq[:, 0:NB], x_sb[:, ft, 0:NB], rstd[:, 0:NB], ALU.mult, [("x", ft), ("rb", 10)], [kq])
                tt(v3(tq[:, 0:NB], blk), v3(tq[:, 0:NB], blk), modb(mod_sb, l * 6 + (1 if which == 0 else 4), ft, blk), ALU.mult,
                   [kq, ("mod", l, 1 if which == 0 else 4)], [kq])
                if want_router:
                    tt(v3(tq[:, 0:NB], blk), v3(tq[:, 0:NB], blk), modb(mod_sb, l * 6 + shk, ft, blk), ALU.add,
                       [kq, ("mod", l, shk)], [kq])
                    mm(psum[1][0:8, 0:NB], router_sb[:, ft, :], tq[:, 0:NB], ft == 0, ft == FT - 1,
                       ["router", kq], [PS(1)])
                    cp("act", h_sb[:, ft, 0:NB], tq[:, 0:NB], [kq], [("h", ft)])
                else:
                    tt(v3(h_sb[:, ft, 0:NB], blk), v3(tq[:, 0:NB], blk), modb(mod_sb, l * 6 + shk, ft, blk), ALU.add,
                       [kq, ("mod", l, shk)], [("h", ft)])

        def proj_tile(w_ap, kchunks, rhs_fn, rhs_keys, pb, NB):
            slab, skey = load_slab(w_ap, kchunks)
            for k in range(kchunks):
                mm(psum[pb][:, 0:NB], slab[:, k, :], rhs_fn(k), k == 0, k == kchunks - 1,
                   [skey] + [rhs_keys(k)], [PS(pb)])

        H_ALL = [("h", f) for f in range(FT)]

        def mixer(l, blk):
            NB, nseq, T = blk["NB"], blk["nseq"], blk["T"]
            NBc[0] = NB
            samp = blk["samp"]
            hk = lambda k: ("h", k)
            hf = lambda k: h_sb[:, k, 0:NB]
            for ct in range(8):
                proj_tile(w_in_d[l, :, ct * 128:(ct + 1) * 128], 16, hf, hk, 0, NB)
                stage(27)
                u32 = rbuf[0]
                cp("act", u32[:, 0:NB], psum[0][:, 0:NB], [PS(0)], [("rb", 0)])
                stage(28)
                for jj in range(4):
                    ts(um[jj][:, 0:NB], psum[0][:, 0:NB], pmask[:, jj:jj + 1], None, ALU.mult, None,
                       [PS(0), "pmask"], [("um", jj)])
                stage(21)
                for jj in range(4):
                    for ri in range(2):
                        cp("act", wcp[jj * 2 + ri][:, 32 * jj:32 * jj + 32], cb[:, l, ri, ct * 4 + jj, :],
                           [("cb", l)], [("wcp", jj * 2 + ri)])
                stage(22)
                PWK = ("pw", l)

                def s5_front(jj):
                    par = jj % 2
                    j = ct * 4 + jj
                    pbs = (1, 2) if par == 0 else (4, 5)
                    dma("sp", tabb[par][:, :, :], tab_d[l * 32 + j].rearrange("p (c t) -> p c t", c=2),
                        [("tabd", l, j)], [("tabb", par)], key=("tabl", par))
                    for ri in range(2):
                        mm(psum[pbs[ri]][:, 0:NB], wbT[:, l, ri, ct, :], um[jj][:, 0:NB], True, True,
                           [("wbT", l), ("um", jj)], [PS(pbs[ri])])

                def s5_scan(jj):
                    par = jj % 2
                    j = ct * 4 + jj
                    pbs = (1, 2) if par == 0 else (4, 5)
                    TBK = ("tabb", par)
                    R = lambda i: rbuf[i][:, 0:NB]
                    RK = lambda i: ("rb", i)
                    if samp:
                        cv = tabb[par][:, 0:1, 0:T].to_broadcast([128, nseq, T])
                        sv = tabb[par][:, 1:2, 0:T].to_broadcast([128, nseq, T])
                    else:
                        cv = tabb[par][:, 0:1, 0:T]
                        sv = tabb[par][:, 1:2, 0:T]
                    bre = v3(psum[pbs[0]][:, 0:NB], blk)
                    bim = v3(psum[pbs[1]][:, 0:NB], blk)
                    rho = pw_rho[:, l, j:j + 1]
                    tt(v3(R(2), blk), cv, bre, ALU.mult, [TBK, PS(pbs[0])], [RK(2)])
                    tt(v3(R(3), blk), sv, bim, ALU.mult, [TBK, PS(pbs[1])], [RK(3)])
                    tt(R(4), R(2), R(3), ALU.add, [RK(2), RK(3)], [RK(4)])
                    tt(v3(R(2), blk), cv, bim, ALU.mult, [TBK, PS(pbs[1])], [RK(2)])
                    tt(v3(R(3), blk), sv, bre, ALU.mult, [TBK, PS(pbs[0])], [RK(3)])
                    tt(R(5), R(2), R(3), ALU.subtract, [RK(2), RK(3)], [RK(5)])
                    if samp:
                        ts(v3(R(8), blk), mask01[:], rho, None, ALU.mult, None, ["mask01", ("rho", l)], [RK(8)])
                        g4 = v3(R(4), blk)
                        g5 = v3(R(5), blk)
                        stt(g4[:, :, 0], s5in[:, l, 0, j, :], rho, g4[:, :, 0], ALU.mult, ALU.add,
                            [("s5in", j), ("rho", l), RK(4)], [RK(4)])
                        stt(g5[:, :, 0], s5in[:, l, 1, j, :], rho, g5[:, :, 0], ALU.mult, ALU.add,
                            [("s5in", j), ("rho", l), RK(5)], [RK(5)])
                        ini_re = ini_im = 0.0
                        ikeys = []
                    else:
                        ts(R(8), ones512[:, 0:NB], rho, None, ALU.mult, None, ["ones512", ("rho", l)], [RK(8)])
                        ini_re = s5car[:, l, 0, j, :]
                        ini_im = s5car[:, l, 1, j, :]
                        ikeys = [("s5car", j)]
                    P.op("dve", lambda e, o=R(6), a=R(8), b=R(4), i=ini_re:
                         e.tensor_tensor_scan(out=o, data0=a, data1=b, initial=i, op0=ALU.mult, op1=ALU.add),
                         [RK(8), RK(4)] + ikeys, [RK(6)])
                    P.op("dve", lambda e, o=R(7), a=R(8), b=R(5), i=ini_im:
                         e.tensor_tensor_scan(out=o, data0=a, data1=b, initial=i, op0=ALU.mult, op1=ALU.add),
                         [RK(8), RK(5)] + ikeys, [RK(7)])
                    g6 = v3(R(6), blk)
                    g7 = v3(R(7), blk)
                    tt(v3(R(2), blk), cv, g6, ALU.mult, [TBK, RK(6)], [RK(2)])
                    tt(v3(R(3), blk), sv, g7, ALU.mult, [TBK, RK(7)], [RK(3)])
                    tt(R(4), R(2), R(3), ALU.subtract, [RK(2), RK(3)], [RK(4)])
                    tt(v3(R(2), blk), cv, g7, ALU.mult, [TBK, RK(7)], [RK(2)])
                    tt(v3(R(3), blk), sv, g6, ALU.mult, [TBK, RK(6)], [RK(3)])
                    tt(R(5), R(2), R(3), ALU.add, [RK(2), RK(3)], [RK(5)])
                    return 0

                def s5_back(jj, cur):
                    par = jj % 2
                    j = ct * 4 + jj
                    R = lambda i: rbuf[i][:, 0:NB]
                    RK = lambda i: ("rb", i)
                    hb = hbfs[par]
                    hbk = [("hbf", par, 0), ("hbf", par, 1)]
                    cp("act", hb[0][:, 0:NB], R(4), [RK(4)], [hbk[0]])
                    cp("act", hb[1][:, 0:NB], R(5), [RK(5)], [hbk[1]])
                    h4 = v3(R(4), blk)
                    h5 = v3(R(5), blk)
                    if samp:
                        cp("act", s5out[:, l, 0, j, :], h4[:, :, T - 1], [RK(4)], [("s5in", j)])
                        cp("act", s5out[:, l, 1, j, :], h5[:, :, T - 1], [RK(5)], [("s5in", j)])
                    else:
                        cp("act", s5car[:, l, 0, j, :], h4[:, :, T - 1], [RK(4)], [("s5car", j)])
                        cp("act", s5car[:, l, 1, j, :], h5[:, :, T - 1], [RK(5)], [("s5car", j)])
                    for ri in range(2):
                        mm(psum[3][:, 0:NB], wcp[jj * 2 + ri][:], hb[ri][:, 0:NB], jj == 0 and ri == 0,
                           jj == 3 and ri == 1, [("wcp", jj * 2 + ri), hbk[ri]], [PS(3)])

                for jj in range(4):
                    s5_front(jj)
                    s5_scan(jj)
                    s5_back(jj, 0)
                stage(26)
                ypre = rbuf[1]
                stt(ypre[:, 0:NB], u32[:, 0:NB], small_sb[:, l, 152 + ct:153 + ct], psum[3][:, 0:NB], ALU.mult, ALU.add,
                    [("rb", 0), "small", PS(3)], [("rb", 1)])
                gelu_from(ypre[:, 0:NB], ("rb", 1), mix_sb[:, ct, 0:NB], [("mix", ct)])
            stage(3)
            for ct in range(8):
                pb = 1 + ct % 2
                proj_tile(w_glu_d[l, :, ct * 128:(ct + 1) * 128], 8, lambda k: mix_sb[:, k, 0:NB], lambda k: ("mix", k), pb, NB)
                tg, kg = T32()
                act(tg[:, 0:NB], psum[pb][:, 0:NB], AF.Sigmoid, [PS(pb), "small"], [kg], bias=small_sb[:, l, 80 + ct:81 + ct])
                tt(mix_sb[:, 8 + ct, 0:NB], mix_sb[:, ct, 0:NB], tg[:, 0:NB], ALU.mult, [("mix", ct), kg], [("mix", 8 + ct)])

            stage(4)
            HX = 3
            for j in range(8):
                proj_tile(w_in_d[l, :, 1024 + j * 128:1024 + (j + 1) * 128], 16, hf, hk, 4, NB)
                proj_tile(w_in_d[l, :, 2048 + j * 128:2048 + (j + 1) * 128], 16, hf, hk, 5, NB)
                xe = xrext[:, 0:nseq * (T + HX)].rearrange("p (q t) -> p q t", t=T + HX)
                XK = "xrext"
                cp("act", xe[:, :, HX:HX + T], v3(psum[4][:, 0:NB], blk), [PS(4)], [XK])
                if samp:
                    cp("dve", xe[:, :, 0:HX], cvin[:, l, j, :, :], ["cvin", XK], [XK])
                else:
                    cp("dve", xe[:, :, 0:HX], cvtail[:, l, j, :, :], ["cvtail", XK], [XK])
                gy32 = rbuf[2]
                cp("act", gy32[:, 0:NB], psum[5][:, 0:NB], [PS(5)], [("rb", 2)])
                gelu_from(gy32[:, 0:NB], ("rb", 2), bft[0][:, 0:NB], [("bft", 0)])
                xc = rbuf[3]
                xcv = v3(xc[:, 0:NB], blk)
                cw = lambda k: small_sb[:, l, 88 + k * 8 + j:89 + k * 8 + j]
                ts(xcv, xe[:, :, 0:T], cw(0), small_sb[:, l, 120 + j:121 + j], ALU.mult, ALU.add, [XK, "small"], [("rb", 3)])
                for k in range(1, 4):
                    stt(xcv, xe[:, :, k:k + T], cw(k), xcv, ALU.mult, ALU.add, [XK, "small", ("rb", 3)], [("rb", 3)])
                if samp:
                    cp("dve", cvout[:, l, j, :, :], xe[:, :, T:T + HX], [XK], ["cvin"])
                else:
                    cp("dve", cvtail[:, l, j, :, :], xe[:, :, T:T + HX], [XK], ["cvtail"])
                cp("act", bft[1][:, 0:NB], xc[:, 0:NB], [("rb", 3)], [("bft", 1)])
                mm(psum[6][:, 0:NB], rgw_sb[:, l, 0, j, :], bft[1][:, 0:NB], True, True, ["rgw", ("bft", 1)], [PS(6)])
                mm(psum[7][:, 0:NB], rgw_sb[:, l, 1, j, :], bft[1][:, 0:NB], True, True, ["rgw", ("bft", 1)], [PS(7)])
                r32, i32, a32, e2 = rbuf[4], rbuf[5], rbuf[6], rbuf[7]
                act(r32[:, 0:NB], psum[6][:, 0:NB], AF.Sigmoid, [PS(6), "small"], [("rb", 4)], bias=small_sb[:, l, 136 + j:137 + j])
                act(i32[:, 0:NB], psum[7][:, 0:NB], AF.Sigmoid, [PS(7), "small"], [("rb", 5)], bias=small_sb[:, l, 144 + j:145 + j])
                act(a32[:, 0:NB], r32[:, 0:NB], AF.Exp, [("rb", 4), ("rgc", l)], [("rb", 6)], scale=rgc[:, l, 0, j:j + 1])
                act(e2[:, 0:NB], r32[:, 0:NB], AF.Exp, [("rb", 4), ("rgc", l)], [("rb", 7)], scale=rgc[:, l, 1, j:j + 1])
                act(e2[:, 0:NB], e2[:, 0:NB], AF.Ln, [("rb", 7)], [("rb", 7)], bias=1.0, scale=-1.0)
                act(e2[:, 0:NB], e2[:, 0:NB], AF.Exp, [("rb", 7)], [("rb", 7)], scale=0.5)
                bx = rbuf[8]
                tt(bx[:, 0:NB], i32[:, 0:NB], xc[:, 0:NB], ALU.mult, [("rb", 5), ("rb", 3)], [("rb", 8)])
                tt(bx[:, 0:NB], bx[:, 0:NB], e2[:, 0:NB], ALU.mult, [("rb", 8), ("rb", 7)], [("rb", 8)])
                a3 = v3(a32[:, 0:NB], blk)
                b3 = v3(bx[:, 0:NB], blk)
                if samp:
                    h0 = rgin[:, l, j, :]
                    h0k = ["rgin"]
                else:
                    h0 = rgcar[:, l, j, :]
                    h0k = ["rgcar"]
                tcar = car4[:, 0, 0:nseq]
                tt(tcar, a3[:, :, 0], h0, ALU.mult, [("rb", 6)] + h0k, ["car4"])
                tt(b3[:, :, 0], b3[:, :, 0], tcar, ALU.add, [("rb", 8), "car4"], [("rb", 8)])
                memset("dve", a3[:, :, 0], 0.0, [("rb", 6)])
                hh = rbuf[9]
                P.op("dve", lambda e, o=hh[:, 0:NB], a=a32[:, 0:NB], b=bx[:, 0:NB]:
                     e.tensor_tensor_scan(out=o, data0=a, data1=b, initial=0.0, op0=ALU.mult, op1=ALU.add),
                     [("rb", 6), ("rb", 8)], [("rb", 9)])
                h3 = v3(hh[:, 0:NB], blk)
                if samp:
                    cp("dve", rgout[:, l, j, :], h3[:, :, T - 1], [("rb", 9)], ["rgin"])
                else:
                    cp("dve", rgcar[:, l, j, :], h3[:, :, T - 1], [("rb", 9)], ["rgcar"])
                tt(mix_sb[:, 16 + j, 0:NB], hh[:, 0:NB], bft[0][:, 0:NB], ALU.mult, [("rb", 9), ("bft", 0)], [("mix", 16 + j)])

            stage(5)
            for f in range(FT):
                pb0 = 4 * (f % 2)
                proj_tile(w_gate_d[l, :, f * 128:(f + 1) * 128], 16, hf, hk, pb0, NB)
                proj_tile(w_gate_d[l, :, D + f * 128:D + (f + 1) * 128], 16, hf, hk, pb0 + 1, NB)
                proj_tile(w_bs_d[l, :, f * 128:(f + 1) * 128], 8, lambda k: mix_sb[:, 8 + k, 0:NB], lambda k: ("mix", 8 + k), pb0 + 2, NB)
                proj_tile(w_br_d[l, :, f * 128:(f + 1) * 128], 8, lambda k: mix_sb[:, 16 + k, 0:NB], lambda k: ("mix", 16 + k), pb0 + 3, NB)
                sa, ka = T32()
                sb_, kb = T32()
                act(sa[:, 0:NB], psum[pb0][:, 0:NB], AF.Sigmoid, [PS(pb0), "small"], [ka], bias=small_sb[:, l, 48 + f:49 + f])
                act(sb_[:, 0:NB], psum[pb0 + 1][:, 0:NB], AF.Sigmoid, [PS(pb0 + 1), "small"], [kb], bias=small_sb[:, l, 64 + f:65 + f])
                tt(sa[:, 0:NB], sa[:, 0:NB], psum[pb0 + 2][:, 0:NB], ALU.mult, [ka, PS(pb0 + 2)], [ka])
                tt(sb_[:, 0:NB], sb_[:, 0:NB], psum[pb0 + 3][:, 0:NB], ALU.mult, [kb, PS(pb0 + 3)], [kb])
                tt(mrg(f, NB), sa[:, 0:NB], sb_[:, 0:NB], ALU.add, [ka, kb], [("rb", f // 2)])
            for f in range(FT):
                pb = f % 4
                proj_tile(w_out_d[l, :, f * 128:(f + 1) * 128], 16, lambda k: mrg(k, NB), lambda k: ("rb", k // 2), pb, NB)
                tq, kq = T32()
                tt(v3(tq[:, 0:NB], blk), v3(psum[pb][:, 0:NB], blk), modb(mod_sb, l * 6 + 2, f, blk), ALU.mult,
                   [PS(pb), ("mod", l, 2)], [kq])
                tt(x_sb[:, f, 0:NB], x_sb[:, f, 0:NB], tq[:, 0:NB], ALU.add, [("x", f), kq], [("x", f)])

        def ffn_half(l, blk, w1_ap, w3_ap, w2_ap, comb_ap, comb_key):
            NB = blk["NB"]
            hk = lambda k: ("h", k)
            hf = lambda k: h_sb[:, k, 0:NB]
            for t_ in range(24):
                pb = 2 * (t_ % 2)
                proj_tile(w1_ap[:, t_ * 128:(t_ + 1) * 128], 16, hf, hk, pb, NB)
                proj_tile(w3_ap[:, t_ * 128:(t_ + 1) * 128], 16, hf, hk, pb + 1, NB)
                tq, kq = T32()
                act(tq[:, 0:NB], psum[pb][:, 0:NB], AF.Silu, [PS(pb)], [kq])
                tt(mix_sb[:, t_, 0:NB], tq[:, 0:NB], psum[pb + 1][:, 0:NB], ALU.mult, [kq, PS(pb + 1)], [("mix", t_)])
            for f in range(FT):
                pb = 4 + f % 3
                s1, k1 = load_slab(w2_ap[0:2048, f * 128:(f + 1) * 128], 16)
                s2, k2 = load_slab(w2_ap[2048:3072, f * 128:(f + 1) * 128], 8)
                for k in range(24):
                    sl, sk = (s1, k1) if k < 16 else (s2, k2)
                    mm(psum[pb][:, 0:NB], sl[:, k % 16, :], mix_sb[:, k, 0:NB], k == 0, k == 23, [sk, ("mix", k)], [PS(pb)])
                tq, kq = T32()
                tt(v3(tq[:, 0:NB], blk), v3(psum[pb][:, 0:NB], blk), modb(mod_sb, l * 6 + 5, f, blk), ALU.mult,
                   [PS(pb), ("mod", l, 5)], [kq])
                if comb_ap is not None:
                    tt(tq[:, 0:NB], tq[:, 0:NB], comb_ap[:, 0:NB], ALU.mult, [kq, comb_key], [kq])
                tt(x_sb[:, f, 0:NB], x_sb[:, f, 0:NB], tq[:, 0:NB], ALU.add, [("x", f), kq], [("x", f)])

        def moe_router(blk):
            NB = blk["NB"]
            nsub = max(1, NB // 128)
            tk = min(128, NB)
            lg_sb = rbuf[0][0:8, :]
            combT = rbuf[1][0:8, :]
            cp("act", lg_sb[:, 0:NB], psum[1][0:8, 0:NB], [PS(1)], [("rb", 0)])
            for s in range(nsub):
                P.op("pe", lambda e, o=psum[2][0:tk, s * 8:(s + 1) * 8], i=lg_sb[:, s * tk:(s + 1) * tk]:
                     e.transpose(o, i, ident[0:8, 0:8]), [("rb", 0), "ident"], [PS(2)])
            cp("act", lgT[0:tk, 0:nsub, :], psum[2][0:tk, 0:nsub * 8].rearrange("p (s e) -> p s e", e=8), [PS(2)], ["lgT"])
            for s in range(nsub):
                P.op("dve", lambda e, o=mx8[0:tk, s, :], i=lgT[0:tk, s, :]: e.max(o, i), ["lgT"], ["mx8"])
                ts(cmb[0:tk, s, :], lgT[0:tk, s, :], mx8[0:tk, s, 0:1], None, ALU.subtract, None, ["lgT", "mx8"], ["cmb"])
                act(cmb[0:tk, s, :], cmb[0:tk, s, :], AF.Exp, ["cmb"], ["cmb"])
                ts(cmbt[0:tk, s, :], lgT[0:tk, s, :], mx8[0:tk, s, 1:2], None, ALU.is_ge, None, ["lgT", "mx8"], ["cmbt"])
                tt(cmb[0:tk, s, :], cmb[0:tk, s, :], cmbt[0:tk, s, :], ALU.mult, ["cmb", "cmbt"], ["cmb"])
                P.op("dve", lambda e, o=den[0:tk, s, :], i=cmb[0:tk, s, :]:
                     e.reduce_sum(o, i, axis=mybir.AxisListType.X), ["cmb"], ["den"])
                P.op("dve", lambda e, o=den[0:tk, s, :]: e.reciprocal(o, o), ["den"], ["den"])
                ts(cmb[0:tk, s, :], cmb[0:tk, s, :], den[0:tk, s, 0:1], None, ALU.mult, None, ["cmb", "den"], ["cmb"])
                P.op("pe", lambda e, o=psum[3][0:8, s * tk:(s + 1) * tk], i=cmb[0:tk, s, :]:
                     e.transpose(o, i, ident[0:tk, 0:tk]), ["cmb", "ident"], [PS(3)])
            cp("act", combT[:, 0:NB], psum[3][0:8, 0:NB], [PS(3)], [("rb", 1)])

        for bi, blk in enumerate(blocks):
            NB = blk["NB"]
            c0 = blk["c0"]
            blk_tile[0] = 0
            dma("sp", x_sb[:, :, 0:NB], xT_d[:, :, c0:c0 + NB], (), [("x", f) for f in range(FT)] + ["s5m", "bbar", ("tabst", 0), ("tabst", 1)], key=("xin",))
            for l in range(DEPTH):
                rmsnorm_mod(l, 0, blk)
                stage(2)
                mixer(l, blk)
                stage(6)
                if l % 2 == 0:
                    rmsnorm_mod(l, 1, blk)
                    for hh_ in range(2):
                        ffn_half(l, blk, f_w1_d[0, :, hh_ * 3072:(hh_ + 1) * 3072], f_w3_d[0, :, hh_ * 3072:(hh_ + 1) * 3072],
                                 f_w2_d[0, hh_ * 3072:(hh_ + 1) * 3072, :], None, None)
                else:
                    rmsnorm_mod(l, 1, blk, want_router=True)
                    moe_router(blk)
                    for e_ in range(8):
                        cbuf = rbuf[2 + e_ % 2]
                        ck = ("rb", 2 + e_ % 2)
                        msk = rbuf[4 + e_ % 2][0:8, :]
                        mk_ = ("rb", 4 + e_ % 2)
                        ts(msk[:, 0:NB], rbuf[1][0:8, 0:NB], ident[0:8, e_:e_ + 1], None, ALU.mult, None,
                           [("rb", 1), "ident"], [mk_])
                        mm(psum[7][:, 0:NB], ones_f[0:8, :], msk[:, 0:NB], True, True, ["ones", mk_], [PS(7)])
                        cp("act", cbuf[:, 0:NB], psum[7][:, 0:NB], [PS(7)], [ck])
                        ffn_half(l, blk, m_w1_d[0, e_], m_w3_d[0, e_], m_w2_d[0, e_], cbuf, ck)
            pb = 0
            for ft in range(FT):
                tq, kq = T32()
                act(tq[:, 0:NB], x_sb[:, ft, 0:NB], AF.Square, [("x", ft)], [kq])
                mm(psum[pb][:, 0:NB], ones_f[:], tq[:, 0:NB], ft == 0, ft == FT - 1, ["ones", kq], [PS(pb)])
            rstd = rbuf[10]
            act(rstd[:, 0:NB], psum[pb][:, 0:NB], AF.Ln, [PS(pb)], [("rb", 10)], bias=1e-6, scale=1.0 / D)
            act(rstd[:, 0:NB], rstd[:, 0:NB], AF.Exp, [("rb", 10)], [("rb", 10)], scale=-0.5)
            for ft in range(FT):
                tt(x_sb[:, ft, 0:NB], x_sb[:, ft, 0:NB], rstd[:, 0:NB], ALU.mult, [("x", ft), ("rb", 10)], [("x", ft)])
                ts(x_sb[:, ft, 0:NB], x_sb[:, ft, 0:NB], small_sb[:, 0, 32 + ft:33 + ft], None, ALU.mult, None,
                   [("x", ft), "small"], [("x", ft)])
            dma("sp", yT_d[:, :, c0:c0 + NB], x_sb[:, :, 0:NB], [("x", f) for f in range(FT)], (), key=("out",))

    except _Stop:
        pass

    dma("sp", o_s5s_d.rearrange("l r p j q -> p l r j q"), s5out[:], [("s5in", j) for j in range(32)], (), key=("out",))
    dma("sp", o_s5p_d.rearrange("l r p j -> p l r j"), s5car[:, :, :, :, 0], [("s5car", j) for j in range(32)], (), key=("out",))
    dma("sp", o_rgs_d.rearrange("l p j q -> p l j q"), rgout[:], ["rgin"], (), key=("out",))
    dma("sp", o_rgp_d.rearrange("l p j -> p l j"), rgcar[:, :, :, 0], ["rgcar"], (), key=("out",))
    dma("sp", o_cvs_d.rearrange("l p j q k -> p l j q k"), cvout[:], ["cvin"], (), key=("out",))
    dma("sp", o_cvp_d.rearrange("l p j k -> p l j k"), cvtail[:, :, :, 0, :], ["cvtail"], (), key=("out",))

    P.emit(final_wait_keys=[("out",)])
    st.close()
    return nc


def _ft_layout(v):
    k = v.shape[-1] // 128
    return np.ascontiguousarray(v.reshape(k, 128).T)


def _prep_small(inp):
    out = np.zeros((DEPTH, 128, NS_SMALL), np.float32)
    for l in range(DEPTH):
        o = out[l]
        o[:, 0:16] = _ft_layout(inp["norm_mix"][l])
        o[:, 16:32] = _ft_layout(inp["norm_ffn"][l])
        o[:, 32:48] = _ft_layout(inp["norm_f"])
        o[:, 48:80] = _ft_layout(inp["b_gate"][l])
        o[:, 80:88] = _ft_layout(inp["s5_b_glu"][l])
        for k in range(4):
            o[:, 88 + k * 8:96 + k * 8] = _ft_layout(inp["rg_conv_w"][l, k])
        o[:, 120:128] = _ft_layout(inp["rg_conv_b"][l])
        o[:, 128:136] = _ft_layout(inp["rg_lam"][l])
        o[:, 136:144] = _ft_layout(inp["rg_b_a"][l].reshape(-1))
        o[:, 144:152] = _ft_layout(inp["rg_b_i"][l].reshape(-1))
        o[:, 152:160] = _ft_layout(inp["s5_d"][l].reshape(-1))
        o[:, 160:256] = _ft_layout(inp["b_ada"][l])
        sp = lambda a: np.ascontiguousarray(a.reshape(32, 2, 64).transpose(1, 2, 0).reshape(128, 32))
        o[:, 256:288] = sp(inp["s5_lam_re"][l])
        o[:, 288:320] = sp(inp["s5_lam_im"][l])
        o[:, 320:352] = sp(np.repeat(inp["s5_log_dt"][l][:, None], 64, axis=1))
    return out


def _prep_s5m(inp):
    out = np.zeros((DEPTH, 128, 4, 32, 32), np.float32)
    for l in range(DEPTH):
        for idx, name in enumerate(("s5_b_re", "s5_b_im")):
            b = inp[name][l].reshape(32, 2, 64, 16)
            for g2 in range(2):
                out[l, g2 * 64:(g2 + 1) * 64, idx, :, g2 * 16:(g2 + 1) * 16] = b[:, g2].transpose(1, 0, 2)
        for idx, name in enumerate(("s5_c_re", "s5_c_im")):
            c = inp[name][l].reshape(32, 2, 16, 64)
            for g2 in range(2):
                out[l, g2 * 64:(g2 + 1) * 64, 2 + idx, :, g2 * 16:(g2 + 1) * 16] = c[:, g2].transpose(2, 0, 1)
    return out


def _prep_rgw(inp):
    out = np.zeros((DEPTH, 128, 2, 8, 128), np.float32)
    for l in range(DEPTH):
        for a, name in enumerate(("rg_w_a", "rg_w_i")):
            w = inp[name][l]
            for j in range(8):
                for h2 in range(2):
                    out[l, h2 * 64:(h2 + 1) * 64, a, j, h2 * 64:(h2 + 1) * 64] = w[j * 2 + h2]
    return out


_NC_CACHE = {}


def kernel(**inp):
    inp = {k: np.asarray(v) for k, v in inp.items()}
    if "nc" not in _NC_CACHE:
        _NC_CACHE["nc"] = build_program()
    nc = _NC_CACHE["nc"]
    in_maps = _make_in_maps(inp)
    res = run_bass_kernel_spmd(nc, in_maps, core_ids=list(range(8)))
    return _assemble(res.results)


def _make_in_maps(inp):
    small = _prep_small(inp)
    s5m = _prep_s5m(inp)
    rgw = _prep_rgw(inp)
    router = np.ascontiguousarray(inp["moe_router"][0].reshape(FT, 128, 8).transpose(1, 0, 2))
    ident = np.eye(128, dtype=np.float32)
    pmask = np.zeros((128, 4), np.float32)
    for jj in range(4):
        pmask[32 * jj:32 * jj + 32, jj] = 1.0
    shared = dict(small=small, s5m=s5m, rgw=rgw, router=router, ident=ident, pmask=pmask)
    for k in ("w_ada", "w_in", "s5_w_glu", "w_gate", "w_br_s5", "w_br_rg", "w_out", "ffn_w1", "ffn_w3", "ffn_w2",
              "moe_w1", "moe_w3", "moe_w2"):
        shared[k] = np.ascontiguousarray(inp[k], dtype=np.float32)
    in_maps = []
    for c in range(8):
        b = c % 4
        qs = slice(c * NSAMP, (c + 1) * NSAMP)
        xtok = np.concatenate([inp["x_prompt"][b], inp["x_sample"][qs].reshape(NSAMP * TS, D)], axis=0)
        xT = np.ascontiguousarray(xtok.reshape(-1, FT, 128).transpose(2, 1, 0))
        cc = np.concatenate([inp["c_prompt"][b:b + 1], inp["c_sample"][qs]], axis=0)
        cT = np.ascontiguousarray(cc.reshape(-1, FT, 128).transpose(2, 1, 0))
        s5st = np.stack([inp["state_s5_re"][:, qs], inp["state_s5_im"][:, qs]], axis=1)
        s5st = s5st.reshape(DEPTH, 2, NSAMP, 32, 2, 64).transpose(0, 1, 4, 5, 3, 2).reshape(DEPTH, 2, 128, 32, NSAMP)
        rgst = inp["state_rglru"][:, qs].reshape(DEPTH, NSAMP, 8, 128).transpose(0, 3, 2, 1)
        cvst = inp["state_conv"][:, qs].reshape(DEPTH, NSAMP, 3, 8, 128).transpose(0, 4, 3, 1, 2)
        m = dict(shared)
        m.update(xT=xT, cT=cT, s5st=np.ascontiguousarray(s5st), rgst=np.ascontiguousarray(rgst),
                 cvst=np.ascontiguousarray(cvst))
        in_maps.append(m)
    return in_maps


def _assemble(R):
    B = 4
    y_prompt = np.zeros((B, SEQ, D), np.float32)
    y_sample = np.zeros((8 * NSAMP, TS, D), np.float32)
    p_s5_re = np.zeros((DEPTH, B, 64, 64), np.float32)
    p_s5_im = np.zeros_like(p_s5_re)
    p_rg = np.zeros((DEPTH, B, 1024), np.float32)
    p_conv = np.zeros((DEPTH, B, 3, 1024), np.float32)
    s_s5_re = np.zeros((DEPTH, 8 * NSAMP, 64, 64), np.float32)
    s_s5_im = np.zeros_like(s_s5_re)
    s_rg = np.zeros((DEPTH, 8 * NSAMP, 1024), np.float32)
    s_conv = np.zeros((DEPTH, 8 * NSAMP, 3, 1024), np.float32)
    for c in range(8):
        r = R[c]
        yT = np.asarray(r["yT"])
        ytok = yT.transpose(2, 1, 0).reshape(-1, D)
        qs = slice(c * NSAMP, (c + 1) * NSAMP)
        y_sample[qs] = ytok[SEQ:].reshape(NSAMP, TS, D)
        s5s = np.asarray(r["o_s5s"]).reshape(DEPTH, 2, 2, 64, 32, NSAMP)
        s5s = s5s.transpose(0, 1, 5, 4, 2, 3).reshape(DEPTH, 2, NSAMP, 64, 64)
        s_s5_re[:, qs] = s5s[:, 0]
        s_s5_im[:, qs] = s5s[:, 1]
        s_rg[:, qs] = np.asarray(r["o_rgs"]).transpose(0, 3, 2, 1).reshape(DEPTH, NSAMP, 1024)
        s_conv[:, qs] = np.asarray(r["o_cvs"]).transpose(0, 3, 4, 2, 1).reshape(DEPTH, NSAMP, 3, 1024)
        if c < B:
            y_prompt[c] = ytok[:SEQ]
            s5p = np.asarray(r["o_s5p"]).reshape(DEPTH, 2, 2, 64, 32)
            s5p = s5p.transpose(0, 1, 4, 2, 3).reshape(DEPTH, 2, 64, 64)
            p_s5_re[:, c] = s5p[:, 0]
            p_s5_im[:, c] = s5p[:, 1]
            p_rg[:, c] = np.asarray(r["o_rgp"]).transpose(0, 2, 1).reshape(DEPTH, 1024)
            p_conv[:, c] = np.asarray(r["o_cvp"]).transpose(0, 3, 2, 1).reshape(DEPTH, 3, 1024)
    return (y_prompt, y_sample, p_s5_re, p_s5_im, p_rg, p_conv, s_s5_re, s_s5_im, s_rg, s_conv)
```

```python
import contextlib
import math
import numpy as np
import concourse.bass as bass
import concourse.mybir as mybir
from concourse.bass_utils import run_bass_kernel_spmd

F32 = mybir.dt.float32
BF16 = mybir.dt.bfloat16
I32 = mybir.dt.int32
AF = mybir.ActivationFunctionType
ALU = mybir.AluOpType

D = 2048
FT = 16
SEQ = 2048
NSAMP = 16
TS = 4
DEPTH = 2
NS_SMALL = 352
TWO_PI = 2.0 * math.pi
GELU_K = 1.5957691216057308


class Prog:
    ENG = ("pe", "act", "dve", "pool", "sp")

    def __init__(self, nc):
        self.nc = nc
        self.ops = []
        self.last_writer = {}
        self.readers = {}
        self.dma_key_count = {}
        self.dma_key_waitall = set()

    def op(self, eng, fn, reads=(), writes=(), dma_key=None, wait_all=False, cc=False):
        ps_r = [k for k in reads if isinstance(k, tuple) and k and k[0] == "ps"]
        if ps_r:
            reads = [k for k in reads if k not in ps_r]
            writes = list(writes) + [k for k in ps_r if k not in writes]
        idx = len(self.ops)
        deps = set()
        war = set()
        for b in reads:
            w = self.last_writer.get(b)
            if w is not None:
                deps.add(w)
        for b in writes:
            w = self.last_writer.get(b)
            if w is not None:
                deps.add(w)
            for r in self.readers.get(b, {}).values():
                war.add(r)
        o = dict(eng=eng, fn=fn, deps=deps, war=war, dma_key=dma_key, signal=False, cc=cc)
        if dma_key is not None:
            self.dma_key_count[dma_key] = self.dma_key_count.get(dma_key, 0) + 1
            o["dma_n"] = self.dma_key_count[dma_key]
            if wait_all:
                self.dma_key_waitall.add(dma_key)
        self.ops.append(o)
        rk = eng if dma_key is None else ("dma", idx)
        for b in reads:
            self.readers.setdefault(b, {})[rk] = idx
        for b in writes:
            self.last_writer[b] = idx
            self.readers[b] = {}
        return idx

    def emit(self, final_wait_keys=()):
        nc = self.nc
        ops = self.ops
        for o in ops:
            nd = set()
            for d in o["deps"] | o["war"]:
                p = ops[d]
                if p["dma_key"] is None and o["dma_key"] is None and p["eng"] == o["eng"]:
                    if o["eng"] == "pe":
                        continue
                nd.add(d)
            o["deps"] = nd
            for d in nd:
                ops[d]["signal"] = True
        cnt = {e: 0 for e in self.ENG}
        for o in ops:
            if o["dma_key"] is not None:
                k = o["dma_key"]
                n = self.dma_key_count[k] if k in self.dma_key_waitall else o["dma_n"]
                o["sig"] = (("dma", k), (1 if o["cc"] else 16) * n)
            elif o["signal"]:
                cnt[o["eng"]] += 1
                o["sig"] = (("eng", o["eng"]), cnt[o["eng"]])
        per_eng = {e: [o for o in ops if o["eng"] == e] for e in self.ENG}
        sem_names = [("eng", e) for e in self.ENG] + [("dma", k) for k in self.dma_key_count]
        with contextlib.ExitStack() as st:
            sems = {}
            for i, sn in enumerate(sem_names):
                sems[sn] = st.enter_context(nc.semaphore("s%d" % i))
            block = st.enter_context(nc.Block())
            engobj = {"pe": "tensor", "act": "scalar", "dve": "vector", "pool": "gpsimd", "sp": "sync"}

            def run(e, eng):
                known = {}
                for o in per_eng[e]:
                    need = {}
                    for d in o["deps"]:
                        s, v = ops[d]["sig"]
                        if v > need.get(s, 0):
                            need[s] = v
                    for s, v in need.items():
                        if known.get(s, 0) < v:
                            eng.wait_ge(sems[s], v)
                            known[s] = v
                    ins = o["fn"](eng)
                    if o["cc"]:
                        ins.then_inc(sems[("dma", o["dma_key"])])
                    elif o["dma_key"] is not None:
                        ins.then_inc(sems[("dma", o["dma_key"])], 16)
                    elif o["signal"]:
                        ins.then_inc(sems[("eng", e)], 1)
                if e == "sp":
                    for k in final_wait_keys:
                        v = 16 * self.dma_key_count[k]
                        if known.get(("dma", k), 0) < v:
                            eng.wait_ge(sems[("dma", k)], v)

            for e in self.ENG:
                def mk(e):
                    def f(eng):
                        run(e, eng)
                    return f
                getattr(block, engobj[e])(mk(e))


class _Stop(Exception):
    pass


def build_program(n_prompt_blocks=SEQ // 512, stop_stage=None):
    nc = bass.Bass("TRN2", target_bir_lowering=False)

    def stage(n):
        if stop_stage is not None and n == stop_stage:
            raise _Stop()

    P = Prog(nc)
    st = contextlib.ExitStack()

    def din(name, shape):
        return nc.dram_tensor(name, list(shape), F32, kind="ExternalInput").ap()

    def dout(name, shape):
        return nc.dram_tensor(name, list(shape), F32, kind="ExternalOutput").ap()

    def sb(name, shape, dt=F32):
        return st.enter_context(nc.sbuf_tensor(name, list(shape), dt))

    NTOK = SEQ + NSAMP * TS
    xT_d = din("xT", [128, FT, NTOK])
    cT_d = din("cT", [128, FT, 1 + NSAMP])
    small_d = din("small", [DEPTH, 128, NS_SMALL])
    s5m_d = din("s5m", [DEPTH, 128, 4, 32, 32])
    rgw_d = din("rgw", [DEPTH, 128, 2, 8, 128])
    router_d = din("router", [128, FT, 8])
    ident_d = din("ident", [128, 128])
    pmask_d = din("pmask", [128, 4])
    s5st_d = din("s5st", [DEPTH, 2, 128, 32, NSAMP])
    rgst_d = din("rgst", [DEPTH, 128, 8, NSAMP])
    cvst_d = din("cvst", [DEPTH, 128, 8, NSAMP, 3])
    w_ada_d = din("w_ada", [DEPTH, D, 6 * D])
    w_in_d = din("w_in", [DEPTH, D, 3072])
    w_glu_d = din("s5_w_glu", [DEPTH, 1024, 1024])
    w_gate_d = din("w_gate", [DEPTH, D, 2 * D])
    w_bs_d = din("w_br_s5", [DEPTH, 1024, D])
    w_br_d = din("w_br_rg", [DEPTH, 1024, D])
    w_out_d = din("w_out", [DEPTH, D, D])
    f_w1_d = din("ffn_w1", [2, D, 3072])
    f_w3_d = din("ffn_w3", [2, D, 3072])
    f_w2_d = din("ffn_w2", [2, 3072, D])
    m_w1_d = din("moe_w1", [8, D, 3072])
    m_w3_d = din("moe_w3", [8, D, 3072])
    m_w2_d = din("moe_w2", [8, 3072, D])
    esel_d = din("esel", [8, 8])
    xold_d = nc.dram_tensor("xold", [128, FT * 512], F32)
    ccin_d = nc.dram_tensor("ccin", [128, FT * 512], F32)
    ccout_d = nc.dram_tensor("ccout", [128, FT * 512], F32)
    PAIR_GROUPS = [[0, 4], [1, 5], [2, 6], [3, 7]]

    yT_d = dout("yT", [128, FT, NTOK])
    o_s5s_d = dout("o_s5s", [DEPTH, 2, 128, 32, NSAMP])
    o_s5p_d = dout("o_s5p", [DEPTH, 2, 128, 32])
    o_rgs_d = dout("o_rgs", [DEPTH, 128, 8, NSAMP])
    o_rgp_d = dout("o_rgp", [DEPTH, 128, 8])
    o_cvs_d = dout("o_cvs", [DEPTH, 128, 8, NSAMP, 3])
    o_cvp_d = dout("o_cvp", [DEPTH, 128, 8, 3])

    NBMAX = 512
    x_sb = sb("x_sb", [128, FT, NBMAX])
    h_sb = sb("h_sb", [128, FT, NBMAX], BF16)
    mix_sb = sb("mix_sb", [128, 24, NBMAX], BF16)
    NSLAB = 3
    slabs = [sb("slab%d" % i, [128, 16, 128], BF16) for i in range(NSLAB)]
    NTMP = 4
    tmps = [sb("tmp%d" % i, [128, NBMAX]) for i in range(NTMP)]
    NR = 11
    rb_all = sb("rb_all", [128, NR, NBMAX])
    rbuf = [rb_all[:, i, :] for i in range(NR)]
    rb_bf = rb_all.bitcast(BF16)

    def mrg(f, NB):
        return rb_bf[:, f // 2, (f % 2) * NBMAX:(f % 2) * NBMAX + NB]
    xrext = sb("xrext", [128, NBMAX + 64])
    bft = [sb("bft%d" % i, [128, NBMAX], BF16) for i in range(2)]
    um = [sb("um%d" % i, [128, NBMAX], BF16) for i in range(4)]
    tabb = [sb("tabb%d" % i, [128, 2, NBMAX]) for i in range(2)]
    ones512 = sb("ones512", [128, NBMAX])
    mask01 = sb("mask01", [128, NSAMP, TS])
    upw = sb("upw", [128, 32, 9, 3])
    tab_d = nc.dram_tensor("tabscr", [DEPTH * 32, 128, 2 * NBMAX], F32).ap()
    bft2 = [sb("bft2%d" % i, [128, NBMAX], BF16) for i in range(2)]
    hbfs = [bft, bft2]
    wcp = [sb("wcp%d" % i, [128, 128], BF16) for i in range(8)]
    small_sb = sb("small_sb", [128, DEPTH, NS_SMALL])
    mod_sb = sb("mod_sb", [128, DEPTH * 6, FT, 1 + NSAMP, 1])
    cT_sb = sb("cT_sb", [128, FT, 1 + NSAMP])
    cs_bf = sb("cs_bf", [128, FT, 1 + NSAMP], BF16)
    ident = sb("ident_sb", [128, 128])
    router_sb = sb("router_sb", [128, FT, 8])
    s5m_sb = x_sb[:, 0:8, :].rearrange("p a b -> p (a b)").rearrange("p (i j m) -> p i j m", i=4, j=32)
    bbar = x_sb[:, 8:12, :].rearrange("p a b -> p (a b)").rearrange("p (i j m) -> p i j m", i=2, j=32)
    wbT = sb("wbT", [128, DEPTH, 2, 8, 128], BF16)
    cb = sb("cb", [128, DEPTH, 2, 32, 32], BF16)
    NLV = 9
    pw = sb("pw", [128, DEPTH, 32, NLV, 3])
    sp_t = [sb("spt%d" % i, [128, 32]) for i in range(14)]
    pw_rho = sb("pw_rho", [128, DEPTH, 32])
    sp_i = sb("spi", [128, 32], I32)
    rgw_sb = sb("rgw_sb", [128, DEPTH, 2, 8, 128], BF16)
    rgc = sb("rgc", [128, DEPTH, 2, 8])
    s5car = sb("s5car", [128, DEPTH, 2, 32, 1])
    rgcar = sb("rgcar", [128, DEPTH, 8, 1])
    cvtail = sb("cvtail", [128, DEPTH, 8, 1, 3])
    s5in = sb("s5in", [128, DEPTH, 2, 32, NSAMP])
    s5out = s5in
    rgin = sb("rgin", [128, DEPTH, 8, NSAMP])
    rgout = rgin
    cvin = sb("cvin", [128, DEPTH, 8, NSAMP, 3])
    cvout = cvin
    car4 = sb("car4", [128, 4, NSAMP])
    lgT = sb("lgT", [128, 4, 8])
    mx8 = sb("mx8", [128, 4, 8])
    cmb = sb("cmb", [128, 4, 8])
    cmbt = sb("cmbt", [128, 4, 8])
    den = sb("den", [128, 4, 1])
    ones_f = sb("ones_f", [128, 128])
    pmask = sb("pmask_sb", [128, 4])
    esel = sb("esel_sb", [8, 8])

    psum = [st.enter_context(nc.psum_tensor("ps%d" % i, [128, 512], F32)) for i in range(8)]

    def mm(out, lhsT, rhs, start, stop, reads, writes):
        P.op("pe", lambda e, a=out, b=lhsT, c=rhs, s=start, t=stop: e.matmul(a, lhsT=b, rhs=c, start=s, stop=t),
             reads, writes)

    def act(out, in_, func, reads, writes, bias=None, scale=None):
        kw = {}
        if bias is not None:
            kw["bias"] = bias
        if scale is not None:
            kw["scale"] = scale
        P.op("act", lambda e, a=out, b=in_, f=func, k=kw: e.activation(a, b, f, **k), reads, writes)

    def tt(out, a, b, op, reads, writes, eng="dve"):
        P.op(eng, lambda e, o=out, x=a, y=b, p=op: e.tensor_tensor(out=o, in0=x, in1=y, op=p), reads, writes)

    def ts(out, a, s1, s2, op0, op1, reads, writes, eng="dve"):
        if s2 is None:
            P.op(eng, lambda e, o=out, x=a, u=s1, p=op0: e.tensor_scalar(o, x, u, None, p), reads, writes)
        else:
            P.op(eng, lambda e, o=out, x=a, u=s1, v=s2, p=op0, q=op1: e.tensor_scalar(o, x, u, v, p, q), reads, writes)

    def stt(out, in0, scalar, in1, op0, op1, reads, writes, eng="dve"):
        P.op(eng, lambda e, o=out, x=in0, s=scalar, y=in1, p=op0, q=op1:
             e.scalar_tensor_tensor(out=o, in0=x, scalar=s, in1=y, op0=p, op1=q), reads, writes)

    def cp(eng, out, in_, reads, writes):
        if eng == "act":
            P.op("act", lambda e, o=out, i=in_: e.copy(o, i), reads, writes)
        else:
            P.op(eng, lambda e, o=out, i=in_: e.tensor_copy(o, i), reads, writes)

    def memset(eng, ap, val, writes):
        P.op(eng, lambda e, a=ap, v=val: e.memset(a, v), (), writes)

    def dma(q, out, in_, reads, writes, key, wait_all=False):
        P.op(q, lambda e, o=out, i=in_: e.dma_start(out=o, in_=i), reads, writes, dma_key=key, wait_all=wait_all)

    slab_ctr = [0]
    NSCR = 1056
    SCR_PER = 448
    wscrs = [nc.dram_tensor("wscr%d" % i, [SCR_PER, 128, 2048], BF16).ap() for i in range(3)]
    scr_ids = {}

    def load_slab(src_ap, K, tag=None):
        s = slab_ctr[0] % NSLAB
        slab_ctr[0] += 1
        key = ("slab", s)
        dst = slabs[s][:, 0:K, :]
        if tag is None:
            dma("pool", dst, src_ap.rearrange("(k p) m -> p k m", p=128), (), [key], key=("slabq", s))
            return slabs[s], key
        first = tag not in scr_ids
        if first:
            scr_ids[tag] = len(scr_ids)
        tid = scr_ids[tag]
        assert tid < 3 * SCR_PER
        scr_v = wscrs[tid // SCR_PER][tid % SCR_PER, :, 0:K * 128].rearrange("p (k m) -> p k m", m=128)
        if first:
            dma("pool", dst, src_ap.rearrange("(k p) m -> p k m", p=128), (), [key], key=("slabq", s))
            dma("sp", scr_v, dst, [key], [("scr", tid)], key=("wbq", s))
        else:
            dma("sp", dst, scr_v, [("scr", tid)], [key], key=("slabh", s))
        return slabs[s], key

    tmp_ctr = [0]

    def T32():
        i = tmp_ctr[0] % NTMP
        tmp_ctr[0] += 1
        return tmps[i], ("tmp", i)

    PS = lambda i: ("ps", i)

    C = "const"
    dma("sp", small_sb[:], small_d.rearrange("l p n -> p l n"), (), ["small"], key=C, wait_all=True)
    dma("sp", cT_sb[:], cT_d, (), ["cT"], key=C, wait_all=True)
    dma("sp", ident[:], ident_d, (), ["ident"], key=C, wait_all=True)
    dma("sp", pmask[:], pmask_d, (), ["pmask"], key=C, wait_all=True)
    dma("sp", esel[:], esel_d, (), ["esel"], key=C, wait_all=True)
    dma("sp", router_sb[:], router_d, (), ["router"], key=C, wait_all=True)
    dma("sp", s5in[:], s5st_d.rearrange("l r p j q -> p l r j q"), (), [("s5in", j) for j in range(32)], key=C, wait_all=True)
    dma("sp", rgin[:], rgst_d.rearrange("l p j q -> p l j q"), (), ["rgin"], key=C, wait_all=True)
    dma("sp", cvin[:], cvst_d.rearrange("l p j q k -> p l j q k"), (), ["cvin"], key=C, wait_all=True)
    dma("pool", rgw_sb[:], rgw_d.rearrange("l p a j m -> p l a j m"), (), ["rgw"], key=("rgwq",))
    memset("dve", ones_f[:], 1.0, ["ones"])
    memset("dve", ones512[:], 1.0, ["ones512"])
    memset("dve", mask01[:], 1.0, ["mask01"])
    memset("dve", mask01[:, :, 0:1], 0.0, ["mask01"])
    for i in range(8):
        memset("dve", wcp[i][:], 0.0, [("wcp", i)])
    memset("dve", s5car[:], 0.0, [("s5car", j) for j in range(32)])
    memset("dve", rgcar[:], 0.0, ["rgcar"])
    memset("dve", cvtail[:], 0.0, ["cvtail"])

    def smallv(l, c0, n):
        return small_sb[:, l, c0:c0 + n]

    try:
        act(cs_bf[:], cT_sb[:], AF.Silu, ["cT"], ["cs"])
        NSQ = 1 + NSAMP
        for l in range(DEPTH):
            for kind in range(6):
                pb = (l * 6 + kind) % 4
                for ft in range(FT):
                    col0 = (kind * FT + ft) * 128
                    slab, skey = load_slab(w_ada_d[l, :, col0:col0 + 128], 16)
                    for k in range(16):
                        mm(psum[pb][:, ft * NSQ:(ft + 1) * NSQ], slab[:, k, :], cs_bf[:, k, :], k == 0, k == 15,
                           [skey, "cs"], [PS(pb)])
                bias_b = small_sb[:, l, 160 + kind * FT:160 + (kind + 1) * FT].rearrange("p (f o) -> p f o", o=1) \
                    .to_broadcast([128, FT, NSQ])
                tt(mod_sb[:, l * 6 + kind, :, :, 0], psum[pb][:, 0:FT * NSQ].rearrange("p (f s) -> p f s", s=NSQ),
                   bias_b, ALU.add, [PS(pb), "small"], [("mod", l, kind)])
            for which, (kind, ncol) in enumerate(((1, 0), (4, 16))):
                tmpv = mod_sb[:, l * 6 + kind, :, :, 0]
                ts(tmpv, tmpv, 1.0, None, ALU.add, None, [("mod", l, kind)], [("mod", l, kind)])
                nb = small_sb[:, l, ncol:ncol + FT].rearrange("p (f o) -> p f o", o=1).to_broadcast([128, FT, NSQ])
                tt(tmpv, tmpv, nb, ALU.mult, [("mod", l, kind), "small"], [("mod", l, kind)])

        stage(0)
        for l in range(DEPTH):
            lamre = smallv(l, 256, 32)
            lamim = smallv(l, 288, 32)
            logdt = smallv(l, 320, 32)
            t = sp_t
            K = lambda i: ("spt", i)
            S = ["small"]
            act(t[0][:], logdt, AF.Exp, S, [K(0)])
            tt(t[1][:], lamre, t[0][:], ALU.mult, S + [K(0)], [K(1)])
            act(t[1][:], t[1][:], AF.Exp, [K(1)], [K(1)])
            tt(t[2][:], lamim, t[0][:], ALU.mult, S + [K(0)], [K(2)])
            ts(t[2][:], t[2][:], 1.0 / TWO_PI, None, ALU.mult, None, [K(2)], [K(2)])

            def sin_of(dst, kdst, shift):
                ts(t[3][:], t[2][:], shift, None, ALU.add, None, [K(2)], [K(3)])
                cp("dve", sp_i[:], t[3][:], [K(3)], ["spi"])
                cp("dve", t[4][:], sp_i[:], ["spi"], [K(4)])
                tt(t[3][:], t[3][:], t[4][:], ALU.subtract, [K(3), K(4)], [K(3)])
                ts(t[4][:], t[3][:], 0.5, None, ALU.is_gt, None, [K(3)], [K(4)])
                tt(t[3][:], t[3][:], t[4][:], ALU.subtract, [K(3), K(4)], [K(3)])
                ts(t[4][:], t[3][:], -0.5, None, ALU.is_lt, None, [K(3)], [K(4)])
                tt(t[3][:], t[3][:], t[4][:], ALU.add, [K(3), K(4)], [K(3)])
                act(dst, t[3][:], AF.Sin, [K(3)], [kdst], scale=TWO_PI)

            cp("dve", pw_rho[:, l, :], t[1][:], [K(1)], [("rho", l)])
            sin_of(t[5][:], K(5), 0.0)
            sin_of(t[6][:], K(6), 0.25)
            lre = pw[:, l, :, 0, 0]
            lim = pw[:, l, :, 0, 1]
            lnim = pw[:, l, :, 0, 2]
            PWK = ("pw", l)
            tt(lre, t[1][:], t[6][:], ALU.mult, [K(1), K(6)], [PWK])
            tt(lim, t[1][:], t[5][:], ALU.mult, [K(1), K(5)], [PWK])
            ts(lnim, lim, -1.0, None, ALU.mult, None, [PWK], [PWK])
            for lv in range(1, NLV):
                a_re = pw[:, l, :, lv - 1, 0]
                a_im = pw[:, l, :, lv - 1, 1]
                tt(t[7][:], a_re, a_re, ALU.mult, [PWK], [K(7)])
                tt(t[8][:], a_im, a_im, ALU.mult, [PWK], [K(8)])
                tt(pw[:, l, :, lv, 0], t[7][:], t[8][:], ALU.subtract, [K(7), K(8)], [PWK])
                tt(t[7][:], a_re, a_im, ALU.mult, [PWK], [K(7)])
                ts(pw[:, l, :, lv, 1], t[7][:], 2.0, None, ALU.mult, None, [K(7)], [PWK])
                ts(pw[:, l, :, lv, 2], t[7][:], -2.0, None, ALU.mult, None, [K(7)], [PWK])
            UK = "upw"
            cp("dve", upw[:, :, 0, 0], t[6][:], [K(6)], [UK])
            cp("dve", upw[:, :, 0, 1], t[5][:], [K(5)], [UK])
            ts(upw[:, :, 0, 2], t[5][:], -1.0, None, ALU.mult, None, [K(5)], [UK])
            for m_ in range(1, 9):
                a_re = upw[:, :, m_ - 1, 0]
                a_im = upw[:, :, m_ - 1, 1]
                tt(t[12][:], a_re, a_re, ALU.mult, [UK], [K(12)])
                tt(t[13][:], a_im, a_im, ALU.mult, [UK], [K(13)])
                tt(upw[:, :, m_, 0], t[12][:], t[13][:], ALU.subtract, [K(12), K(13)], [UK])
                tt(t[12][:], a_re, a_im, ALU.mult, [UK], [K(12)])
                ts(upw[:, :, m_, 1], t[12][:], 2.0, None, ALU.mult, None, [K(12)], [UK])
                ts(upw[:, :, m_, 2], t[12][:], -2.0, None, ALU.mult, None, [K(12)], [UK])
            for j in range(32):
                tb = x_sb[:, 12 + 2 * (j % 2):14 + 2 * (j % 2), :]
                TK = ("tabst", j % 2)
                cp("dve", tb[:, 0, 0:1], upw[:, j, 0, 0:1], [UK], [TK])
                cp("dve", tb[:, 1, 0:1], upw[:, j, 0, 1:2], [UK], [TK])
                n_ = 1
                m_ = 0
                while n_ < NBMAX:
                    ar = upw[:, j, m_, 0:1]
                    ai = upw[:, j, m_, 1:2]
                    nai = upw[:, j, m_, 2:3]
                    ts(tb[:, 0, n_:2 * n_], tb[:, 0, 0:n_], ar, None, ALU.mult, None, [TK, UK], [TK])
                    stt(tb[:, 0, n_:2 * n_], tb[:, 1, 0:n_], nai, tb[:, 0, n_:2 * n_], ALU.mult, ALU.add, [TK, UK], [TK])
                    ts(tb[:, 1, n_:2 * n_], tb[:, 1, 0:n_], ar, None, ALU.mult, None, [TK, UK], [TK])
                    stt(tb[:, 1, n_:2 * n_], tb[:, 0, 0:n_], ai, tb[:, 1, n_:2 * n_], ALU.mult, ALU.add, [TK, UK], [TK])
                    n_ *= 2
                    m_ += 1
                dma("sp", tab_d[l * 32 + j].rearrange("p (c t) -> p c t", c=2), tb, [TK], [("tabd", l, j)], key=("tabw", j % 2))
            ts(t[7][:], lre, -1.0, None, ALU.add, None, [PWK], [K(7)])
            tt(t[8][:], lamre, lamre, ALU.mult, S, [K(8)])
            tt(t[9][:], lamim, lamim, ALU.mult, S, [K(9)])
            tt(t[8][:], t[8][:], t[9][:], ALU.add, [K(8), K(9)], [K(8)])
            P.op("dve", lambda e, o=t[8][:]: e.reciprocal(o, o), [K(8)], [K(8)])
            tt(t[9][:], t[7][:], lamre, ALU.mult, [K(7)] + S, [K(9)])
            tt(t[10][:], lim, lamim, ALU.mult, [PWK] + S, [K(10)])
            tt(t[9][:], t[9][:], t[10][:], ALU.add, [K(9), K(10)], [K(9)])
            tt(t[9][:], t[9][:], t[8][:], ALU.mult, [K(9), K(8)], [K(9)])
            tt(t[10][:], lim, lamre, ALU.mult, [PWK] + S, [K(10)])
            tt(t[11][:], t[7][:], lamim, ALU.mult, [K(7)] + S, [K(11)])
            tt(t[10][:], t[10][:], t[11][:], ALU.subtract, [K(10), K(11)], [K(10)])
            tt(t[10][:], t[10][:], t[8][:], ALU.mult, [K(10), K(8)], [K(10)])
            dma("sp", s5m_sb, s5m_d[l], (), ["s5m"], key=("s5m",))
            bre = s5m_sb[:, 0]
            bim = s5m_sb[:, 1]
            for hlf in range(2):
                js = slice(hlf * 16, hlf * 16 + 16)
                s0 = rbuf[0][:, :].rearrange("p (j m) -> p j m", m=32)
                s1 = rbuf[1][:, :].rearrange("p (j m) -> p j m", m=32)
                cre_h = t[9][:, js].rearrange("p (j o) -> p j o", o=1).to_broadcast([128, 16, 32])
                cim_h = t[10][:, js].rearrange("p (j o) -> p j o", o=1).to_broadcast([128, 16, 32])
                tt(s0, bre[:, js, :], cre_h, ALU.mult, ["s5m", K(9)], [("rb", 0)])
                tt(s1, bim[:, js, :], cim_h, ALU.mult, ["s5m", K(10)], [("rb", 1)])
                tt(bbar[:, 0, js, :], s0, s1, ALU.subtract, [("rb", 0), ("rb", 1)], ["bbar"])
                tt(s0, bim[:, js, :], cre_h, ALU.mult, ["s5m", K(9)], [("rb", 0)])
                tt(s1, bre[:, js, :], cim_h, ALU.mult, ["s5m", K(10)], [("rb", 1)])
                tt(bbar[:, 1, js, :], s0, s1, ALU.add, [("rb", 0), ("rb", 1)], ["bbar"])
            for ri in range(2):
                for ct in range(8):
                    pb = 4 + (ri * 8 + ct) % 4
                    src = bbar[:, ri, ct * 4:(ct + 1) * 4, :].rearrange("p j m -> p (j m)")
                    P.op("pe", lambda e, o=psum[pb][:, 0:128], i=src: e.transpose(o, i, ident[:]),
                         ["bbar", "ident"], [PS(pb)])
                    cp("act", wbT[:, l, ri, ct, :], psum[pb][:, 0:128], [PS(pb)], [("wbT", l)])
            cp("act", cb[:, l, 0], s5m_sb[:, 2], ["s5m"], [("cb", l)])
            ts(cb[:, l, 1], s5m_sb[:, 3], -1.0, None, ALU.mult, None, ["s5m"], [("cb", l)])
            rl = smallv(l, 128, 8)
            act(rgc[:, l, 0, :], rl, AF.Exp, S, [("rgc", l)], scale=-1.0)
            act(rgc[:, l, 0, :], rgc[:, l, 0, :], AF.Ln, [("rgc", l)], [("rgc", l)], bias=1.0)
            ts(rgc[:, l, 1, :], rgc[:, l, 0, :], -16.0, None, ALU.mult, None, [("rgc", l)], [("rgc", l)])
            ts(rgc[:, l, 0, :], rgc[:, l, 0, :], -8.0, None, ALU.mult, None, [("rgc", l)], [("rgc", l)])

        stage(1)
        blocks = []
        for b in range(n_prompt_blocks):
            blocks.append(dict(c0=b * 512, NB=512, nseq=1, T=512, samp=False, last=(b == SEQ // 512 - 1)))
        blocks.append(dict(c0=SEQ, NB=NSAMP * TS, nseq=NSAMP, T=TS, samp=True, last=True))

        def v3(ap, blk):
            return ap.rearrange("p (q t) -> p q t", t=blk["T"])

        def modb(idx_tensor, idx, ft, blk):
            s0, s1 = (1, 1 + NSAMP) if blk["samp"] else (0, 1)
            return idx_tensor[:, idx, ft, s0:s1, :].to_broadcast([128, blk["nseq"], blk["T"]])

        def gelu_from(src32, skey, out_bf, okeys):
            t1, k1 = T32()
            act(t1[:, 0:NBc[0]], src32, AF.Square, [skey], [k1])
            ts(t1[:, 0:NBc[0]], t1[:, 0:NBc[0]], 0.044715, 1.0, ALU.mult, ALU.add, [k1], [k1])
            tt(t1[:, 0:NBc[0]], t1[:, 0:NBc[0]], src32, ALU.mult, [k1, skey], [k1])
            act(t1[:, 0:NBc[0]], t1[:, 0:NBc[0]], AF.Sigmoid, [k1], [k1], scale=GELU_K)
            tt(out_bf, src32, t1[:, 0:NBc[0]], ALU.mult, [skey, k1], okeys)

        NBc = [512]

        def rmsnorm_mod(l, which, blk, want_router=False):
            NB = blk["NB"]
            shk = 0 if which == 0 else 3
            pb = 0
            for ft in range(FT):
                tq, kq = T32()
                act(tq[:, 0:NB], x_sb[:, ft, 0:NB], AF.Square, [("x", ft)], [kq])
                mm(psum[pb][:, 0:NB], ones_f[:], tq[:, 0:NB], ft == 0, ft == FT - 1, ["ones", kq], [PS(pb)])
            rstd = rbuf[10]
            act(rstd[:, 0:NB], psum[pb][:, 0:NB], AF.Ln, [PS(pb)], [("rb", 10)], bias=1e-6, scale=1.0 / D)
            act(rstd[:, 0:NB], rstd[:, 0:NB], AF.Exp, [("rb", 10)], [("rb", 10)], scale=-0.5)
            for ft in range(FT):
                tq, kq = T32()
                tt(tq[:, 0:NB], x_sb[:, ft, 0:NB], rstd[:, 0:NB], ALU.mult, [("x", ft), ("rb", 10)], [kq])
                tt(v3(tq[:, 0:NB], blk), v3(tq[:, 0:NB], blk), modb(mod_sb, l * 6 + (1 if which == 0 else 4), ft, blk), ALU.mult,
                   [kq, ("mod", l, 1 if which == 0 else 4)], [kq])
                if want_router:
                    tt(v3(tq[:, 0:NB], blk), v3(tq[:, 0:NB], blk), modb(mod_sb, l * 6 + shk, ft, blk), ALU.add,
                       [kq, ("mod", l, shk)], [kq])
                    mm(psum[1][0:8, 0:NB], router_sb[:, ft, :], tq[:, 0:NB], ft == 0, ft == FT - 1,
                       ["router", kq], [PS(1)])
                    cp("act", h_sb[:, ft, 0:NB], tq[:, 0:NB], [kq], [("h", ft)])
                else:
                    tt(v3(h_sb[:, ft, 0:NB], blk), v3(tq[:, 0:NB], blk), modb(mod_sb, l * 6 + shk, ft, blk), ALU.add,
                       [kq, ("mod", l, shk)], [("h", ft)])

        def proj_tile(w_ap, kchunks, rhs_fn, rhs_keys, pb, NB, tag):
            slab, skey = load_slab(w_ap, kchunks, tag)
            for k in range(kchunks):
                mm(psum[pb][:, 0:NB], slab[:, k, :], rhs_fn(k), k == 0, k == kchunks - 1,
                   [skey] + [rhs_keys(k)], [PS(pb)])

        H_ALL = [("h", f) for f in range(FT)]

        def mixer(l, blk):
            NB, nseq, T = blk["NB"], blk["nseq"], blk["T"]
            NBc[0] = NB
            samp = blk["samp"]
            hk = lambda k: ("h", k)
            hf = lambda k: h_sb[:, k, 0:NB]
            for ct in range(8):
                proj_tile(w_in_d[l, :, ct * 128:(ct + 1) * 128], 16, hf, hk, 0, NB, ("in", l, ct))
                stage(27)
                u32 = rbuf[0]
                cp("act", u32[:, 0:NB], psum[0][:, 0:NB], [PS(0)], [("rb", 0)])
                stage(28)
                for jj in range(4):
                    ts(um[jj][:, 0:NB], psum[0][:, 0:NB], pmask[:, jj:jj + 1], None, ALU.mult, None,
                       [PS(0), "pmask"], [("um", jj)])
                stage(21)
                for jj in range(4):
                    for ri in range(2):
                        cp("act", wcp[jj * 2 + ri][:, 32 * jj:32 * jj + 32], cb[:, l, ri, ct * 4 + jj, :],
                           [("cb", l)], [("wcp", jj * 2 + ri)])
                stage(22)
                PWK = ("pw", l)

                def s5_front(jj):
                    par = jj % 2
                    j = ct * 4 + jj
                    pbs = (1, 2) if par == 0 else (4, 5)
                    dma("sp", tabb[par][:, :, :], tab_d[l * 32 + j].rearrange("p (c t) -> p c t", c=2),
                        [("tabd", l, j)], [("tabb", par)], key=("tabl", par))
                    for ri in range(2):
                        mm(psum[pbs[ri]][:, 0:NB], wbT[:, l, ri, ct, :], um[jj][:, 0:NB], True, True,
                           [("wbT", l), ("um", jj)], [PS(pbs[ri])])

                def s5_scan(jj):
                    par = jj % 2
                    j = ct * 4 + jj
                    pbs = (1, 2) if par == 0 else (4, 5)
                    TBK = ("tabb", par)
                    R = lambda i: rbuf[i][:, 0:NB]
                    RK = lambda i: ("rb", i)
                    if samp:
                        cv = tabb[par][:, 0:1, 0:T].to_broadcast([128, nseq, T])
                        sv = tabb[par][:, 1:2, 0:T].to_broadcast([128, nseq, T])
                    else:
                        cv = tabb[par][:, 0:1, 0:T]
                        sv = tabb[par][:, 1:2, 0:T]
                    bre = v3(psum[pbs[0]][:, 0:NB], blk)
                    bim = v3(psum[pbs[1]][:, 0:NB], blk)
                    rho = pw_rho[:, l, j:j + 1]
                    tt(v3(R(2), blk), cv, bre, ALU.mult, [TBK, PS(pbs[0])], [RK(2)])
                    tt(v3(R(3), blk), sv, bim, ALU.mult, [TBK, PS(pbs[1])], [RK(3)])
                    tt(R(4), R(2), R(3), ALU.add, [RK(2), RK(3)], [RK(4)])
                    tt(v3(R(2), blk), cv, bim, ALU.mult, [TBK, PS(pbs[1])], [RK(2)])
                    tt(v3(R(3), blk), sv, bre, ALU.mult, [TBK, PS(pbs[0])], [RK(3)])
                    tt(R(5), R(2), R(3), ALU.subtract, [RK(2), RK(3)], [RK(5)])
                    if samp:
                        ts(v3(R(8), blk), mask01[:], rho, None, ALU.mult, None, ["mask01", ("rho", l)], [RK(8)])
                        g4 = v3(R(4), blk)
                        g5 = v3(R(5), blk)
                        stt(g4[:, :, 0], s5in[:, l, 0, j, :], rho, g4[:, :, 0], ALU.mult, ALU.add,
                            [("s5in", j), ("rho", l), RK(4)], [RK(4)])
                        stt(g5[:, :, 0], s5in[:, l, 1, j, :], rho, g5[:, :, 0], ALU.mult, ALU.add,
                            [("s5in", j), ("rho", l), RK(5)], [RK(5)])
                        ini_re = ini_im = 0.0
                        ikeys = []
                    else:
                        ts(R(8), ones512[:, 0:NB], rho, None, ALU.mult, None, ["ones512", ("rho", l)], [RK(8)])
                        ini_re = s5car[:, l, 0, j, :]
                        ini_im = s5car[:, l, 1, j, :]
                        ikeys = [("s5car", j)]
                    P.op("dve", lambda e, o=R(6), a=R(8), b=R(4), i=ini_re:
                         e.tensor_tensor_scan(out=o, data0=a, data1=b, initial=i, op0=ALU.mult, op1=ALU.add),
                         [RK(8), RK(4)] + ikeys, [RK(6)])
                    P.op("dve", lambda e, o=R(7), a=R(8), b=R(5), i=ini_im:
                         e.tensor_tensor_scan(out=o, data0=a, data1=b, initial=i, op0=ALU.mult, op1=ALU.add),
                         [RK(8), RK(5)] + ikeys, [RK(7)])
                    g6 = v3(R(6), blk)
                    g7 = v3(R(7), blk)
                    tt(v3(R(2), blk), cv, g6, ALU.mult, [TBK, RK(6)], [RK(2)])
                    tt(v3(R(3), blk), sv, g7, ALU.mult, [TBK, RK(7)], [RK(3)])
                    tt(R(4), R(2), R(3), ALU.subtract, [RK(2), RK(3)], [RK(4)])
                    tt(v3(R(2), blk), cv, g7, ALU.mult, [TBK, RK(7)], [RK(2)])
                    tt(v3(R(3), blk), sv, g6, ALU.mult, [TBK, RK(6)], [RK(3)])
                    tt(R(5), R(2), R(3), ALU.add, [RK(2), RK(3)], [RK(5)])
                    return 0

                def s5_back(jj, cur):
                    par = jj % 2
                    j = ct * 4 + jj
                    R = lambda i: rbuf[i][:, 0:NB]
                    RK = lambda i: ("rb", i)
                    hb = hbfs[par]
                    hbk = [("hbf", par, 0), ("hbf", par, 1)]
                    cp("act", hb[0][:, 0:NB], R(4), [RK(4)], [hbk[0]])
                    cp("act", hb[1][:, 0:NB], R(5), [RK(5)], [hbk[1]])
                    h4 = v3(R(4), blk)
                    h5 = v3(R(5), blk)
                    if samp:
                        cp("act", s5out[:, l, 0, j, :], h4[:, :, T - 1], [RK(4)], [("s5in", j)])
                        cp("act", s5out[:, l, 1, j, :], h5[:, :, T - 1], [RK(5)], [("s5in", j)])
                    else:
                        cp("act", s5car[:, l, 0, j, :], h4[:, :, T - 1], [RK(4)], [("s5car", j)])
                        cp("act", s5car[:, l, 1, j, :], h5[:, :, T - 1], [RK(5)], [("s5car", j)])
                    for ri in range(2):
                        mm(psum[3][:, 0:NB], wcp[jj * 2 + ri][:], hb[ri][:, 0:NB], jj == 0 and ri == 0,
                           jj == 3 and ri == 1, [("wcp", jj * 2 + ri), hbk[ri]], [PS(3)])

                for jj in range(4):
                    s5_front(jj)
                    s5_scan(jj)
                    s5_back(jj, 0)
                stage(26)
                ypre = rbuf[1]
                stt(ypre[:, 0:NB], u32[:, 0:NB], small_sb[:, l, 152 + ct:153 + ct], psum[3][:, 0:NB], ALU.mult, ALU.add,
                    [("rb", 0), "small", PS(3)], [("rb", 1)])
                gelu_from(ypre[:, 0:NB], ("rb", 1), mix_sb[:, ct, 0:NB], [("mix", ct)])
            stage(3)
            for ct in range(8):
                pb = 1 + ct % 2
                proj_tile(w_glu_d[l, :, ct * 128:(ct + 1) * 128], 8, lambda k: mix_sb[:, k, 0:NB], lambda k: ("mix", k), pb, NB, ("glu", l, ct))
                tg, kg = T32()
                act(tg[:, 0:NB], psum[pb][:, 0:NB], AF.Sigmoid, [PS(pb), "small"], [kg], bias=small_sb[:, l, 80 + ct:81 + ct])
                tt(mix_sb[:, 8 + ct, 0:NB], mix_sb[:, ct, 0:NB], tg[:, 0:NB], ALU.mult, [("mix", ct), kg], [("mix", 8 + ct)])

            stage(4)
            HX = 3
            for j in range(8):
                proj_tile(w_in_d[l, :, 1024 + j * 128:1024 + (j + 1) * 128], 16, hf, hk, 4, NB, ("in", l, 8 + j))
                proj_tile(w_in_d[l, :, 2048 + j * 128:2048 + (j + 1) * 128], 16, hf, hk, 5, NB, ("in", l, 16 + j))
                xe = xrext[:, 0:nseq * (T + HX)].rearrange("p (q t) -> p q t", t=T + HX)
                XK = "xrext"
                cp("act", xe[:, :, HX:HX + T], v3(psum[4][:, 0:NB], blk), [PS(4)], [XK])
                if samp:
                    cp("dve", xe[:, :, 0:HX], cvin[:, l, j, :, :], ["cvin", XK], [XK])
                else:
                    cp("dve", xe[:, :, 0:HX], cvtail[:, l, j, :, :], ["cvtail", XK], [XK])
                gy32 = rbuf[2]
                cp("act", gy32[:, 0:NB], psum[5][:, 0:NB], [PS(5)], [("rb", 2)])
                gelu_from(gy32[:, 0:NB], ("rb", 2), bft[0][:, 0:NB], [("bft", 0)])
                xc = rbuf[3]
                xcv = v3(xc[:, 0:NB], blk)
                cw = lambda k: small_sb[:, l, 88 + k * 8 + j:89 + k * 8 + j]
                ts(xcv, xe[:, :, 0:T], cw(0), small_sb[:, l, 120 + j:121 + j], ALU.mult, ALU.add, [XK, "small"], [("rb", 3)])
                for k in range(1, 4):
                    stt(xcv, xe[:, :, k:k + T], cw(k), xcv, ALU.mult, ALU.add, [XK, "small", ("rb", 3)], [("rb", 3)])
                if samp:
                    cp("dve", cvout[:, l, j, :, :], xe[:, :, T:T + HX], [XK], ["cvin"])
                else:
                    cp("dve", cvtail[:, l, j, :, :], xe[:, :, T:T + HX], [XK], ["cvtail"])
                cp("act", bft[1][:, 0:NB], xc[:, 0:NB], [("rb", 3)], [("bft", 1)])
                mm(psum[6][:, 0:NB], rgw_sb[:, l, 0, j, :], bft[1][:, 0:NB], True, True, ["rgw", ("bft", 1)], [PS(6)])
                mm(psum[7][:, 0:NB], rgw_sb[:, l, 1, j, :], bft[1][:, 0:NB], True, True, ["rgw", ("bft", 1)], [PS(7)])
                r32, i32, a32, e2 = rbuf[4], rbuf[5], rbuf[6], rbuf[7]
                act(r32[:, 0:NB], psum[6][:, 0:NB], AF.Sigmoid, [PS(6), "small"], [("rb", 4)], bias=small_sb[:, l, 136 + j:137 + j])
                act(i32[:, 0:NB], psum[7][:, 0:NB], AF.Sigmoid, [PS(7), "small"], [("rb", 5)], bias=small_sb[:, l, 144 + j:145 + j])
                act(a32[:, 0:NB], r32[:, 0:NB], AF.Exp, [("rb", 4), ("rgc", l)], [("rb", 6)], scale=rgc[:, l, 0, j:j + 1])
                act(e2[:, 0:NB], r32[:, 0:NB], AF.Exp, [("rb", 4), ("rgc", l)], [("rb", 7)], scale=rgc[:, l, 1, j:j + 1])
                act(e2[:, 0:NB], e2[:, 0:NB], AF.Ln, [("rb", 7)], [("rb", 7)], bias=1.0, scale=-1.0)
                act(e2[:, 0:NB], e2[:, 0:NB], AF.Exp, [("rb", 7)], [("rb", 7)], scale=0.5)
                bx = rbuf[8]
                tt(bx[:, 0:NB], i32[:, 0:NB], xc[:, 0:NB], ALU.mult, [("rb", 5), ("rb", 3)], [("rb", 8)])
                tt(bx[:, 0:NB], bx[:, 0:NB], e2[:, 0:NB], ALU.mult, [("rb", 8), ("rb", 7)], [("rb", 8)])
                a3 = v3(a32[:, 0:NB], blk)
                b3 = v3(bx[:, 0:NB], blk)
                if samp:
                    h0 = rgin[:, l, j, :]
                    h0k = ["rgin"]
                else:
                    h0 = rgcar[:, l, j, :]
                    h0k = ["rgcar"]
                tcar = car4[:, 0, 0:nseq]
                tt(tcar, a3[:, :, 0], h0, ALU.mult, [("rb", 6)] + h0k, ["car4"])
                tt(b3[:, :, 0], b3[:, :, 0], tcar, ALU.add, [("rb", 8), "car4"], [("rb", 8)])
                memset("dve", a3[:, :, 0], 0.0, [("rb", 6)])
                hh = rbuf[9]
                P.op("dve", lambda e, o=hh[:, 0:NB], a=a32[:, 0:NB], b=bx[:, 0:NB]:
                     e.tensor_tensor_scan(out=o, data0=a, data1=b, initial=0.0, op0=ALU.mult, op1=ALU.add),
                     [("rb", 6), ("rb", 8)], [("rb", 9)])
                h3 = v3(hh[:, 0:NB], blk)
                if samp:
                    cp("dve", rgout[:, l, j, :], h3[:, :, T - 1], [("rb", 9)], ["rgin"])
                else:
                    cp("dve", rgcar[:, l, j, :], h3[:, :, T - 1], [("rb", 9)], ["rgcar"])
                tt(mix_sb[:, 16 + j, 0:NB], hh[:, 0:NB], bft[0][:, 0:NB], ALU.mult, [("rb", 9), ("bft", 0)], [("mix", 16 + j)])

            stage(5)
            for f in range(FT):
                pb0 = 4 * (f % 2)
                proj_tile(w_gate_d[l, :, f * 128:(f + 1) * 128], 16, hf, hk, pb0, NB, ("ga", l, f))
                proj_tile(w_gate_d[l, :, D + f * 128:D + (f + 1) * 128], 16, hf, hk, pb0 + 1, NB, ("gb", l, f))
                proj_tile(w_bs_d[l, :, f * 128:(f + 1) * 128], 8, lambda k: mix_sb[:, 8 + k, 0:NB], lambda k: ("mix", 8 + k), pb0 + 2, NB, ("bs", l, f))
                proj_tile(w_br_d[l, :, f * 128:(f + 1) * 128], 8, lambda k: mix_sb[:, 16 + k, 0:NB], lambda k: ("mix", 16 + k), pb0 + 3, NB, ("br", l, f))
                sa, ka = T32()
                sb_, kb = T32()
                act(sa[:, 0:NB], psum[pb0][:, 0:NB], AF.Sigmoid, [PS(pb0), "small"], [ka], bias=small_sb[:, l, 48 + f:49 + f])
                act(sb_[:, 0:NB], psum[pb0 + 1][:, 0:NB], AF.Sigmoid, [PS(pb0 + 1), "small"], [kb], bias=small_sb[:, l, 64 + f:65 + f])
                tt(sa[:, 0:NB], sa[:, 0:NB], psum[pb0 + 2][:, 0:NB], ALU.mult, [ka, PS(pb0 + 2)], [ka])
                tt(sb_[:, 0:NB], sb_[:, 0:NB], psum[pb0 + 3][:, 0:NB], ALU.mult, [kb, PS(pb0 + 3)], [kb])
                tt(mrg(f, NB), sa[:, 0:NB], sb_[:, 0:NB], ALU.add, [ka, kb], [("rb", f // 2)])
            for f in range(FT):
                pb = f % 4
                proj_tile(w_out_d[l, :, f * 128:(f + 1) * 128], 16, lambda k: mrg(k, NB), lambda k: ("rb", k // 2), pb, NB, ("out", l, f))
                tq, kq = T32()
                tt(v3(tq[:, 0:NB], blk), v3(psum[pb][:, 0:NB], blk), modb(mod_sb, l * 6 + 2, f, blk), ALU.mult,
                   [PS(pb), ("mod", l, 2)], [kq])
                tt(x_sb[:, f, 0:NB], x_sb[:, f, 0:NB], tq[:, 0:NB], ALU.add, [("x", f), kq], [("x", f)])

        def ffn_half(l, blk, w1_ap, w3_ap, w2_ap, comb_ap, comb_key, wtag):
            NB = blk["NB"]
            hk = lambda k: ("h", k)
            hf = lambda k: h_sb[:, k, 0:NB]
            for t_ in range(24):
                pb = 2 * (t_ % 2)
                proj_tile(w1_ap[:, t_ * 128:(t_ + 1) * 128], 16, hf, hk, pb, NB, (wtag, 1, t_))
                proj_tile(w3_ap[:, t_ * 128:(t_ + 1) * 128], 16, hf, hk, pb + 1, NB, (wtag, 3, t_))
                tq, kq = T32()
                act(tq[:, 0:NB], psum[pb][:, 0:NB], AF.Silu, [PS(pb)], [kq])
                tt(mix_sb[:, t_, 0:NB], tq[:, 0:NB], psum[pb + 1][:, 0:NB], ALU.mult, [kq, PS(pb + 1)], [("mix", t_)])
            for f in range(FT):
                pb = 4 + f % 3
                s1, k1 = load_slab(w2_ap[0:2048, f * 128:(f + 1) * 128], 16, (wtag, 2, f, 0))
                s2, k2 = load_slab(w2_ap[2048:3072, f * 128:(f + 1) * 128], 8, (wtag, 2, f, 1))
                for k in range(24):
                    sl, sk = (s1, k1) if k < 16 else (s2, k2)
                    mm(psum[pb][:, 0:NB], sl[:, k % 16, :], mix_sb[:, k, 0:NB], k == 0, k == 23, [sk, ("mix", k)], [PS(pb)])
                tq, kq = T32()
                tt(v3(tq[:, 0:NB], blk), v3(psum[pb][:, 0:NB], blk), modb(mod_sb, l * 6 + 5, f, blk), ALU.mult,
                   [PS(pb), ("mod", l, 5)], [kq])
                if comb_ap is not None:
                    tt(tq[:, 0:NB], tq[:, 0:NB], comb_ap[:, 0:NB], ALU.mult, [kq, comb_key], [kq])
                tt(x_sb[:, f, 0:NB], x_sb[:, f, 0:NB], tq[:, 0:NB], ALU.add, [("x", f), kq], [("x", f)])

        def moe_router(blk):
            NB = blk["NB"]
            nsub = max(1, NB // 128)
            tk = min(128, NB)
            lg_sb = rbuf[0][0:8, :]
            combT = rbuf[1][0:8, :]
            cp("act", lg_sb[:, 0:NB], psum[1][0:8, 0:NB], [PS(1)], [("rb", 0)])
            for s in range(nsub):
                P.op("pe", lambda e, o=psum[2][0:tk, s * 8:(s + 1) * 8], i=lg_sb[:, s * tk:(s + 1) * tk]:
                     e.transpose(o, i, ident[0:8, 0:8]), [("rb", 0), "ident"], [PS(2)])
            cp("act", lgT[0:tk, 0:nsub, :], psum[2][0:tk, 0:nsub * 8].rearrange("p (s e) -> p s e", e=8), [PS(2)], ["lgT"])
            for s in range(nsub):
                P.op("dve", lambda e, o=mx8[0:tk, s, :], i=lgT[0:tk, s, :]: e.max(o, i), ["lgT"], ["mx8"])
                ts(cmb[0:tk, s, :], lgT[0:tk, s, :], mx8[0:tk, s, 0:1], None, ALU.subtract, None, ["lgT", "mx8"], ["cmb"])
                act(cmb[0:tk, s, :], cmb[0:tk, s, :], AF.Exp, ["cmb"], ["cmb"])
                ts(cmbt[0:tk, s, :], lgT[0:tk, s, :], mx8[0:tk, s, 1:2], None, ALU.is_ge, None, ["lgT", "mx8"], ["cmbt"])
                tt(cmb[0:tk, s, :], cmb[0:tk, s, :], cmbt[0:tk, s, :], ALU.mult, ["cmb", "cmbt"], ["cmb"])
                P.op("dve", lambda e, o=den[0:tk, s, :], i=cmb[0:tk, s, :]:
                     e.reduce_sum(o, i, axis=mybir.AxisListType.X), ["cmb"], ["den"])
                P.op("dve", lambda e, o=den[0:tk, s, :]: e.reciprocal(o, o), ["den"], ["den"])
                ts(cmb[0:tk, s, :], cmb[0:tk, s, :], den[0:tk, s, 0:1], None, ALU.mult, None, ["cmb", "den"], ["cmb"])
                P.op("pe", lambda e, o=psum[3][0:8, s * tk:(s + 1) * tk], i=cmb[0:tk, s, :]:
                     e.transpose(o, i, ident[0:tk, 0:tk]), ["cmb", "ident"], [PS(3)])
            cp("act", combT[:, 0:NB], psum[3][0:8, 0:NB], [PS(3)], [("rb", 1)])

        for bi, blk in enumerate(blocks):
            NB = blk["NB"]
            c0 = blk["c0"]
            dma("sp", x_sb[:, :, 0:NB], xT_d[:, :, c0:c0 + NB], (), [("x", f) for f in range(FT)] + ["s5m", "bbar", ("tabst", 0), ("tabst", 1)], key=("xin",))
            for l in range(DEPTH):
                rmsnorm_mod(l, 0, blk)
                stage(2)
                mixer(l, blk)
                stage(6)
                shared_pair = not blk["samp"]
                if l % 2 == 0:
                    rmsnorm_mod(l, 1, blk)
                else:
                    rmsnorm_mod(l, 1, blk, want_router=True)
                    moe_router(blk)
                XK_ALL = [("x", f) for f in range(FT)]
                if shared_pair:
                    dma("sp", xold_d.ap().rearrange("p (f n) -> p f n", n=512), x_sb[:, :, :], XK_ALL, ["xold"], key=("xoldw",))
                if l % 2 == 0:
                    for hh_ in range(1 if shared_pair else 2):
                        ffn_half(l, blk, f_w1_d[hh_], f_w3_d[hh_], f_w2_d[hh_], None, None, ("ffn", hh_))
                else:
                    for e_ in range(4 if shared_pair else 8):
                        cbuf = rbuf[2 + e_ % 2]
                        ck = ("rb", 2 + e_ % 2)
                        msk = rbuf[4 + e_ % 2][0:8, :]
                        mk_ = ("rb", 4 + e_ % 2)
                        ts(msk[:, 0:NB], rbuf[1][0:8, 0:NB], esel[0:8, e_:e_ + 1], None, ALU.mult, None,
                           [("rb", 1), "esel"], [mk_])
                        mm(psum[7][:, 0:NB], ones_f[0:8, :], msk[:, 0:NB], True, True, ["ones", mk_], [PS(7)])
                        cp("act", cbuf[:, 0:NB], psum[7][:, 0:NB], [PS(7)], [ck])
                        ffn_half(l, blk, m_w1_d[e_], m_w3_d[e_], m_w2_d[e_], cbuf, ck, ("moe", e_))
                if shared_pair:
                    dma("sp", ccin_d.ap().rearrange("p (f n) -> p f n", n=512), x_sb[:, :, :], XK_ALL, ["ccin"], key=("ccinw",))
                    P.op("pool", lambda e: e.collective_compute("AllReduce", ALU.add, replica_groups=PAIR_GROUPS,
                                                                  ins=[ccin_d.ap().opt()], outs=[ccout_d.ap().opt()]),
                         ["ccin"], ["ccout"], dma_key=("cc",), cc=True)
                    dma("sp", x_sb[:, :, :], ccout_d.ap().rearrange("p (f n) -> p f n", n=512), ["ccout"], XK_ALL, key=("ccoutr",))
                    for ft in range(FT):
                        tq, kq = T32()
                        dma("sp", tq[:, :], xold_d.ap()[:, ft * 512:(ft + 1) * 512], ["xold"], [kq], key=("xoldr", kq[1]))
                        tt(x_sb[:, ft, :], x_sb[:, ft, :], tq[:, :], ALU.subtract, [("x", ft), kq], [("x", ft)])
            pb = 0
            for ft in range(FT):
                tq, kq = T32()
                act(tq[:, 0:NB], x_sb[:, ft, 0:NB], AF.Square, [("x", ft)], [kq])
                mm(psum[pb][:, 0:NB], ones_f[:], tq[:, 0:NB], ft == 0, ft == FT - 1, ["ones", kq], [PS(pb)])
            rstd = rbuf[10]
            act(rstd[:, 0:NB], psum[pb][:, 0:NB], AF.Ln, [PS(pb)], [("rb", 10)], bias=1e-6, scale=1.0 / D)
            act(rstd[:, 0:NB], rstd[:, 0:NB], AF.Exp, [("rb", 10)], [("rb", 10)], scale=-0.5)
            for ft in range(FT):
                tt(x_sb[:, ft, 0:NB], x_sb[:, ft, 0:NB], rstd[:, 0:NB], ALU.mult, [("x", ft), ("rb", 10)], [("x", ft)])
                ts(x_sb[:, ft, 0:NB], x_sb[:, ft, 0:NB], small_sb[:, 0, 32 + ft:33 + ft], None, ALU.mult, None,
                   [("x", ft), "small"], [("x", ft)])
            dma("sp", yT_d[:, :, c0:c0 + NB], x_sb[:, :, 0:NB], [("x", f) for f in range(FT)], (), key=("out",))

    except _Stop:
        pass

    dma("sp", o_s5s_d.rearrange("l r p j q -> p l r j q"), s5out[:], [("s5in", j) for j in range(32)], (), key=("out",))
    dma("sp", o_s5p_d.rearrange("l r p j -> p l r j"), s5car[:, :, :, :, 0], [("s5car", j) for j in range(32)], (), key=("out",))
    dma("sp", o_rgs_d.rearrange("l p j q -> p l j q"), rgout[:], ["rgin"], (), key=("out",))
    dma("sp", o_rgp_d.rearrange("l p j -> p l j"), rgcar[:, :, :, 0], ["rgcar"], (), key=("out",))
    dma("sp", o_cvs_d.rearrange("l p j q k -> p l j q k"), cvout[:], ["cvin"], (), key=("out",))
    dma("sp", o_cvp_d.rearrange("l p j k -> p l j k"), cvtail[:, :, :, 0, :], ["cvtail"], (), key=("out",))

    P.emit(final_wait_keys=[("out",)])
    st.close()
    return nc


def _ft_layout(v):
    k = v.shape[-1] // 128
    return np.ascontiguousarray(v.reshape(k, 128).T)


def _prep_small(inp):
    out = np.zeros((DEPTH, 128, NS_SMALL), np.float32)
    for l in range(DEPTH):
        o = out[l]
        o[:, 0:16] = _ft_layout(inp["norm_mix"][l])
        o[:, 16:32] = _ft_layout(inp["norm_ffn"][l])
        o[:, 32:48] = _ft_layout(inp["norm_f"])
        o[:, 48:80] = _ft_layout(inp["b_gate"][l])
        o[:, 80:88] = _ft_layout(inp["s5_b_glu"][l])
        for k in range(4):
            o[:, 88 + k * 8:96 + k * 8] = _ft_layout(inp["rg_conv_w"][l, k])
        o[:, 120:128] = _ft_layout(inp["rg_conv_b"][l])
        o[:, 128:136] = _ft_layout(inp["rg_lam"][l])
        o[:, 136:144] = _ft_layout(inp["rg_b_a"][l].reshape(-1))
        o[:, 144:152] = _ft_layout(inp["rg_b_i"][l].reshape(-1))
        o[:, 152:160] = _ft_layout(inp["s5_d"][l].reshape(-1))
        o[:, 160:256] = _ft_layout(inp["b_ada"][l])
        sp = lambda a: np.ascontiguousarray(a.reshape(32, 2, 64).transpose(1, 2, 0).reshape(128, 32))
        o[:, 256:288] = sp(inp["s5_lam_re"][l])
        o[:, 288:320] = sp(inp["s5_lam_im"][l])
        o[:, 320:352] = sp(np.repeat(inp["s5_log_dt"][l][:, None], 64, axis=1))
    return out


def _prep_s5m(inp):
    out = np.zeros((DEPTH, 128, 4, 32, 32), np.float32)
    for l in range(DEPTH):
        for idx, name in enumerate(("s5_b_re", "s5_b_im")):
            b = inp[name][l].reshape(32, 2, 64, 16)
            for g2 in range(2):
                out[l, g2 * 64:(g2 + 1) * 64, idx, :, g2 * 16:(g2 + 1) * 16] = b[:, g2].transpose(1, 0, 2)
        for idx, name in enumerate(("s5_c_re", "s5_c_im")):
            c = inp[name][l].reshape(32, 2, 16, 64)
            for g2 in range(2):
                out[l, g2 * 64:(g2 + 1) * 64, 2 + idx, :, g2 * 16:(g2 + 1) * 16] = c[:, g2].transpose(2, 0, 1)
    return out


def _prep_rgw(inp):
    out = np.zeros((DEPTH, 128, 2, 8, 128), np.float32)
    for l in range(DEPTH):
        for a, name in enumerate(("rg_w_a", "rg_w_i")):
            w = inp[name][l]
            for j in range(8):
                for h2 in range(2):
                    out[l, h2 * 64:(h2 + 1) * 64, a, j, h2 * 64:(h2 + 1) * 64] = w[j * 2 + h2]
    return out


_NC_CACHE = {}


def kernel(**inp):
    inp = {k: np.asarray(v) for k, v in inp.items()}
    if "nc" not in _NC_CACHE:
        _NC_CACHE["nc"] = build_program()
    nc = _NC_CACHE["nc"]
    in_maps = _make_in_maps(inp)
    res = run_bass_kernel_spmd(nc, in_maps, core_ids=list(range(8)))
    return _assemble(res.results)


def _make_in_maps(inp):
    small = _prep_small(inp)
    s5m = _prep_s5m(inp)
    rgw = _prep_rgw(inp)
    router = np.ascontiguousarray(inp["moe_router"][0].reshape(FT, 128, 8).transpose(1, 0, 2))
    ident = np.eye(128, dtype=np.float32)
    pmask = np.zeros((128, 4), np.float32)
    for jj in range(4):
        pmask[32 * jj:32 * jj + 32, jj] = 1.0
    shared = dict(small=small, s5m=s5m, rgw=rgw, router=router, ident=ident, pmask=pmask)
    for k in ("w_ada", "w_in", "s5_w_glu", "w_gate", "w_br_s5", "w_br_rg", "w_out"):
        shared[k] = np.ascontiguousarray(inp[k], dtype=np.float32)
    half_maps = []
    for hf in range(2):
        order = [hf, 1 - hf]
        eorder = list(range(4 * hf, 4 * hf + 4)) + list(range(4 * (1 - hf), 4 * (1 - hf) + 4))
        es = np.zeros((8, 8), np.float32)
        for i, e in enumerate(eorder):
            es[e, i] = 1.0
        half_maps.append(dict(
            ffn_w1=np.ascontiguousarray(np.stack([inp["ffn_w1"][0][:, o * 3072:(o + 1) * 3072] for o in order])),
            ffn_w3=np.ascontiguousarray(np.stack([inp["ffn_w3"][0][:, o * 3072:(o + 1) * 3072] for o in order])),
            ffn_w2=np.ascontiguousarray(np.stack([inp["ffn_w2"][0][o * 3072:(o + 1) * 3072, :] for o in order])),
            moe_w1=np.ascontiguousarray(inp["moe_w1"][0][eorder]),
            moe_w3=np.ascontiguousarray(inp["moe_w3"][0][eorder]),
            moe_w2=np.ascontiguousarray(inp["moe_w2"][0][eorder]),
            esel=es))
    in_maps = []
    for c in range(8):
        b = c % 4
        qs = slice(c * NSAMP, (c + 1) * NSAMP)
        xtok = np.concatenate([inp["x_prompt"][b], inp["x_sample"][qs].reshape(NSAMP * TS, D)], axis=0)
        xT = np.ascontiguousarray(xtok.reshape(-1, FT, 128).transpose(2, 1, 0))
        cc = np.concatenate([inp["c_prompt"][b:b + 1], inp["c_sample"][qs]], axis=0)
        cT = np.ascontiguousarray(cc.reshape(-1, FT, 128).transpose(2, 1, 0))
        s5st = np.stack([inp["state_s5_re"][:, qs], inp["state_s5_im"][:, qs]], axis=1)
        s5st = s5st.reshape(DEPTH, 2, NSAMP, 32, 2, 64).transpose(0, 1, 4, 5, 3, 2).reshape(DEPTH, 2, 128, 32, NSAMP)
        rgst = inp["state_rglru"][:, qs].reshape(DEPTH, NSAMP, 8, 128).transpose(0, 3, 2, 1)
        cvst = inp["state_conv"][:, qs].reshape(DEPTH, NSAMP, 3, 8, 128).transpose(0, 4, 3, 1, 2)
        m = dict(shared)
        m.update(half_maps[c // 4])
        m.update(xT=xT, cT=cT, s5st=np.ascontiguousarray(s5st), rgst=np.ascontiguousarray(rgst),
                 cvst=np.ascontiguousarray(cvst))
        in_maps.append(m)
    return in_maps


def _assemble(R):
    B = 4
    y_prompt = np.zeros((B, SEQ, D), np.float32)
    y_sample = np.zeros((8 * NSAMP, TS, D), np.float32)
    p_s5_re = np.zeros((DEPTH, B, 64, 64), np.float32)
    p_s5_im = np.zeros_like(p_s5_re)
    p_rg = np.zeros((DEPTH, B, 1024), np.float32)
    p_conv = np.zeros((DEPTH, B, 3, 1024), np.float32)
    s_s5_re = np.zeros((DEPTH, 8 * NSAMP, 64, 64), np.float32)
    s_s5_im = np.zeros_like(s_s5_re)
    s_rg = np.zeros((DEPTH, 8 * NSAMP, 1024), np.float32)
    s_conv = np.zeros((DEPTH, 8 * NSAMP, 3, 1024), np.float32)
    for c in range(8):
        r = R[c]
        yT = np.asarray(r["yT"])
        ytok = yT.transpose(2, 1, 0).reshape(-1, D)
        qs = slice(c * NSAMP, (c + 1) * NSAMP)
        y_sample[qs] = ytok[SEQ:].reshape(NSAMP, TS, D)
        s5s = np.asarray(r["o_s5s"]).reshape(DEPTH, 2, 2, 64, 32, NSAMP)
        s5s = s5s.transpose(0, 1, 5, 4, 2, 3).reshape(DEPTH, 2, NSAMP, 64, 64)
        s_s5_re[:, qs] = s5s[:, 0]
        s_s5_im[:, qs] = s5s[:, 1]
        s_rg[:, qs] = np.asarray(r["o_rgs"]).transpose(0, 3, 2, 1).reshape(DEPTH, NSAMP, 1024)
        s_conv[:, qs] = np.asarray(r["o_cvs"]).transpose(0, 3, 4, 2, 1).reshape(DEPTH, NSAMP, 3, 1024)
        if c < B:
            y_prompt[c] = ytok[:SEQ]
            s5p = np.asarray(r["o_s5p"]).reshape(DEPTH, 2, 2, 64, 32)
            s5p = s5p.transpose(0, 1, 4, 2, 3).reshape(DEPTH, 2, 64, 64)
            p_s5_re[:, c] = s5p[:, 0]
            p_s5_im[:, c] = s5p[:, 1]
            p_rg[:, c] = np.asarray(r["o_rgp"]).transpose(0, 2, 1).reshape(DEPTH, 1024)
            p_conv[:, c] = np.asarray(r["o_cvp"]).transpose(0, 3, 2, 1).reshape(DEPTH, 3, 1024)
    return (y_prompt, y_sample, p_s5_re, p_s5_im, p_rg, p_conv, s_s5_re, s_s5_im, s_rg, s_conv)
```

```python
import contextlib
import math
import numpy as np
import concourse.bass as bass
import concourse.mybir as mybir
from concourse.bass_utils import run_bass_kernel_spmd

F32 = mybir.dt.float32
BF16 = mybir.dt.bfloat16
I32 = mybir.dt.int32
AF = mybir.ActivationFunctionType
ALU = mybir.AluOpType

D = 2048
FT = 16
SEQ = 2048
NSAMP = 16
TS = 4
DEPTH = 2
NS_SMALL = 352
TWO_PI = 2.0 * math.pi
GELU_K = 1.5957691216057308


class Prog:
    ENG = ("pe", "act", "dve", "pool", "sp")

    def __init__(self, nc):
        self.nc = nc
        self.ops = []
        self.last_writer = {}
        self.readers = {}
        self.dma_key_count = {}
        self.dma_key_waitall = set()

    def op(self, eng, fn, reads=(), writes=(), dma_key=None, wait_all=False, cc=False):
        ps_r = [k for k in reads if isinstance(k, tuple) and k and k[0] == "ps"]
        if ps_r:
            reads = [k for k in reads if k not in ps_r]
            writes = list(writes) + [k for k in ps_r if k not in writes]
        idx = len(self.ops)
        deps = set()
        war = set()
        for b in reads:
            w = self.last_writer.get(b)
            if w is not None:
                deps.add(w)
        for b in writes:
            w = self.last_writer.get(b)
            if w is not None:
                deps.add(w)
            for r in self.readers.get(b, {}).values():
                war.add(r)
        o = dict(eng=eng, fn=fn, deps=deps, war=war, dma_key=dma_key, signal=False, cc=cc)
        if dma_key is not None:
            self.dma_key_count[dma_key] = self.dma_key_count.get(dma_key, 0) + 1
            o["dma_n"] = self.dma_key_count[dma_key]
            if wait_all:
                self.dma_key_waitall.add(dma_key)
        self.ops.append(o)
        rk = eng if dma_key is None else ("dma", idx)
        for b in reads:
            self.readers.setdefault(b, {})[rk] = idx
        for b in writes:
            self.last_writer[b] = idx
            self.readers[b] = {}
        return idx

    def emit(self, final_wait_keys=()):
        nc = self.nc
        ops = self.ops
        for o in ops:
            nd = set()
            for d in o["deps"] | o["war"]:
                p = ops[d]
                if p["dma_key"] is None and o["dma_key"] is None and p["eng"] == o["eng"]:
                    if o["eng"] == "pe":
                        continue
                nd.add(d)
            o["deps"] = nd
            for d in nd:
                ops[d]["signal"] = True
        cnt = {e: 0 for e in self.ENG}
        for o in ops:
            if o["dma_key"] is not None:
                k = o["dma_key"]
                n = self.dma_key_count[k] if k in self.dma_key_waitall else o["dma_n"]
                o["sig"] = (("dma", k), (1 if o["cc"] else 16) * n)
            elif o["signal"]:
                cnt[o["eng"]] += 1
                o["sig"] = (("eng", o["eng"]), cnt[o["eng"]])
        per_eng = {e: [o for o in ops if o["eng"] == e] for e in self.ENG}
        sem_names = [("eng", e) for e in self.ENG] + [("dma", k) for k in self.dma_key_count]
        with contextlib.ExitStack() as st:
            sems = {}
            for i, sn in enumerate(sem_names):
                sems[sn] = st.enter_context(nc.semaphore("s%d" % i))
            block = st.enter_context(nc.Block())
            engobj = {"pe": "tensor", "act": "scalar", "dve": "vector", "pool": "gpsimd", "sp": "sync"}

            def run(e, eng):
                known = {}
                for o in per_eng[e]:
                    need = {}
                    for d in o["deps"]:
                        s, v = ops[d]["sig"]
                        if v > need.get(s, 0):
                            need[s] = v
                    for s, v in need.items():
                        if known.get(s, 0) < v:
                            eng.wait_ge(sems[s], v)
                            known[s] = v
                    ins = o["fn"](eng)
                    if o["cc"]:
                        ins.then_inc(sems[("dma", o["dma_key"])])
                    elif o["dma_key"] is not None:
                        ins.then_inc(sems[("dma", o["dma_key"])], 16)
                    elif o["signal"]:
                        ins.then_inc(sems[("eng", e)], 1)
                if e == "sp":
                    for k in final_wait_keys:
                        v = 16 * self.dma_key_count[k]
                        if known.get(("dma", k), 0) < v:
                            eng.wait_ge(sems[("dma", k)], v)

            for e in self.ENG:
                def mk(e):
                    def f(eng):
                        run(e, eng)
                    return f
                getattr(block, engobj[e])(mk(e))


class _Stop(Exception):
    pass


def build_program(n_prompt_blocks=SEQ // 512, stop_stage=None):
    nc = bass.Bass("TRN2", target_bir_lowering=False)

    def stage(n):
        if stop_stage is not None and n == stop_stage:
            raise _Stop()

    P = Prog(nc)
    st = contextlib.ExitStack()

    def din(name, shape):
        return nc.dram_tensor(name, list(shape), F32, kind="ExternalInput").ap()

    def dout(name, shape):
        return nc.dram_tensor(name, list(shape), F32, kind="ExternalOutput").ap()

    def sb(name, shape, dt=F32):
        return st.enter_context(nc.sbuf_tensor(name, list(shape), dt))

    NTOK = SEQ + NSAMP * TS
    xT_d = din("xT", [128, FT, NTOK])
    cT_d = din("cT", [128, FT, 1 + NSAMP])
    small_d = din("small", [DEPTH, 128, NS_SMALL])
    s5m_d = din("s5m", [DEPTH, 128, 4, 32, 32])
    rgw_d = din("rgw", [DEPTH, 128, 2, 8, 128])
    router_d = din("router", [128, FT, 8])
    ident_d = din("ident", [128, 128])
    pmask_d = din("pmask", [128, 4])
    s5st_d = din("s5st", [DEPTH, 2, 128, 32, NSAMP])
    rgst_d = din("rgst", [DEPTH, 128, 8, NSAMP])
    cvst_d = din("cvst", [DEPTH, 128, 8, NSAMP, 3])
    w_ada_d = din("w_ada", [DEPTH, D, 6 * D])
    w_in_d = din("w_in", [DEPTH, D, 3072])
    w_glu_d = din("s5_w_glu", [DEPTH, 1024, 1024])
    w_gate_d = din("w_gate", [DEPTH, D, 2 * D])
    w_bs_d = din("w_br_s5", [DEPTH, 1024, D])
    w_br_d = din("w_br_rg", [DEPTH, 1024, D])
    w_out_d = din("w_out", [DEPTH, D, D])
    f_w1_d = din("ffn_w1", [2, D, 3072])
    f_w3_d = din("ffn_w3", [2, D, 3072])
    f_w2_d = din("ffn_w2", [2, 3072, D])
    m_w1_d = din("moe_w1", [8, D, 3072])
    m_w3_d = din("moe_w3", [8, D, 3072])
    m_w2_d = din("moe_w2", [8, 3072, D])
    esel_d = din("esel", [8, 8])
    xold_d = nc.dram_tensor("xold", [128, FT * 512], F32)
    ccin_d = nc.dram_tensor("ccin", [128, FT * 512], F32)
    ccout_d = nc.dram_tensor("ccout", [128, FT * 512], F32)
    PAIR_GROUPS = [[0, 4], [1, 5], [2, 6], [3, 7]]

    yT_d = dout("yT", [128, FT, NTOK])
    o_s5s_d = dout("o_s5s", [DEPTH, 2, 128, 32, NSAMP])
    o_s5p_d = dout("o_s5p", [DEPTH, 2, 128, 32])
    o_rgs_d = dout("o_rgs", [DEPTH, 128, 8, NSAMP])
    o_rgp_d = dout("o_rgp", [DEPTH, 128, 8])
    o_cvs_d = dout("o_cvs", [DEPTH, 128, 8, NSAMP, 3])
    o_cvp_d = dout("o_cvp", [DEPTH, 128, 8, 3])

    NBMAX = 512
    x_sb = sb("x_sb", [128, FT, NBMAX])
    h_sb = sb("h_sb", [128, FT, NBMAX], BF16)
    mix_sb = sb("mix_sb", [128, 24, NBMAX], BF16)
    NSLAB = 3
    slabs = [sb("slab%d" % i, [128, 16, 128], BF16) for i in range(NSLAB)]
    NTMP = 4
    tmps = [sb("tmp%d" % i, [128, NBMAX]) for i in range(NTMP)]
    NR = 11
    rb_all = sb("rb_all", [128, NR, NBMAX])
    rbuf = [rb_all[:, i, :] for i in range(NR)]
    rb_bf = rb_all.bitcast(BF16)

    def mrg(f, NB):
        return rb_bf[:, f // 2, (f % 2) * NBMAX:(f % 2) * NBMAX + NB]
    xrext = sb("xrext", [128, NBMAX + 64])
    bft = [sb("bft%d" % i, [128, NBMAX], BF16) for i in range(2)]
    um = [sb("um%d" % i, [128, NBMAX], BF16) for i in range(4)]
    tabb = [sb("tabb%d" % i, [128, 2, NBMAX]) for i in range(2)]
    ones512 = sb("ones512", [128, NBMAX])
    mask01 = sb("mask01", [128, NSAMP, TS])
    upw = sb("upw", [128, 32, 9, 3])
    tab_d = nc.dram_tensor("tabscr", [DEPTH * 32, 128, 2 * NBMAX], F32).ap()
    bft2 = [sb("bft2%d" % i, [128, NBMAX], BF16) for i in range(2)]
    hbfs = [bft, bft2]
    wcp = [sb("wcp%d" % i, [128, 128], BF16) for i in range(8)]
    small_sb = sb("small_sb", [128, DEPTH, NS_SMALL])
    mod_sb = sb("mod_sb", [128, DEPTH * 6, FT, 1 + NSAMP, 1])
    cT_sb = sb("cT_sb", [128, FT, 1 + NSAMP])
    cs_bf = sb("cs_bf", [128, FT, 1 + NSAMP], BF16)
    ident = sb("ident_sb", [128, 128])
    router_sb = sb("router_sb", [128, FT, 8])
    s5m_sb = x_sb[:, 0:8, :].rearrange("p a b -> p (a b)").rearrange("p (i j m) -> p i j m", i=4, j=32)
    bbar = x_sb[:, 8:12, :].rearrange("p a b -> p (a b)").rearrange("p (i j m) -> p i j m", i=2, j=32)
    wbT = sb("wbT", [128, DEPTH, 2, 8, 128], BF16)
    cb = sb("cb", [128, DEPTH, 2, 32, 32], BF16)
    NLV = 9
    pw = sb("pw", [128, DEPTH, 32, NLV, 3])
    sp_t = [sb("spt%d" % i, [128, 32]) for i in range(14)]
    pw_rho = sb("pw_rho", [128, DEPTH, 32])
    sp_i = sb("spi", [128, 32], I32)
    rgw_sb = sb("rgw_sb", [128, DEPTH, 2, 8, 128], BF16)
    rgc = sb("rgc", [128, DEPTH, 2, 8])
    s5car = sb("s5car", [128, DEPTH, 2, 32, 1])
    rgcar = sb("rgcar", [128, DEPTH, 8, 1])
    cvtail = sb("cvtail", [128, DEPTH, 8, 1, 3])
    s5in = sb("s5in", [128, DEPTH, 2, 32, NSAMP])
    s5out = s5in
    rgin = sb("rgin", [128, DEPTH, 8, NSAMP])
    rgout = rgin
    cvin = sb("cvin", [128, DEPTH, 8, NSAMP, 3])
    cvout = cvin
    car4 = sb("car4", [128, 4, NSAMP])
    lgT = sb("lgT", [128, 4, 8])
    mx8 = sb("mx8", [128, 4, 8])
    cmb = sb("cmb", [128, 4, 8])
    cmbt = sb("cmbt", [128, 4, 8])
    den = sb("den", [128, 4, 1])
    ones_f = sb("ones_f", [128, 128])
    pmask = sb("pmask_sb", [128, 4])
    esel = sb("esel_sb", [8, 8])

    psum = [st.enter_context(nc.psum_tensor("ps%d" % i, [128, 512], F32)) for i in range(8)]

    def mm(out, lhsT, rhs, start, stop, reads, writes):
        P.op("pe", lambda e, a=out, b=lhsT, c=rhs, s=start, t=stop: e.matmul(a, lhsT=b, rhs=c, start=s, stop=t),
             reads, writes)

    def act(out, in_, func, reads, writes, bias=None, scale=None):
        kw = {}
        if bias is not None:
            kw["bias"] = bias
        if scale is not None:
            kw["scale"] = scale
        P.op("act", lambda e, a=out, b=in_, f=func, k=kw: e.activation(a, b, f, **k), reads, writes)

    def tt(out, a, b, op, reads, writes, eng="dve"):
        P.op(eng, lambda e, o=out, x=a, y=b, p=op: e.tensor_tensor(out=o, in0=x, in1=y, op=p), reads, writes)

    def ts(out, a, s1, s2, op0, op1, reads, writes, eng="dve"):
        if s2 is None:
            P.op(eng, lambda e, o=out, x=a, u=s1, p=op0: e.tensor_scalar(o, x, u, None, p), reads, writes)
        else:
            P.op(eng, lambda e, o=out, x=a, u=s1, v=s2, p=op0, q=op1: e.tensor_scalar(o, x, u, v, p, q), reads, writes)

    def stt(out, in0, scalar, in1, op0, op1, reads, writes, eng="dve"):
        P.op(eng, lambda e, o=out, x=in0, s=scalar, y=in1, p=op0, q=op1:
             e.scalar_tensor_tensor(out=o, in0=x, scalar=s, in1=y, op0=p, op1=q), reads, writes)

    def cp(eng, out, in_, reads, writes):
        if eng == "act":
            P.op("act", lambda e, o=out, i=in_: e.copy(o, i), reads, writes)
        else:
            P.op(eng, lambda e, o=out, i=in_: e.tensor_copy(o, i), reads, writes)

    def memset(eng, ap, val, writes):
        P.op(eng, lambda e, a=ap, v=val: e.memset(a, v), (), writes)

    def dma(q, out, in_, reads, writes, key, wait_all=False):
        P.op(q, lambda e, o=out, i=in_: e.dma_start(out=o, in_=i), reads, writes, dma_key=key, wait_all=wait_all)

    slab_ctr = [0]
    NSCR = 1056
    SCR_PER = 448
    wscrs = [nc.dram_tensor("wscr%d" % i, [SCR_PER, 128, 2048], BF16).ap() for i in range(3)]
    scr_ids = {}

    def load_slab(src_ap, K, tag=None):
        s = slab_ctr[0] % NSLAB
        slab_ctr[0] += 1
        key = ("slab", s)
        dst = slabs[s][:, 0:K, :]
        if tag is None:
            dma("pool", dst, src_ap.rearrange("(k p) m -> p k m", p=128), (), [key], key=("slabq", s))
            return slabs[s], key
        first = tag not in scr_ids
        if first:
            scr_ids[tag] = len(scr_ids)
        tid = scr_ids[tag]
        assert tid < 3 * SCR_PER
        scr_v = wscrs[tid // SCR_PER][tid % SCR_PER, :, 0:K * 128].rearrange("p (k m) -> p k m", m=128)
        if first:
            dma("pool", dst, src_ap.rearrange("(k p) m -> p k m", p=128), (), [key], key=("slabq", s))
            dma("sp", scr_v, dst, [key], [("scr", tid)], key=("wbq", s))
        else:
            dma("sp", dst, scr_v, [("scr", tid)], [key], key=("slabh", s))
        return slabs[s], key

    tmp_ctr = [0]

    def T32():
        i = tmp_ctr[0] % NTMP
        tmp_ctr[0] += 1
        return tmps[i], ("tmp", i)

    PS = lambda i: ("ps", i)

    C = "const"
    dma("sp", small_sb[:], small_d.rearrange("l p n -> p l n"), (), ["small"], key=C, wait_all=True)
    dma("sp", cT_sb[:], cT_d, (), ["cT"], key=C, wait_all=True)
    dma("sp", ident[:], ident_d, (), ["ident"], key=C, wait_all=True)
    dma("sp", pmask[:], pmask_d, (), ["pmask"], key=C, wait_all=True)
    dma("sp", esel[:], esel_d, (), ["esel"], key=C, wait_all=True)
    dma("sp", router_sb[:], router_d, (), ["router"], key=C, wait_all=True)
    dma("sp", s5in[:], s5st_d.rearrange("l r p j q -> p l r j q"), (), [("s5in", j) for j in range(32)], key=C, wait_all=True)
    dma("sp", rgin[:], rgst_d.rearrange("l p j q -> p l j q"), (), ["rgin"], key=C, wait_all=True)
    dma("sp", cvin[:], cvst_d.rearrange("l p j q k -> p l j q k"), (), ["cvin"], key=C, wait_all=True)
    dma("pool", rgw_sb[:], rgw_d.rearrange("l p a j m -> p l a j m"), (), ["rgw"], key=("rgwq",))
    memset("dve", ones_f[:], 1.0, ["ones"])
    memset("dve", ones512[:], 1.0, ["ones512"])
    memset("dve", mask01[:], 1.0, ["mask01"])
    memset("dve", mask01[:, :, 0:1], 0.0, ["mask01"])
    for i in range(8):
        memset("dve", wcp[i][:], 0.0, [("wcp", i)])
    memset("dve", s5car[:], 0.0, [("s5car", j) for j in range(32)])
    memset("dve", rgcar[:], 0.0, ["rgcar"])
    memset("dve", cvtail[:], 0.0, ["cvtail"])

    def smallv(l, c0, n):
        return small_sb[:, l, c0:c0 + n]

    try:
        act(cs_bf[:], cT_sb[:], AF.Silu, ["cT"], ["cs"])
        NSQ = 1 + NSAMP
        for l in range(DEPTH):
            for kind in range(6):
                pb = (l * 6 + kind) % 4
                for ft in range(FT):
                    col0 = (kind * FT + ft) * 128
                    slab, skey = load_slab(w_ada_d[l, :, col0:col0 + 128], 16)
                    for k in range(16):
                        mm(psum[pb][:, ft * NSQ:(ft + 1) * NSQ], slab[:, k, :], cs_bf[:, k, :], k == 0, k == 15,
                           [skey, "cs"], [PS(pb)])
                bias_b = small_sb[:, l, 160 + kind * FT:160 + (kind + 1) * FT].rearrange("p (f o) -> p f o", o=1) \
                    .to_broadcast([128, FT, NSQ])
                tt(mod_sb[:, l * 6 + kind, :, :, 0], psum[pb][:, 0:FT * NSQ].rearrange("p (f s) -> p f s", s=NSQ),
                   bias_b, ALU.add, [PS(pb), "small"], [("mod", l, kind)])
            for which, (kind, ncol) in enumerate(((1, 0), (4, 16))):
                tmpv = mod_sb[:, l * 6 + kind, :, :, 0]
                ts(tmpv, tmpv, 1.0, None, ALU.add, None, [("mod", l, kind)], [("mod", l, kind)])
                nb = small_sb[:, l, ncol:ncol + FT].rearrange("p (f o) -> p f o", o=1).to_broadcast([128, FT, NSQ])
                tt(tmpv, tmpv, nb, ALU.mult, [("mod", l, kind), "small"], [("mod", l, kind)])

        stage(0)
        for l in range(DEPTH):
            lamre = smallv(l, 256, 32)
            lamim = smallv(l, 288, 32)
            logdt = smallv(l, 320, 32)
            t = sp_t
            K = lambda i: ("spt", i)
            S = ["small"]
            act(t[0][:], logdt, AF.Exp, S, [K(0)])
            tt(t[1][:], lamre, t[0][:], ALU.mult, S + [K(0)], [K(1)])
            act(t[1][:], t[1][:], AF.Exp, [K(1)], [K(1)])
            tt(t[2][:], lamim, t[0][:], ALU.mult, S + [K(0)], [K(2)])
            ts(t[2][:], t[2][:], 1.0 / TWO_PI, None, ALU.mult, None, [K(2)], [K(2)])

            def sin_of(dst, kdst, shift):
                ts(t[3][:], t[2][:], shift, None, ALU.add, None, [K(2)], [K(3)])
                cp("dve", sp_i[:], t[3][:], [K(3)], ["spi"])
                cp("dve", t[4][:], sp_i[:], ["spi"], [K(4)])
                tt(t[3][:], t[3][:], t[4][:], ALU.subtract, [K(3), K(4)], [K(3)])
                ts(t[4][:], t[3][:], 0.5, None, ALU.is_gt, None, [K(3)], [K(4)])
                tt(t[3][:], t[3][:], t[4][:], ALU.subtract, [K(3), K(4)], [K(3)])
                ts(t[4][:], t[3][:], -0.5, None, ALU.is_lt, None, [K(3)], [K(4)])
                tt(t[3][:], t[3][:], t[4][:], ALU.add, [K(3), K(4)], [K(3)])
                act(dst, t[3][:], AF.Sin, [K(3)], [kdst], scale=TWO_PI)

            cp("dve", pw_rho[:, l, :], t[1][:], [K(1)], [("rho", l)])
            sin_of(t[5][:], K(5), 0.0)
            sin_of(t[6][:], K(6), 0.25)
            lre = pw[:, l, :, 0, 0]
            lim = pw[:, l, :, 0, 1]
            lnim = pw[:, l, :, 0, 2]
            PWK = ("pw", l)
            tt(lre, t[1][:], t[6][:], ALU.mult, [K(1), K(6)], [PWK])
            tt(lim, t[1][:], t[5][:], ALU.mult, [K(1), K(5)], [PWK])
            ts(lnim, lim, -1.0, None, ALU.mult, None, [PWK], [PWK])
            for lv in range(1, NLV):
                a_re = pw[:, l, :, lv - 1, 0]
                a_im = pw[:, l, :, lv - 1, 1]
                tt(t[7][:], a_re, a_re, ALU.mult, [PWK], [K(7)])
                tt(t[8][:], a_im, a_im, ALU.mult, [PWK], [K(8)])
                tt(pw[:, l, :, lv, 0], t[7][:], t[8][:], ALU.subtract, [K(7), K(8)], [PWK])
                tt(t[7][:], a_re, a_im, ALU.mult, [PWK], [K(7)])
                ts(pw[:, l, :, lv, 1], t[7][:], 2.0, None, ALU.mult, None, [K(7)], [PWK])
                ts(pw[:, l, :, lv, 2], t[7][:], -2.0, None, ALU.mult, None, [K(7)], [PWK])
            UK = "upw"
            cp("dve", upw[:, :, 0, 0], t[6][:], [K(6)], [UK])
            cp("dve", upw[:, :, 0, 1], t[5][:], [K(5)], [UK])
            ts(upw[:, :, 0, 2], t[5][:], -1.0, None, ALU.mult, None, [K(5)], [UK])
            for m_ in range(1, 9):
                a_re = upw[:, :, m_ - 1, 0]
                a_im = upw[:, :, m_ - 1, 1]
                tt(t[12][:], a_re, a_re, ALU.mult, [UK], [K(12)])
                tt(t[13][:], a_im, a_im, ALU.mult, [UK], [K(13)])
                tt(upw[:, :, m_, 0], t[12][:], t[13][:], ALU.subtract, [K(12), K(13)], [UK])
                tt(t[12][:], a_re, a_im, ALU.mult, [UK], [K(12)])
                ts(upw[:, :, m_, 1], t[12][:], 2.0, None, ALU.mult, None, [K(12)], [UK])
                ts(upw[:, :, m_, 2], t[12][:], -2.0, None, ALU.mult, None, [K(12)], [UK])
            for j in range(32):
                tb = x_sb[:, 12 + 2 * (j % 2):14 + 2 * (j % 2), :]
                TK = ("tabst", j % 2)
                cp("dve", tb[:, 0, 0:1], upw[:, j, 0, 0:1], [UK], [TK])
                cp("dve", tb[:, 1, 0:1], upw[:, j, 0, 1:2], [UK], [TK])
                n_ = 1
                m_ = 0
                while n_ < NBMAX:
                    ar = upw[:, j, m_, 0:1]
                    ai = upw[:, j, m_, 1:2]
                    nai = upw[:, j, m_, 2:3]
                    ts(tb[:, 0, n_:2 * n_], tb[:, 0, 0:n_], ar, None, ALU.mult, None, [TK, UK], [TK])
                    stt(tb[:, 0, n_:2 * n_], tb[:, 1, 0:n_], nai, tb[:, 0, n_:2 * n_], ALU.mult, ALU.add, [TK, UK], [TK])
                    ts(tb[:, 1, n_:2 * n_], tb[:, 1, 0:n_], ar, None, ALU.mult, None, [TK, UK], [TK])
                    stt(tb[:, 1, n_:2 * n_], tb[:, 0, 0:n_], ai, tb[:, 1, n_:2 * n_], ALU.mult, ALU.add, [TK, UK], [TK])
                    n_ *= 2
                    m_ += 1
                dma("sp", tab_d[l * 32 + j].rearrange("p (c t) -> p c t", c=2), tb, [TK], [("tabd", l, j)], key=("tabw", j % 2))
            ts(t[7][:], lre, -1.0, None, ALU.add, None, [PWK], [K(7)])
            tt(t[8][:], lamre, lamre, ALU.mult, S, [K(8)])
            tt(t[9][:], lamim, lamim, ALU.mult, S, [K(9)])
            tt(t[8][:], t[8][:], t[9][:], ALU.add, [K(8), K(9)], [K(8)])
            P.op("dve", lambda e, o=t[8][:]: e.reciprocal(o, o), [K(8)], [K(8)])
            tt(t[9][:], t[7][:], lamre, ALU.mult, [K(7)] + S, [K(9)])
            tt(t[10][:], lim, lamim, ALU.mult, [PWK] + S, [K(10)])
            tt(t[9][:], t[9][:], t[10][:], ALU.add, [K(9), K(10)], [K(9)])
            tt(t[9][:], t[9][:], t[8][:], ALU.mult, [K(9), K(8)], [K(9)])
            tt(t[10][:], lim, lamre, ALU.mult, [PWK] + S, [K(10)])
            tt(t[11][:], t[7][:], lamim, ALU.mult, [K(7)] + S, [K(11)])
            tt(t[10][:], t[10][:], t[11][:], ALU.subtract, [K(10), K(11)], [K(10)])
            tt(t[10][:], t[10][:], t[8][:], ALU.mult, [K(10), K(8)], [K(10)])
            dma("sp", s5m_sb, s5m_d[l], (), ["s5m"], key=("s5m",))
            bre = s5m_sb[:, 0]
            bim = s5m_sb[:, 1]
            for hlf in range(2):
                js = slice(hlf * 16, hlf * 16 + 16)
                s0 = rbuf[0][:, :].rearrange("p (j m) -> p j m", m=32)
                s1 = rbuf[1][:, :].rearrange("p (j m) -> p j m", m=32)
                cre_h = t[9][:, js].rearrange("p (j o) -> p j o", o=1).to_broadcast([128, 16, 32])
                cim_h = t[10][:, js].rearrange("p (j o) -> p j o", o=1).to_broadcast([128, 16, 32])
                tt(s0, bre[:, js, :], cre_h, ALU.mult, ["s5m", K(9)], [("rb", 0)])
                tt(s1, bim[:, js, :], cim_h, ALU.mult, ["s5m", K(10)], [("rb", 1)])
                tt(bbar[:, 0, js, :], s0, s1, ALU.subtract, [("rb", 0), ("rb", 1)], ["bbar"])
                tt(s0, bim[:, js, :], cre_h, ALU.mult, ["s5m", K(9)], [("rb", 0)])
                tt(s1, bre[:, js, :], cim_h, ALU.mult, ["s5m", K(10)], [("rb", 1)])
                tt(bbar[:, 1, js, :], s0, s1, ALU.add, [("rb", 0), ("rb", 1)], ["bbar"])
            for ri in range(2):
                for ct in range(8):
                    pb = 4 + (ri * 8 + ct) % 4
                    src = bbar[:, ri, ct * 4:(ct + 1) * 4, :].rearrange("p j m -> p (j m)")
                    P.op("pe", lambda e, o=psum[pb][:, 0:128], i=src: e.transpose(o, i, ident[:]),
                         ["bbar", "ident"], [PS(pb)])
                    cp("act", wbT[:, l, ri, ct, :], psum[pb][:, 0:128], [PS(pb)], [("wbT", l)])
            cp("act", cb[:, l, 0], s5m_sb[:, 2], ["s5m"], [("cb", l)])
            ts(cb[:, l, 1], s5m_sb[:, 3], -1.0, None, ALU.mult, None, ["s5m"], [("cb", l)])
            rl = smallv(l, 128, 8)
            act(rgc[:, l, 0, :], rl, AF.Exp, S, [("rgc", l)], scale=-1.0)
            act(rgc[:, l, 0, :], rgc[:, l, 0, :], AF.Ln, [("rgc", l)], [("rgc", l)], bias=1.0)
            ts(rgc[:, l, 1, :], rgc[:, l, 0, :], -16.0, None, ALU.mult, None, [("rgc", l)], [("rgc", l)])
            ts(rgc[:, l, 0, :], rgc[:, l, 0, :], -8.0, None, ALU.mult, None, [("rgc", l)], [("rgc", l)])

        stage(1)
        blocks = []
        for b in range(n_prompt_blocks):
            blocks.append(dict(c0=b * 512, NB=512, nseq=1, T=512, samp=False, last=(b == SEQ // 512 - 1)))
        blocks.append(dict(c0=SEQ, NB=NSAMP * TS, nseq=NSAMP, T=TS, samp=True, last=True))

        def v3(ap, blk):
            return ap.rearrange("p (q t) -> p q t", t=blk["T"])

        def modb(idx_tensor, idx, ft, blk):
            s0, s1 = (1, 1 + NSAMP) if blk["samp"] else (0, 1)
            return idx_tensor[:, idx, ft, s0:s1, :].to_broadcast([128, blk["nseq"], blk["T"]])

        def gelu_from(src32, skey, out_bf, okeys):
            t1, k1 = T32()
            act(t1[:, 0:NBc[0]], src32, AF.Square, [skey], [k1])
            ts(t1[:, 0:NBc[0]], t1[:, 0:NBc[0]], 0.044715, 1.0, ALU.mult, ALU.add, [k1], [k1])
            tt(t1[:, 0:NBc[0]], t1[:, 0:NBc[0]], src32, ALU.mult, [k1, skey], [k1])
            act(t1[:, 0:NBc[0]], t1[:, 0:NBc[0]], AF.Sigmoid, [k1], [k1], scale=GELU_K)
            tt(out_bf, src32, t1[:, 0:NBc[0]], ALU.mult, [skey, k1], okeys)

        NBc = [512]

        def rmsnorm_mod(l, which, blk, want_router=False):
            NB = blk["NB"]
            shk = 0 if which == 0 else 3
            pb = 0
            for ft in range(FT):
                tq, kq = T32()
                act(tq[:, 0:NB], x_sb[:, ft, 0:NB], AF.Square, [("x", ft)], [kq])
                mm(psum[pb][:, 0:NB], ones_f[:], tq[:, 0:NB], ft == 0, ft == FT - 1, ["ones", kq], [PS(pb)])
            rstd = rbuf[10]
            act(rstd[:, 0:NB], psum[pb][:, 0:NB], AF.Ln, [PS(pb)], [("rb", 10)], bias=1e-6, scale=1.0 / D)
            act(rstd[:, 0:NB], rstd[:, 0:NB], AF.Exp, [("rb", 10)], [("rb", 10)], scale=-0.5)
            for ft in range(FT):
                tq, kq = T32()
                tt(tq[:, 0:NB], x_sb[:, ft, 0:NB], rstd[:, 0:NB], ALU.mult, [("x", ft), ("rb", 10)], [kq])
                tt(v3(tq[:, 0:NB], blk), v3(tq[:, 0:NB], blk), modb(mod_sb, l * 6 + (1 if which == 0 else 4), ft, blk), ALU.mult,
                   [kq, ("mod", l, 1 if which == 0 else 4)], [kq])
                if want_router:
                    tt(v3(tq[:, 0:NB], blk), v3(tq[:, 0:NB], blk), modb(mod_sb, l * 6 + shk, ft, blk), ALU.add,
                       [kq, ("mod", l, shk)], [kq])
                    mm(psum[1][0:8, 0:NB], router_sb[:, ft, :], tq[:, 0:NB], ft == 0, ft == FT - 1,
                       ["router", kq], [PS(1)])
                    cp("act", h_sb[:, ft, 0:NB], tq[:, 0:NB], [kq], [("h", ft)])
                else:
                    tt(v3(h_sb[:, ft, 0:NB], blk), v3(tq[:, 0:NB], blk), modb(mod_sb, l * 6 + shk, ft, blk), ALU.add,
                       [kq, ("mod", l, shk)], [("h", ft)])

        def proj_tile(w_ap, kchunks, rhs_fn, rhs_keys, pb, NB, tag):
            slab, skey = load_slab(w_ap, kchunks, tag)
            for k in range(kchunks):
                mm(psum[pb][:, 0:NB], slab[:, k, :], rhs_fn(k), k == 0, k == kchunks - 1,
                   [skey] + [rhs_keys(k)], [PS(pb)])

        H_ALL = [("h", f) for f in range(FT)]

        def mixer(l, blk):
            NB, nseq, T = blk["NB"], blk["nseq"], blk["T"]
            NBc[0] = NB
            samp = blk["samp"]
            hk = lambda k: ("h", k)
            hf = lambda k: h_sb[:, k, 0:NB]
            for ct in range(8):
                proj_tile(w_in_d[l, :, ct * 128:(ct + 1) * 128], 16, hf, hk, 0, NB, ("in", l, ct))
                stage(27)
                u32 = rbuf[0]
                cp("act", u32[:, 0:NB], psum[0][:, 0:NB], [PS(0)], [("rb", 0)])
                stage(28)
                for jj in range(4):
                    ts(um[jj][:, 0:NB], psum[0][:, 0:NB], pmask[:, jj:jj + 1], None, ALU.mult, None,
                       [PS(0), "pmask"], [("um", jj)])
                stage(21)
                for jj in range(4):
                    for ri in range(2):
                        cp("act", wcp[jj * 2 + ri][:, 32 * jj:32 * jj + 32], cb[:, l, ri, ct * 4 + jj, :],
                           [("cb", l)], [("wcp", jj * 2 + ri)])
                stage(22)
                PWK = ("pw", l)

                def s5_front(jj):
                    par = jj % 2
                    j = ct * 4 + jj
                    pbs = (1, 2) if par == 0 else (4, 5)
                    dma("sp", tabb[par][:, :, :], tab_d[l * 32 + j].rearrange("p (c t) -> p c t", c=2),
                        [("tabd", l, j)], [("tabb", par)], key=("tabl", par))
                    for ri in range(2):
                        mm(psum[pbs[ri]][:, 0:NB], wbT[:, l, ri, ct, :], um[jj][:, 0:NB], True, True,
                           [("wbT", l), ("um", jj)], [PS(pbs[ri])])

                def s5_scan(jj):
                    par = jj % 2
                    j = ct * 4 + jj
                    pbs = (1, 2) if par == 0 else (4, 5)
                    TBK = ("tabb", par)
                    R = lambda i: rbuf[i][:, 0:NB]
                    RK = lambda i: ("rb", i)
                    if samp:
                        cv = tabb[par][:, 0:1, 0:T].to_broadcast([128, nseq, T])
                        sv = tabb[par][:, 1:2, 0:T].to_broadcast([128, nseq, T])
                    else:
                        cv = tabb[par][:, 0:1, 0:T]
                        sv = tabb[par][:, 1:2, 0:T]
                    bre = v3(psum[pbs[0]][:, 0:NB], blk)
                    bim = v3(psum[pbs[1]][:, 0:NB], blk)
                    rho = pw_rho[:, l, j:j + 1]
                    tt(v3(R(2), blk), cv, bre, ALU.mult, [TBK, PS(pbs[0])], [RK(2)])
                    tt(v3(R(3), blk), sv, bim, ALU.mult, [TBK, PS(pbs[1])], [RK(3)])
                    tt(R(4), R(2), R(3), ALU.add, [RK(2), RK(3)], [RK(4)])
                    tt(v3(R(2), blk), cv, bim, ALU.mult, [TBK, PS(pbs[1])], [RK(2)])
                    tt(v3(R(3), blk), sv, bre, ALU.mult, [TBK, PS(pbs[0])], [RK(3)])
                    tt(R(5), R(2), R(3), ALU.subtract, [RK(2), RK(3)], [RK(5)])
                    if samp:
                        ts(v3(R(8), blk), mask01[:], rho, None, ALU.mult, None, ["mask01", ("rho", l)], [RK(8)])
                        g4 = v3(R(4), blk)
                        g5 = v3(R(5), blk)
                        stt(g4[:, :, 0], s5in[:, l, 0, j, :], rho, g4[:, :, 0], ALU.mult, ALU.add,
                            [("s5in", j), ("rho", l), RK(4)], [RK(4)])
                        stt(g5[:, :, 0], s5in[:, l, 1, j, :], rho, g5[:, :, 0], ALU.mult, ALU.add,
                            [("s5in", j), ("rho", l), RK(5)], [RK(5)])
                        ini_re = ini_im = 0.0
                        ikeys = []
                    else:
                        ts(R(8), ones512[:, 0:NB], rho, None, ALU.mult, None, ["ones512", ("rho", l)], [RK(8)])
                        ini_re = s5car[:, l, 0, j, :]
                        ini_im = s5car[:, l, 1, j, :]
                        ikeys = [("s5car", j)]
                    P.op("dve", lambda e, o=R(6), a=R(8), b=R(4), i=ini_re:
                         e.tensor_tensor_scan(out=o, data0=a, data1=b, initial=i, op0=ALU.mult, op1=ALU.add),
                         [RK(8), RK(4)] + ikeys, [RK(6)])
                    P.op("dve", lambda e, o=R(7), a=R(8), b=R(5), i=ini_im:
                         e.tensor_tensor_scan(out=o, data0=a, data1=b, initial=i, op0=ALU.mult, op1=ALU.add),
                         [RK(8), RK(5)] + ikeys, [RK(7)])
                    g6 = v3(R(6), blk)
                    g7 = v3(R(7), blk)
                    tt(v3(R(2), blk), cv, g6, ALU.mult, [TBK, RK(6)], [RK(2)])
                    tt(v3(R(3), blk), sv, g7, ALU.mult, [TBK, RK(7)], [RK(3)])
                    tt(R(4), R(2), R(3), ALU.subtract, [RK(2), RK(3)], [RK(4)])
                    tt(v3(R(2), blk), cv, g7, ALU.mult, [TBK, RK(7)], [RK(2)])
                    tt(v3(R(3), blk), sv, g6, ALU.mult, [TBK, RK(6)], [RK(3)])
                    tt(R(5), R(2), R(3), ALU.add, [RK(2), RK(3)], [RK(5)])
                    return 0

                def s5_back(jj, cur):
                    par = jj % 2
                    j = ct * 4 + jj
                    R = lambda i: rbuf[i][:, 0:NB]
                    RK = lambda i: ("rb", i)
                    hb = hbfs[par]
                    hbk = [("hbf", par, 0), ("hbf", par, 1)]
                    cp("act", hb[0][:, 0:NB], R(4), [RK(4)], [hbk[0]])
                    cp("act", hb[1][:, 0:NB], R(5), [RK(5)], [hbk[1]])
                    h4 = v3(R(4), blk)
                    h5 = v3(R(5), blk)
                    if samp:
                        cp("act", s5out[:, l, 0, j, :], h4[:, :, T - 1], [RK(4)], [("s5in", j)])
                        cp("act", s5out[:, l, 1, j, :], h5[:, :, T - 1], [RK(5)], [("s5in", j)])
                    else:
                        cp("act", s5car[:, l, 0, j, :], h4[:, :, T - 1], [RK(4)], [("s5car", j)])
                        cp("act", s5car[:, l, 1, j, :], h5[:, :, T - 1], [RK(5)], [("s5car", j)])
                    for ri in range(2):
                        mm(psum[3][:, 0:NB], wcp[jj * 2 + ri][:], hb[ri][:, 0:NB], jj == 0 and ri == 0,
                           jj == 3 and ri == 1, [("wcp", jj * 2 + ri), hbk[ri]], [PS(3)])

                s5_front(0)
                for jj in range(4):
                    if jj + 1 < 4:
                        s5_front(jj + 1)
                    s5_scan(jj)
                    s5_back(jj, 0)
                stage(26)
                ypre = rbuf[1]
                stt(ypre[:, 0:NB], u32[:, 0:NB], small_sb[:, l, 152 + ct:153 + ct], psum[3][:, 0:NB], ALU.mult, ALU.add,
                    [("rb", 0), "small", PS(3)], [("rb", 1)])
                gelu_from(ypre[:, 0:NB], ("rb", 1), mix_sb[:, ct, 0:NB], [("mix", ct)])
            stage(3)
            for ct in range(8):
                pb = 1 + ct % 2
                proj_tile(w_glu_d[l, :, ct * 128:(ct + 1) * 128], 8, lambda k: mix_sb[:, k, 0:NB], lambda k: ("mix", k), pb, NB, ("glu", l, ct))
                tg, kg = T32()
                act(tg[:, 0:NB], psum[pb][:, 0:NB], AF.Sigmoid, [PS(pb), "small"], [kg], bias=small_sb[:, l, 80 + ct:81 + ct])
                tt(mix_sb[:, 8 + ct, 0:NB], mix_sb[:, ct, 0:NB], tg[:, 0:NB], ALU.mult, [("mix", ct), kg], [("mix", 8 + ct)])

            stage(4)
            HX = 3
            for j in range(8):
                proj_tile(w_in_d[l, :, 1024 + j * 128:1024 + (j + 1) * 128], 16, hf, hk, 4, NB, ("in", l, 8 + j))
                proj_tile(w_in_d[l, :, 2048 + j * 128:2048 + (j + 1) * 128], 16, hf, hk, 5, NB, ("in", l, 16 + j))
                xe = xrext[:, 0:nseq * (T + HX)].rearrange("p (q t) -> p q t", t=T + HX)
                XK = "xrext"
                cp("act", xe[:, :, HX:HX + T], v3(psum[4][:, 0:NB], blk), [PS(4)], [XK])
                if samp:
                    cp("dve", xe[:, :, 0:HX], cvin[:, l, j, :, :], ["cvin", XK], [XK])
                else:
                    cp("dve", xe[:, :, 0:HX], cvtail[:, l, j, :, :], ["cvtail", XK], [XK])
                gy32 = rbuf[2]
                cp("act", gy32[:, 0:NB], psum[5][:, 0:NB], [PS(5)], [("rb", 2)])
                gelu_from(gy32[:, 0:NB], ("rb", 2), bft[0][:, 0:NB], [("bft", 0)])
                xc = rbuf[3]
                xcv = v3(xc[:, 0:NB], blk)
                cw = lambda k: small_sb[:, l, 88 + k * 8 + j:89 + k * 8 + j]
                ts(xcv, xe[:, :, 0:T], cw(0), small_sb[:, l, 120 + j:121 + j], ALU.mult, ALU.add, [XK, "small"], [("rb", 3)])
                for k in range(1, 4):
                    stt(xcv, xe[:, :, k:k + T], cw(k), xcv, ALU.mult, ALU.add, [XK, "small", ("rb", 3)], [("rb", 3)])
                if samp:
                    cp("dve", cvout[:, l, j, :, :], xe[:, :, T:T + HX], [XK], ["cvin"])
                else:
                    cp("dve", cvtail[:, l, j, :, :], xe[:, :, T:T + HX], [XK], ["cvtail"])
                cp("act", bft[1][:, 0:NB], xc[:, 0:NB], [("rb", 3)], [("bft", 1)])
                mm(psum[6][:, 0:NB], rgw_sb[:, l, 0, j, :], bft[1][:, 0:NB], True, True, ["rgw", ("bft", 1)], [PS(6)])
                mm(psum[7][:, 0:NB], rgw_sb[:, l, 1, j, :], bft[1][:, 0:NB], True, True, ["rgw", ("bft", 1)], [PS(7)])
                r32, i32, a32, e2 = rbuf[4], rbuf[5], rbuf[6], rbuf[7]
                act(r32[:, 0:NB], psum[6][:, 0:NB], AF.Sigmoid, [PS(6), "small"], [("rb", 4)], bias=small_sb[:, l, 136 + j:137 + j])
                act(i32[:, 0:NB], psum[7][:, 0:NB], AF.Sigmoid, [PS(7), "small"], [("rb", 5)], bias=small_sb[:, l, 144 + j:145 + j])
                act(a32[:, 0:NB], r32[:, 0:NB], AF.Exp, [("rb", 4), ("rgc", l)], [("rb", 6)], scale=rgc[:, l, 0, j:j + 1])
                act(e2[:, 0:NB], r32[:, 0:NB], AF.Exp, [("rb", 4), ("rgc", l)], [("rb", 7)], scale=rgc[:, l, 1, j:j + 1])
                act(e2[:, 0:NB], e2[:, 0:NB], AF.Ln, [("rb", 7)], [("rb", 7)], bias=1.0, scale=-1.0)
                act(e2[:, 0:NB], e2[:, 0:NB], AF.Exp, [("rb", 7)], [("rb", 7)], scale=0.5)
                bx = rbuf[8]
                tt(bx[:, 0:NB], i32[:, 0:NB], xc[:, 0:NB], ALU.mult, [("rb", 5), ("rb", 3)], [("rb", 8)])
                tt(bx[:, 0:NB], bx[:, 0:NB], e2[:, 0:NB], ALU.mult, [("rb", 8), ("rb", 7)], [("rb", 8)])
                a3 = v3(a32[:, 0:NB], blk)
                b3 = v3(bx[:, 0:NB], blk)
                if samp:
                    h0 = rgin[:, l, j, :]
                    h0k = ["rgin"]
                else:
                    h0 = rgcar[:, l, j, :]
                    h0k = ["rgcar"]
                tcar = car4[:, 0, 0:nseq]
                tt(tcar, a3[:, :, 0], h0, ALU.mult, [("rb", 6)] + h0k, ["car4"])
                tt(b3[:, :, 0], b3[:, :, 0], tcar, ALU.add, [("rb", 8), "car4"], [("rb", 8)])
                memset("dve", a3[:, :, 0], 0.0, [("rb", 6)])
                hh = rbuf[9]
                P.op("dve", lambda e, o=hh[:, 0:NB], a=a32[:, 0:NB], b=bx[:, 0:NB]:
                     e.tensor_tensor_scan(out=o, data0=a, data1=b, initial=0.0, op0=ALU.mult, op1=ALU.add),
                     [("rb", 6), ("rb", 8)], [("rb", 9)])
                h3 = v3(hh[:, 0:NB], blk)
                if samp:
                    cp("dve", rgout[:, l, j, :], h3[:, :, T - 1], [("rb", 9)], ["rgin"])
                else:
                    cp("dve", rgcar[:, l, j, :], h3[:, :, T - 1], [("rb", 9)], ["rgcar"])
                tt(mix_sb[:, 16 + j, 0:NB], hh[:, 0:NB], bft[0][:, 0:NB], ALU.mult, [("rb", 9), ("bft", 0)], [("mix", 16 + j)])

            stage(5)
            for f in range(FT):
                pb0 = 4 * (f % 2)
                proj_tile(w_gate_d[l, :, f * 128:(f + 1) * 128], 16, hf, hk, pb0, NB, ("ga", l, f))
                proj_tile(w_gate_d[l, :, D + f * 128:D + (f + 1) * 128], 16, hf, hk, pb0 + 1, NB, ("gb", l, f))
                proj_tile(w_bs_d[l, :, f * 128:(f + 1) * 128], 8, lambda k: mix_sb[:, 8 + k, 0:NB], lambda k: ("mix", 8 + k), pb0 + 2, NB, ("bs", l, f))
                proj_tile(w_br_d[l, :, f * 128:(f + 1) * 128], 8, lambda k: mix_sb[:, 16 + k, 0:NB], lambda k: ("mix", 16 + k), pb0 + 3, NB, ("br", l, f))
                sa, ka = T32()
                sb_, kb = T32()
                act(sa[:, 0:NB], psum[pb0][:, 0:NB], AF.Sigmoid, [PS(pb0), "small"], [ka], bias=small_sb[:, l, 48 + f:49 + f])
                act(sb_[:, 0:NB], psum[pb0 + 1][:, 0:NB], AF.Sigmoid, [PS(pb0 + 1), "small"], [kb], bias=small_sb[:, l, 64 + f:65 + f])
                tt(sa[:, 0:NB], sa[:, 0:NB], psum[pb0 + 2][:, 0:NB], ALU.mult, [ka, PS(pb0 + 2)], [ka])
                tt(sb_[:, 0:NB], sb_[:, 0:NB], psum[pb0 + 3][:, 0:NB], ALU.mult, [kb, PS(pb0 + 3)], [kb])
                tt(mrg(f, NB), sa[:, 0:NB], sb_[:, 0:NB], ALU.add, [ka, kb], [("rb", f // 2)])
            for f in range(FT):
                pb = f % 4
                proj_tile(w_out_d[l, :, f * 128:(f + 1) * 128], 16, lambda k: mrg(k, NB), lambda k: ("rb", k // 2), pb, NB, ("out", l, f))
                tq, kq = T32()
                tt(v3(tq[:, 0:NB], blk), v3(psum[pb][:, 0:NB], blk), modb(mod_sb, l * 6 + 2, f, blk), ALU.mult,
                   [PS(pb), ("mod", l, 2)], [kq])
                tt(x_sb[:, f, 0:NB], x_sb[:, f, 0:NB], tq[:, 0:NB], ALU.add, [("x", f), kq], [("x", f)])

        def ffn_half(l, blk, w1_ap, w3_ap, w2_ap, comb_ap, comb_key, wtag):
            NB = blk["NB"]
            hk = lambda k: ("h", k)
            hf = lambda k: h_sb[:, k, 0:NB]
            for t_ in range(24):
                pb = 2 * (t_ % 2)
                proj_tile(w1_ap[:, t_ * 128:(t_ + 1) * 128], 16, hf, hk, pb, NB, None if wtag is None else (wtag, 1, t_))
                proj_tile(w3_ap[:, t_ * 128:(t_ + 1) * 128], 16, hf, hk, pb + 1, NB, None if wtag is None else (wtag, 3, t_))
                tq, kq = T32()
                act(tq[:, 0:NB], psum[pb][:, 0:NB], AF.Silu, [PS(pb)], [kq])
                tt(mix_sb[:, t_, 0:NB], tq[:, 0:NB], psum[pb + 1][:, 0:NB], ALU.mult, [kq, PS(pb + 1)], [("mix", t_)])
            for f in range(FT):
                pb = 4 + f % 3
                s1, k1 = load_slab(w2_ap[0:2048, f * 128:(f + 1) * 128], 16, None if wtag is None else (wtag, 2, f, 0))
                s2, k2 = load_slab(w2_ap[2048:3072, f * 128:(f + 1) * 128], 8, None if wtag is None else (wtag, 2, f, 1))
                for k in range(24):
                    sl, sk = (s1, k1) if k < 16 else (s2, k2)
                    mm(psum[pb][:, 0:NB], sl[:, k % 16, :], mix_sb[:, k, 0:NB], k == 0, k == 23, [sk, ("mix", k)], [PS(pb)])
                tq, kq = T32()
                tt(v3(tq[:, 0:NB], blk), v3(psum[pb][:, 0:NB], blk), modb(mod_sb, l * 6 + 5, f, blk), ALU.mult,
                   [PS(pb), ("mod", l, 5)], [kq])
                if comb_ap is not None:
                    tt(tq[:, 0:NB], tq[:, 0:NB], comb_ap[:, 0:NB], ALU.mult, [kq, comb_key], [kq])
                tt(x_sb[:, f, 0:NB], x_sb[:, f, 0:NB], tq[:, 0:NB], ALU.add, [("x", f), kq], [("x", f)])

        def moe_router(blk):
            NB = blk["NB"]
            nsub = max(1, NB // 128)
            tk = min(128, NB)
            lg_sb = rbuf[0][0:8, :]
            combT = rbuf[1][0:8, :]
            cp("act", lg_sb[:, 0:NB], psum[1][0:8, 0:NB], [PS(1)], [("rb", 0)])
            for s in range(nsub):
                P.op("pe", lambda e, o=psum[2][0:tk, s * 8:(s + 1) * 8], i=lg_sb[:, s * tk:(s + 1) * tk]:
                     e.transpose(o, i, ident[0:8, 0:8]), [("rb", 0), "ident"], [PS(2)])
            cp("act", lgT[0:tk, 0:nsub, :], psum[2][0:tk, 0:nsub * 8].rearrange("p (s e) -> p s e", e=8), [PS(2)], ["lgT"])
            for s in range(nsub):
                P.op("dve", lambda e, o=mx8[0:tk, s, :], i=lgT[0:tk, s, :]: e.max(o, i), ["lgT"], ["mx8"])
                ts(cmb[0:tk, s, :], lgT[0:tk, s, :], mx8[0:tk, s, 0:1], None, ALU.subtract, None, ["lgT", "mx8"], ["cmb"])
                act(cmb[0:tk, s, :], cmb[0:tk, s, :], AF.Exp, ["cmb"], ["cmb"])
                ts(cmbt[0:tk, s, :], lgT[0:tk, s, :], mx8[0:tk, s, 1:2], None, ALU.is_ge, None, ["lgT", "mx8"], ["cmbt"])
                tt(cmb[0:tk, s, :], cmb[0:tk, s, :], cmbt[0:tk, s, :], ALU.mult, ["cmb", "cmbt"], ["cmb"])
                P.op("dve", lambda e, o=den[0:tk, s, :], i=cmb[0:tk, s, :]:
                     e.reduce_sum(o, i, axis=mybir.AxisListType.X), ["cmb"], ["den"])
                P.op("dve", lambda e, o=den[0:tk, s, :]: e.reciprocal(o, o), ["den"], ["den"])
                ts(cmb[0:tk, s, :], cmb[0:tk, s, :], den[0:tk, s, 0:1], None, ALU.mult, None, ["cmb", "den"], ["cmb"])
                P.op("pe", lambda e, o=psum[3][0:8, s * tk:(s + 1) * tk], i=cmb[0:tk, s, :]:
                     e.transpose(o, i, ident[0:tk, 0:tk]), ["cmb", "ident"], [PS(3)])
            cp("act", combT[:, 0:NB], psum[3][0:8, 0:NB], [PS(3)], [("rb", 1)])

        for bi, blk in enumerate(blocks):
            NB = blk["NB"]
            c0 = blk["c0"]
            dma("sp", x_sb[:, :, 0:NB], xT_d[:, :, c0:c0 + NB], (), [("x", f) for f in range(FT)] + ["s5m", "bbar", ("tabst", 0), ("tabst", 1)], key=("xin",))
            for l in range(DEPTH):
                rmsnorm_mod(l, 0, blk)
                stage(2)
                mixer(l, blk)
                stage(6)
                shared_pair = not blk["samp"]
                if l % 2 == 0:
                    rmsnorm_mod(l, 1, blk)
                else:
                    rmsnorm_mod(l, 1, blk, want_router=True)
                    moe_router(blk)
                XK_ALL = [("x", f) for f in range(FT)]
                if shared_pair:
                    dma("sp", xold_d.ap().rearrange("p (f n) -> p f n", n=512), x_sb[:, :, :], XK_ALL, ["xold"], key=("xoldw",))
                if l % 2 == 0:
                    for hh_ in range(1 if shared_pair else 2):
                        ffn_half(l, blk, f_w1_d[hh_], f_w3_d[hh_], f_w2_d[hh_], None, None, ("ffn", hh_) if hh_ == 0 else None)
                else:
                    for e_ in range(4 if shared_pair else 8):
                        cbuf = rbuf[2 + e_ % 2]
                        ck = ("rb", 2 + e_ % 2)
                        msk = rbuf[4 + e_ % 2][0:8, :]
                        mk_ = ("rb", 4 + e_ % 2)
                        ts(msk[:, 0:NB], rbuf[1][0:8, 0:NB], esel[0:8, e_:e_ + 1], None, ALU.mult, None,
                           [("rb", 1), "esel"], [mk_])
                        mm(psum[7][:, 0:NB], ones_f[0:8, :], msk[:, 0:NB], True, True, ["ones", mk_], [PS(7)])
                        cp("act", cbuf[:, 0:NB], psum[7][:, 0:NB], [PS(7)], [ck])
                        ffn_half(l, blk, m_w1_d[e_], m_w3_d[e_], m_w2_d[e_], cbuf, ck, ("moe", e_) if e_ < 4 else None)
                if shared_pair:
                    dma("sp", ccin_d.ap().rearrange("p (f n) -> p f n", n=512), x_sb[:, :, :], XK_ALL, ["ccin"], key=("ccinw",))
                    P.op("pool", lambda e: e.collective_compute("AllReduce", ALU.add, replica_groups=PAIR_GROUPS,
                                                                  ins=[ccin_d.ap().opt()], outs=[ccout_d.ap().opt()]),
                         ["ccin"], ["ccout"], dma_key=("cc",), cc=True)
                    dma("sp", x_sb[:, :, :], ccout_d.ap().rearrange("p (f n) -> p f n", n=512), ["ccout"], XK_ALL, key=("ccoutr",))
                    for ft in range(FT):
                        tq, kq = T32()
                        dma("sp", tq[:, :], xold_d.ap()[:, ft * 512:(ft + 1) * 512], ["xold"], [kq], key=("xoldr", kq[1]))
                        tt(x_sb[:, ft, :], x_sb[:, ft, :], tq[:, :], ALU.subtract, [("x", ft), kq], [("x", ft)])
            pb = 0
            for ft in range(FT):
                tq, kq = T32()
                act(tq[:, 0:NB], x_sb[:, ft, 0:NB], AF.Square, [("x", ft)], [kq])
                mm(psum[pb][:, 0:NB], ones_f[:], tq[:, 0:NB], ft == 0, ft == FT - 1, ["ones", kq], [PS(pb)])
            rstd = rbuf[10]
            act(rstd[:, 0:NB], psum[pb][:, 0:NB], AF.Ln, [PS(pb)], [("rb", 10)], bias=1e-6, scale=1.0 / D)
            act(rstd[:, 0:NB], rstd[:, 0:NB], AF.Exp, [("rb", 10)], [("rb", 10)], scale=-0.5)
            for ft in range(FT):
                tt(x_sb[:, ft, 0:NB], x_sb[:, ft, 0:NB], rstd[:, 0:NB], ALU.mult, [("x", ft), ("rb", 10)], [("x", ft)])
                ts(x_sb[:, ft, 0:NB], x_sb[:, ft, 0:NB], small_sb[:, 0, 32 + ft:33 + ft], None, ALU.mult, None,
                   [("x", ft), "small"], [("x", ft)])
            dma("sp", yT_d[:, :, c0:c0 + NB], x_sb[:, :, 0:NB], [("x", f) for f in range(FT)], (), key=("out",))

    except _Stop:
        pass

    dma("sp", o_s5s_d.rearrange("l r p j q -> p l r j q"), s5out[:], [("s5in", j) for j in range(32)], (), key=("out",))
    dma("sp", o_s5p_d.rearrange("l r p j -> p l r j"), s5car[:, :, :, :, 0], [("s5car", j) for j in range(32)], (), key=("out",))
    dma("sp", o_rgs_d.rearrange("l p j q -> p l j q"), rgout[:], ["rgin"], (), key=("out",))
    dma("sp", o_rgp_d.rearrange("l p j -> p l j"), rgcar[:, :, :, 0], ["rgcar"], (), key=("out",))
    dma("sp", o_cvs_d.rearrange("l p j q k -> p l j q k"), cvout[:], ["cvin"], (), key=("out",))
    dma("sp", o_cvp_d.rearrange("l p j k -> p l j k"), cvtail[:, :, :, 0, :], ["cvtail"], (), key=("out",))

    P.emit(final_wait_keys=[("out",)])
    st.close()
    return nc


def _ft_layout(v):
    k = v.shape[-1] // 128
    return np.ascontiguousarray(v.reshape(k, 128).T)


def _prep_small(inp):
    out = np.zeros((DEPTH, 128, NS_SMALL), np.float32)
    for l in range(DEPTH):
        o = out[l]
        o[:, 0:16] = _ft_layout(inp["norm_mix"][l])
        o[:, 16:32] = _ft_layout(inp["norm_ffn"][l])
        o[:, 32:48] = _ft_layout(inp["norm_f"])
        o[:, 48:80] = _ft_layout(inp["b_gate"][l])
        o[:, 80:88] = _ft_layout(inp["s5_b_glu"][l])
        for k in range(4):
            o[:, 88 + k * 8:96 + k * 8] = _ft_layout(inp["rg_conv_w"][l, k])
        o[:, 120:128] = _ft_layout(inp["rg_conv_b"][l])
        o[:, 128:136] = _ft_layout(inp["rg_lam"][l])
        o[:, 136:144] = _ft_layout(inp["rg_b_a"][l].reshape(-1))
        o[:, 144:152] = _ft_layout(inp["rg_b_i"][l].reshape(-1))
        o[:, 152:160] = _ft_layout(inp["s5_d"][l].reshape(-1))
        o[:, 160:256] = _ft_layout(inp["b_ada"][l])
        sp = lambda a: np.ascontiguousarray(a.reshape(32, 2, 64).transpose(1, 2, 0).reshape(128, 32))
        o[:, 256:288] = sp(inp["s5_lam_re"][l])
        o[:, 288:320] = sp(inp["s5_lam_im"][l])
        o[:, 320:352] = sp(np.repeat(inp["s5_log_dt"][l][:, None], 64, axis=1))
    return out


def _prep_s5m(inp):
    out = np.zeros((DEPTH, 128, 4, 32, 32), np.float32)
    for l in range(DEPTH):
        for idx, name in enumerate(("s5_b_re", "s5_b_im")):
            b = inp[name][l].reshape(32, 2, 64, 16)
            for g2 in range(2):
                out[l, g2 * 64:(g2 + 1) * 64, idx, :, g2 * 16:(g2 + 1) * 16] = b[:, g2].transpose(1, 0, 2)
        for idx, name in enumerate(("s5_c_re", "s5_c_im")):
            c = inp[name][l].reshape(32, 2, 16, 64)
            for g2 in range(2):
                out[l, g2 * 64:(g2 + 1) * 64, 2 + idx, :, g2 * 16:(g2 + 1) * 16] = c[:, g2].transpose(2, 0, 1)
    return out


def _prep_rgw(inp):
    out = np.zeros((DEPTH, 128, 2, 8, 128), np.float32)
    for l in range(DEPTH):
        for a, name in enumerate(("rg_w_a", "rg_w_i")):
            w = inp[name][l]
            for j in range(8):
                for h2 in range(2):
                    out[l, h2 * 64:(h2 + 1) * 64, a, j, h2 * 64:(h2 + 1) * 64] = w[j * 2 + h2]
    return out


_NC_CACHE = {}


def kernel(**inp):
    inp = {k: np.asarray(v) for k, v in inp.items()}
    if "nc" not in _NC_CACHE:
        _NC_CACHE["nc"] = build_program()
    nc = _NC_CACHE["nc"]
    in_maps = _make_in_maps(inp)
    res = run_bass_kernel_spmd(nc, in_maps, core_ids=list(range(8)))
    return _assemble(res.results)


def _make_in_maps(inp):
    small = _prep_small(inp)
    s5m = _prep_s5m(inp)
    rgw = _prep_rgw(inp)
    router = np.ascontiguousarray(inp["moe_router"][0].reshape(FT, 128, 8).transpose(1, 0, 2))
    ident = np.eye(128, dtype=np.float32)
    pmask = np.zeros((128, 4), np.float32)
    for jj in range(4):
        pmask[32 * jj:32 * jj + 32, jj] = 1.0
    shared = dict(small=small, s5m=s5m, rgw=rgw, router=router, ident=ident, pmask=pmask)
    for k in ("w_ada", "w_in", "s5_w_glu", "w_gate", "w_br_s5", "w_br_rg", "w_out"):
        shared[k] = np.ascontiguousarray(inp[k], dtype=np.float32)
    half_maps = []
    for hf in range(2):
        order = [hf, 1 - hf]
        eorder = list(range(4 * hf, 4 * hf + 4)) + list(range(4 * (1 - hf), 4 * (1 - hf) + 4))
        es = np.zeros((8, 8), np.float32)
        for i, e in enumerate(eorder):
            es[e, i] = 1.0
        half_maps.append(dict(
            ffn_w1=np.ascontiguousarray(np.stack([inp["ffn_w1"][0][:, o * 3072:(o + 1) * 3072] for o in order])),
            ffn_w3=np.ascontiguousarray(np.stack([inp["ffn_w3"][0][:, o * 3072:(o + 1) * 3072] for o in order])),
            ffn_w2=np.ascontiguousarray(np.stack([inp["ffn_w2"][0][o * 3072:(o + 1) * 3072, :] for o in order])),
            moe_w1=np.ascontiguousarray(inp["moe_w1"][0][eorder]),
            moe_w3=np.ascontiguousarray(inp["moe_w3"][0][eorder]),
            moe_w2=np.ascontiguousarray(inp["moe_w2"][0][eorder]),
            esel=es))
    in_maps = []
    for c in range(8):
        b = c % 4
        qs = slice(c * NSAMP, (c + 1) * NSAMP)
        xtok = np.concatenate([inp["x_prompt"][b], inp["x_sample"][qs].reshape(NSAMP * TS, D)], axis=0)
        xT = np.ascontiguousarray(xtok.reshape(-1, FT, 128).transpose(2, 1, 0))
        cc = np.concatenate([inp["c_prompt"][b:b + 1], inp["c_sample"][qs]], axis=0)
        cT = np.ascontiguousarray(cc.reshape(-1, FT, 128).transpose(2, 1, 0))
        s5st = np.stack([inp["state_s5_re"][:, qs], inp["state_s5_im"][:, qs]], axis=1)
        s5st = s5st.reshape(DEPTH, 2, NSAMP, 32, 2, 64).transpose(0, 1, 4, 5, 3, 2).reshape(DEPTH, 2, 128, 32, NSAMP)
        rgst = inp["state_rglru"][:, qs].reshape(DEPTH, NSAMP, 8, 128).transpose(0, 3, 2, 1)
        cvst = inp["state_conv"][:, qs].reshape(DEPTH, NSAMP, 3, 8, 128).transpose(0, 4, 3, 1, 2)
        m = dict(shared)
        m.update(half_maps[c // 4])
        m.update(xT=xT, cT=cT, s5st=np.ascontiguousarray(s5st), rgst=np.ascontiguousarray(rgst),
                 cvst=np.ascontiguousarray(cvst))
        in_maps.append(m)
    return in_maps


def _assemble(R):
    B = 4
    y_prompt = np.zeros((B, SEQ, D), np.float32)
    y_sample = np.zeros((8 * NSAMP, TS, D), np.float32)
    p_s5_re = np.zeros((DEPTH, B, 64, 64), np.float32)
    p_s5_im = np.zeros_like(p_s5_re)
    p_rg = np.zeros((DEPTH, B, 1024), np.float32)
    p_conv = np.zeros((DEPTH, B, 3, 1024), np.float32)
    s_s5_re = np.zeros((DEPTH, 8 * NSAMP, 64, 64), np.float32)
    s_s5_im = np.zeros_like(s_s5_re)
    s_rg = np.zeros((DEPTH, 8 * NSAMP, 1024), np.float32)
    s_conv = np.zeros((DEPTH, 8 * NSAMP, 3, 1024), np.float32)
    for c in range(8):
        r = R[c]
        yT = np.asarray(r["yT"])
        ytok = yT.transpose(2, 1, 0).reshape(-1, D)
        qs = slice(c * NSAMP, (c + 1) * NSAMP)
        y_sample[qs] = ytok[SEQ:].reshape(NSAMP, TS, D)
        s5s = np.asarray(r["o_s5s"]).reshape(DEPTH, 2, 2, 64, 32, NSAMP)
        s5s = s5s.transpose(0, 1, 5, 4, 2, 3).reshape(DEPTH, 2, NSAMP, 64, 64)
        s_s5_re[:, qs] = s5s[:, 0]
        s_s5_im[:, qs] = s5s[:, 1]
        s_rg[:, qs] = np.asarray(r["o_rgs"]).transpose(0, 3, 2, 1).reshape(DEPTH, NSAMP, 1024)
        s_conv[:, qs] = np.asarray(r["o_cvs"]).transpose(0, 3, 4, 2, 1).reshape(DEPTH, NSAMP, 3, 1024)
        if c < B:
            y_prompt[c] = ytok[:SEQ]
            s5p = np.asarray(r["o_s5p"]).reshape(DEPTH, 2, 2, 64, 32)
            s5p = s5p.transpose(0, 1, 4, 2, 3).reshape(DEPTH, 2, 64, 64)
            p_s5_re[:, c] = s5p[:, 0]
            p_s5_im[:, c] = s5p[:, 1]
            p_rg[:, c] = np.asarray(r["o_rgp"]).transpose(0, 2, 1).reshape(DEPTH, 1024)
            p_conv[:, c] = np.asarray(r["o_cvp"]).transpose(0, 3, 2, 1).reshape(DEPTH, 3, 1024)
    return (y_prompt, y_sample, p_s5_re, p_s5_im, p_rg, p_conv, s_s5_re, s_s5_im, s_rg, s_conv)
```
